# Optimizing a Trainium2 kernel written in Bass

```python
import math
import jax
import jax.numpy as jnp
from jax import lax
import numpy as np

D_MODEL = 2048
BATCH = 32
SEQ = 256
DEPTH = 4
DEC_BATCH = 2
DEC_SEQ = 2048
PAST_LEN = 256

GRID_W = 64
MIX_WIDTH = D_MODEL
W_A = MIX_WIDTH // 4
W_B = MIX_WIDTH // 2
W_C = MIX_WIDTH - W_A - W_B
DA_QK = 64
DA_V = 2 * DA_QK
DA_HEADS = W_A // DA_V
MLA_Q_RANK = 512
MLA_KV_RANK = 256
MLA_NOPE = 128
MLA_ROPE = 64
MLA_V = 128
MLA_HEADS = W_B // MLA_V
MLA_SCALE = (MLA_NOPE + MLA_ROPE) ** -0.5
NA_DIM = 128
NA_HEADS = W_C // NA_DIM
NA_KH = 8
NA_KW = 16
IN_SPLITS = [W_A, W_A, W_A, MLA_Q_RANK, MLA_KV_RANK, MLA_ROPE, W_C, W_C, W_C]
IN_OFFSETS = [sum(IN_SPLITS[:i + 1]) for i in range(len(IN_SPLITS) - 1)]
D_IN = sum(IN_SPLITS)
D_FF = 5504
N_EXPERTS = 8
TOP_K = 2
D_FF_EXPERT = 2816
N_DENSE = (DEPTH + 1) // 2
N_MOE = DEPTH // 2
ROPE_THETA = 10000.0
LN_EPS = 1e-5
RMS_EPS = 1e-6
NEG_INF = -1e30
QBLK = 128
DEEPNORM_ALPHA = (2.0 * DEPTH) ** 0.25
DEEPNORM_BETA = (8.0 * DEPTH) ** -0.25
F32 = jnp.float32

kernel_name = 'hybrid_diffusion_trunk_step'


def layer_norm(x, g, b):
    xf = x.astype(F32)
    mu = jnp.mean(xf, axis=-1, keepdims=True)
    var = jnp.mean(jnp.square(xf - mu), axis=-1, keepdims=True)
    return ((xf - mu) * lax.rsqrt(var + LN_EPS) * g + b).astype(x.dtype)


def rms_norm(x, g):
    xf = x.astype(F32)
    return (xf * lax.rsqrt(jnp.mean(xf * xf, axis=-1, keepdims=True) + RMS_EPS) * g).astype(x.dtype)


def axial_rope_tables(n_tokens, dim):
    t = jnp.arange(n_tokens)
    row = (t // GRID_W).astype(F32)
    col = (t % GRID_W).astype(F32)
    half = dim // 2
    inv_freq = ROPE_THETA ** (-jnp.arange(0, half, 2, dtype=F32) / half)
    ar = row[:, None] * inv_freq[None, :]
    ac = col[:, None] * inv_freq[None, :]
    ang = jnp.concatenate([ar, ar, ac, ac], axis=-1)
    return jnp.cos(ang), jnp.sin(ang)


def apply_axial_rope(x, cos, sin):
    d = x.shape[-1]
    xr = x.reshape(x.shape[:-1] + (2, 2, d // 4))
    rot = jnp.stack([-xr[..., 1, :], xr[..., 0, :]], axis=-2).reshape(x.shape)
    return (x.astype(F32) * cos + rot.astype(F32) * sin).astype(x.dtype)


def blockwise(f, *qs):
    b, s = qs[0].shape[:2]
    n = s // QBLK
    qb = tuple(jnp.moveaxis(q.reshape((b, n, QBLK) + q.shape[2:]), 1, 0) for q in qs)
    out = lax.map(lambda blk: f(*blk), qb)
    out = jnp.moveaxis(out, 0, 1)
    return out.reshape((b, s) + out.shape[3:])


def softmax_attn(q, k, v):
    s = jnp.einsum('bqhd,bkhd->bhqk', q, k).astype(F32) * (q.shape[-1] ** -0.5)
    p = jax.nn.softmax(s, axis=-1).astype(v.dtype)
    return jnp.einsum('bhqk,bkhd->bqhd', p, v)


def diff_attn(q, k, v, lam, lam_init, g_sub):
    s = jnp.einsum('bqhcd,bkhcd->bhcqk', q, k).astype(F32) * (DA_QK ** -0.5)
    p = jax.nn.softmax(s, axis=-1)
    p = (p[:, :, 0] - lam * p[:, :, 1]).astype(v.dtype)
    o = jnp.einsum('bhqk,bkhd->bqhd', p, v)
    return rms_norm(o, g_sub) * (1.0 - lam_init)


def mla_queries(cq_raw, g_q, w_uq):
    b, s, _ = cq_raw.shape
    q = (rms_norm(cq_raw, g_q) @ w_uq).reshape(b, s, MLA_HEADS, MLA_NOPE + MLA_ROPE)
    return q[..., :MLA_NOPE], q[..., MLA_NOPE:]


def mla_expand(ckv, w_ukv):
    b, t, _ = ckv.shape
    kv = (ckv @ w_ukv).reshape(b, t, MLA_HEADS, MLA_NOPE + MLA_V)
    return kv[..., :MLA_NOPE], kv[..., MLA_NOPE:]


def mla_attn(q_nope, q_rope, k_nope, k_rope, v):
    s = (jnp.einsum('bqhd,bkhd->bhqk', q_nope, k_nope)
         + jnp.einsum('bqhr,bkr->bhqk', q_rope, k_rope)).astype(F32) * MLA_SCALE
    p = jax.nn.softmax(s, axis=-1).astype(v.dtype)
    return jnp.einsum('bhqk,bkhd->bqhd', p, v)


def na_latent(q, k, v, k_ctx, v_ctx, rpb):
    b, s, h, d = q.shape
    rows_n = s // GRID_W
    kh = min(NA_KH, rows_n)
    kw = NA_KW
    scale = d ** -0.5
    qg = q.reshape(b, rows_n, GRID_W, h, d)
    kg = k.reshape(b, rows_n, GRID_W, h, d)
    vg = v.reshape(b, rows_n, GRID_W, h, d)
    r = jnp.arange(rows_n)
    row_start = jnp.clip(r - kh // 2, 0, rows_n - kh)
    rows = row_start[:, None] + jnp.arange(kh)[None, :]
    cq = jnp.arange(GRID_W)
    col_start = jnp.clip(cq - kw // 2, 0, GRID_W - kw)
    kc = jnp.arange(GRID_W)
    col_valid = (kc[None, :] >= col_start[:, None]) & (kc[None, :] < col_start[:, None] + kw)
    k_rows = kg[:, rows]
    v_rows = vg[:, rows]
    s_win = jnp.einsum('brqhd,brkwhd->bhrqkw', qg, k_rows).astype(F32) * scale
    roff = rows - r[:, None] + (NA_KH - 1)
    coff = jnp.clip(kc[None, :] - cq[:, None], -(NA_KW - 1), NA_KW - 1) + (NA_KW - 1)
    bias = rpb[:, roff[:, None, :, None], coff[None, :, None, :]]
    s_win = s_win + bias[None].astype(F32)
    s_win = jnp.where(col_valid[None, None, None, :, None, :], s_win, NEG_INF)
    s_ctx = jnp.einsum('brqhd,bthd->bhrqt', qg, k_ctx).astype(F32) * scale
    n_win = kh * GRID_W
    s_all = jnp.concatenate([s_win.reshape(b, h, rows_n, GRID_W, n_win), s_ctx], axis=-1)
    p = jax.nn.softmax(s_all, axis=-1).astype(v.dtype)
    p_win = p[..., :n_win].reshape(b, h, rows_n, GRID_W, kh, GRID_W)
    p_ctx = p[..., n_win:]
    o = (jnp.einsum('bhrqkw,brkwhd->brqhd', p_win, v_rows)
         + jnp.einsum('bhrqt,bthd->brqhd', p_ctx, v_ctx))
    return o.reshape(b, s, h, d)


def merge_heads(o_a, o_b, o_c, w_out):
    b, s = o_a.shape[:2]
    o = jnp.concatenate([o_a.reshape(b, s, W_A), o_b.reshape(b, s, W_B), o_c.reshape(b, s, W_C)], axis=-1)
    return o @ w_out


def mixer_context(h, w_in, w_out, lam, lam_init, da_subln, mla_gq, mla_gkv, mla_wuq, mla_wukv):
    b, n, _ = h.shape
    da_q, da_k, da_v, cq, ckv_raw, kr, na_q, na_k, na_v = jnp.split(h @ w_in, IN_OFFSETS, axis=-1)
    da_q = da_q.reshape(b, n, DA_HEADS, 2, DA_QK)
    da_k = da_k.reshape(b, n, DA_HEADS, 2, DA_QK)
    da_v = da_v.reshape(b, n, DA_HEADS, DA_V)
    o_a = blockwise(lambda q: diff_attn(q, da_k, da_v, lam, lam_init, da_subln), da_q)
    q_nope, q_rope = mla_queries(cq, mla_gq, mla_wuq)
    ckv = rms_norm(ckv_raw, mla_gkv)
    k_nope, v_b = mla_expand(ckv, mla_wukv)
    o_b = blockwise(lambda qn, qr: mla_attn(qn, qr, k_nope, kr, v_b), q_nope, q_rope)
    na_q = na_q.reshape(b, n, NA_HEADS, NA_DIM)
    na_k = na_k.reshape(b, n, NA_HEADS, NA_DIM)
    na_v = na_v.reshape(b, n, NA_HEADS, NA_DIM)
    o_c = blockwise(lambda q: softmax_attn(q, na_k, na_v), na_q)
    return merge_heads(o_a, o_b, o_c, w_out), (da_k, da_v, ckv, kr, na_k, na_v)


def mixer_latent(h, ck_da, cv_da, c_ckv, c_kr, ck_na, cv_na, rope_da, rope_mla,
                 w_in, w_out, lam, lam_init, da_subln, mla_gq, mla_gkv, mla_wuq, mla_wukv, na_rpb):
    b, s, _ = h.shape
    da_q, da_k, da_v, cq, ckv_raw, kr, na_q, na_k, na_v = jnp.split(h @ w_in, IN_OFFSETS, axis=-1)
    cos_a, sin_a = rope_da
    ca, sa = cos_a[:, None, None, :], sin_a[:, None, None, :]
    da_q = apply_axial_rope(da_q.reshape(b, s, DA_HEADS, 2, DA_QK), ca, sa)
    da_k = apply_axial_rope(da_k.reshape(b, s, DA_HEADS, 2, DA_QK), ca, sa)
    k_all = jnp.concatenate([da_k, ck_da], axis=1)
    v_all = jnp.concatenate([da_v.reshape(b, s, DA_HEADS, DA_V), cv_da], axis=1)
    o_a = blockwise(lambda q: diff_attn(q, k_all, v_all, lam, lam_init, da_subln), da_q)
    cos_m, sin_m = rope_mla
    q_nope, q_rope = mla_queries(cq, mla_gq, mla_wuq)
    q_rope = apply_axial_rope(q_rope, cos_m[:, None, :], sin_m[:, None, :])
    kr = apply_axial_rope(kr, cos_m, sin_m)
    ckv_all = jnp.concatenate([rms_norm(ckv_raw, mla_gkv), c_ckv], axis=1)
    kr_all = jnp.concatenate([kr, c_kr], axis=1)
    k_nope, v_b = mla_expand(ckv_all, mla_wukv)
    o_b = blockwise(lambda qn, qr: mla_attn(qn, qr, k_nope, kr_all, v_b), q_nope, q_rope)
    o_c = na_latent(na_q.reshape(b, s, NA_HEADS, NA_DIM), na_k.reshape(b, s, NA_HEADS, NA_DIM),
                    na_v.reshape(b, s, NA_HEADS, NA_DIM), ck_na, cv_na, na_rpb)
    return merge_heads(o_a, o_b, o_c, w_out)


def swiglu(h, w1, w3, w2):
    return (jax.nn.silu(h @ w1) * (h @ w3)) @ w2


def moe_ffn(h, w_router, w1, w3, w2):
    logits = (h @ w_router).astype(F32)
    top_v, top_i = lax.top_k(logits, TOP_K)
    gates = jax.nn.softmax(top_v, axis=-1)
    dense_gate = jnp.sum(jax.nn.one_hot(top_i, N_EXPERTS, dtype=F32) * gates[..., None], axis=-2).astype(h.dtype)
    out = jnp.zeros_like(h)
    for e in range(N_EXPERTS):
        out = out + dense_gate[..., e:e + 1] * swiglu(h, w1[e], w3[e], w2[e])
    return out


def ada_modulation(cond, w_ada, b_ada):
    m = (jax.nn.silu(cond) @ w_ada + b_ada).reshape(-1, 1, 6 * D_MODEL)
    return jnp.split(m, 6, axis=-1)


def modulate(x, shift, scale):
    return x * (1.0 + scale) + shift


def setup_inputs(seed: int = 0) -> dict:
    key = jax.random.key(seed)
    keys = jax.random.split(key, 40)
    counter = [0]

    def nrm(shape, scale=1.0):
        k = keys[counter[0]]
        counter[0] += 1
        return jax.random.normal(k, shape, jnp.float32) * scale

    def gain(shape):
        return 1.0 + nrm(shape, 0.02)

    D = D_MODEL
    inp = {}
    inp['x_prompt'] = nrm((BATCH, SEQ, D))
    inp['x_sample'] = nrm((DEC_BATCH, DEC_SEQ, D))
    inp['cache_da_k'] = nrm((DEC_BATCH, DEPTH, PAST_LEN, DA_HEADS, 2, DA_QK))
    inp['cache_da_v'] = nrm((DEC_BATCH, DEPTH, PAST_LEN, DA_HEADS, DA_V))
    inp['cache_mla_ckv'] = nrm((DEC_BATCH, DEPTH, PAST_LEN, MLA_KV_RANK))
    inp['cache_mla_krope'] = nrm((DEC_BATCH, DEPTH, PAST_LEN, MLA_ROPE))
    inp['cache_na_k'] = nrm((DEC_BATCH, DEPTH, PAST_LEN, NA_HEADS, NA_DIM))
    inp['cache_na_v'] = nrm((DEC_BATCH, DEPTH, PAST_LEN, NA_HEADS, NA_DIM))
    inp['c'] = nrm((DEC_BATCH, D))
    inp['c_ctx'] = nrm((D,))
    inp['w_ada'] = nrm((DEPTH, D, 6 * D), 0.5 * D ** -0.5)
    inp['b_ada'] = nrm((DEPTH, 6 * D), 0.02)
    inp['w_in'] = nrm((DEPTH, D, D_IN), D ** -0.5)
    inp['da_lq1'] = nrm((DEPTH, DA_QK), 0.1)
    inp['da_lk1'] = nrm((DEPTH, DA_QK), 0.1)
    inp['da_lq2'] = nrm((DEPTH, DA_QK), 0.1)
    inp['da_lk2'] = nrm((DEPTH, DA_QK), 0.1)
    inp['da_subln'] = gain((DEPTH, DA_V))
    inp['mla_gq'] = gain((DEPTH, MLA_Q_RANK))
    inp['mla_gkv'] = gain((DEPTH, MLA_KV_RANK))
    inp['mla_wuq'] = nrm((DEPTH, MLA_Q_RANK, MLA_HEADS * (MLA_NOPE + MLA_ROPE)), MLA_Q_RANK ** -0.5)
    inp['mla_wukv'] = nrm((DEPTH, MLA_KV_RANK, MLA_HEADS * (MLA_NOPE + MLA_V)), MLA_KV_RANK ** -0.5)
    inp['na_rpb'] = nrm((DEPTH, NA_HEADS, 2 * NA_KH - 1, 2 * NA_KW - 1), 0.02)
    inp['w_out'] = nrm((DEPTH, MIX_WIDTH, D), DEEPNORM_BETA * MIX_WIDTH ** -0.5)
    inp['ln1_g'] = gain((DEPTH, D))
    inp['ln1_b'] = nrm((DEPTH, D), 0.02)
    inp['ln2_g'] = gain((DEPTH, D))
    inp['ln2_b'] = nrm((DEPTH, D), 0.02)
    inp['ffn_w1'] = nrm((N_DENSE, D, D_FF), D ** -0.5)
    inp['ffn_w3'] = nrm((N_DENSE, D, D_FF), D ** -0.5)
    inp['ffn_w2'] = nrm((N_DENSE, D_FF, D), DEEPNORM_BETA * D_FF ** -0.5)
    inp['moe_router'] = nrm((N_MOE, D, N_EXPERTS), D ** -0.5)
    inp['moe_w1'] = nrm((N_MOE, N_EXPERTS, D, D_FF_EXPERT), D ** -0.5)
    inp['moe_w3'] = nrm((N_MOE, N_EXPERTS, D, D_FF_EXPERT), D ** -0.5)
    inp['moe_w2'] = nrm((N_MOE, N_EXPERTS, D_FF_EXPERT, D), DEEPNORM_BETA * D_FF_EXPERT ** -0.5)
    return inp


def reference(x_prompt, x_sample, cache_da_k, cache_da_v, cache_mla_ckv, cache_mla_krope,
              cache_na_k, cache_na_v, c, c_ctx, w_ada, b_ada, w_in, da_lq1, da_lk1, da_lq2, da_lk2,
              da_subln, mla_gq, mla_gkv, mla_wuq, mla_wukv, na_rpb, w_out, ln1_g, ln1_b, ln2_g, ln2_b,
              ffn_w1, ffn_w3, ffn_w2, moe_router, moe_w1, moe_w3, moe_w2):
    n_lat = x_sample.shape[1]
    rope_da = axial_rope_tables(n_lat, DA_QK)
    rope_mla = axial_rope_tables(n_lat, MLA_ROPE)
    y_p = x_prompt
    y_s = x_sample
    st_da_k, st_da_v, st_ckv, st_kr, st_na_k, st_na_v = [], [], [], [], [], []
    for l in range(DEPTH):
        lam_init = 0.8 - 0.6 * math.exp(-0.3 * l)
        lam = (jnp.exp(jnp.sum(da_lq1[l].astype(F32) * da_lk1[l].astype(F32)))
               - jnp.exp(jnp.sum(da_lq2[l].astype(F32) * da_lk2[l].astype(F32))) + lam_init)
        sh1_p, sc1_p, g1_p, sh2_p, sc2_p, g2_p = ada_modulation(c_ctx, w_ada[l], b_ada[l])
        sh1_s, sc1_s, g1_s, sh2_s, sc2_s, g2_s = ada_modulation(c, w_ada[l], b_ada[l])
        mix_p, (dk, dv, ckv, kr, nk, nv) = mixer_context(
            modulate(y_p, sh1_p, sc1_p), w_in[l], w_out[l], lam, lam_init, da_subln[l],
            mla_gq[l], mla_gkv[l], mla_wuq[l], mla_wukv[l])
        st_da_k.append(dk)
        st_da_v.append(dv)
        st_ckv.append(ckv)
        st_kr.append(kr)
        st_na_k.append(nk)
        st_na_v.append(nv)
        mix_s = mixer_latent(
            modulate(y_s, sh1_s, sc1_s), cache_da_k[:, l], cache_da_v[:, l], cache_mla_ckv[:, l],
            cache_mla_krope[:, l], cache_na_k[:, l], cache_na_v[:, l], rope_da, rope_mla,
            w_in[l], w_out[l], lam, lam_init, da_subln[l], mla_gq[l], mla_gkv[l], mla_wuq[l],
            mla_wukv[l], na_rpb[l])
        y_p = layer_norm(DEEPNORM_ALPHA * y_p + g1_p * mix_p, ln1_g[l], ln1_b[l])
        y_s = layer_norm(DEEPNORM_ALPHA * y_s + g1_s * mix_s, ln1_g[l], ln1_b[l])
        h_p = modulate(y_p, sh2_p, sc2_p)
        h_s = modulate(y_s, sh2_s, sc2_s)
        i = l // 2
        if l % 2 == 0:
            f_p = swiglu(h_p, ffn_w1[i], ffn_w3[i], ffn_w2[i])
            f_s = swiglu(h_s, ffn_w1[i], ffn_w3[i], ffn_w2[i])
        else:
            f_p = moe_ffn(h_p, moe_router[i], moe_w1[i], moe_w3[i], moe_w2[i])
            f_s = moe_ffn(h_s, moe_router[i], moe_w1[i], moe_w3[i], moe_w2[i])
        y_p = layer_norm(DEEPNORM_ALPHA * y_p + g2_p * f_p, ln2_g[l], ln2_b[l])
        y_s = layer_norm(DEEPNORM_ALPHA * y_s + g2_s * f_s, ln2_g[l], ln2_b[l])
    new_da_k = jnp.stack(st_da_k, axis=1)
    new_da_v = jnp.stack(st_da_v, axis=1)
    new_mla_ckv = jnp.stack(st_ckv, axis=1)
    new_mla_krope = jnp.stack(st_kr, axis=1)
    new_na_k = jnp.stack(st_na_k, axis=1)
    new_na_v = jnp.stack(st_na_v, axis=1)
    return (y_p, y_s, new_da_k, new_da_v, new_mla_ckv, new_mla_krope, new_na_k, new_na_v)
```

```python
import math
import numpy as np
import concourse.bass as bass
import concourse.mybir as mybir
from concourse.bass_utils import run_bass_kernel_spmd

F32 = mybir.dt.float32
BF16 = mybir.dt.bfloat16
AF = mybir.ActivationFunctionType
ALU = mybir.AluOpType

L = 4
NT = 2048
NKEY = 2304
ALPHA = 8.0 ** 0.25
LAM_INIT = [0.8 - 0.6 * math.exp(-0.3 * l) for l in range(L)]
NBIG = -32768.0
NA_CH = []
for _qt in range(4):
    NA_CH.append(list(range(max(0, 4 * _qt - 2), min(16, 4 * _qt + 6))) + [16, 17])
NA_PAIR = {}
for _qt in range(4):
    for _kc in NA_CH[_qt]:
        NA_PAIR[(_qt, _kc)] = len(NA_PAIR)
NPAIR = len(NA_PAIR)
GF = 3


class Tok:
    __slots__ = ("sem", "val")

    def __init__(self, sem, val):
        self.sem = sem
        self.val = val


class Buf:
    def __init__(self, name=""):
        self.name = name
        self.w = None
        self.r = {}


class Eng:
    def __init__(self, e, sem, is_pe=False):
        self.e = e
        self.sem = sem
        self.cnt = 0
        self.known = {}
        self.is_pe = is_pe

    def wait(self, tok):
        if tok is None:
            return
        if self.is_pe and tok.sem is self.sem:
            return
        k = id(tok.sem)
        if self.known.get(k, 0) >= tok.val:
            return
        self.e.wait_ge(tok.sem, tok.val)
        self.nwait = getattr(self, 'nwait', 0) + 1
        self.known[k] = tok.val


def _deps(E, reads, writes):
    for b in reads:
        E.wait(b.w)
    for b in writes:
        E.wait(b.w)
        for t in list(b.r.values()):
            E.wait(t)


def _commit(tok, reads, writes):
    for b in reads:
        b.r[id(tok.sem)] = tok
    for b in writes:
        b.w = tok
        b.r = {}


class K:
    pass


def op(E, fn, reads=(), writes=()):
    _deps(E, reads, writes)
    ins = fn(E.e)
    E.cnt += 1
    ins.then_inc(E.sem, 1)
    tok = Tok(E.sem, E.cnt)
    _commit(tok, reads, writes)
    return tok


def mm(mms, reads=(), writes=()):
    PE = K.PE
    _deps(PE, reads, writes)
    ins = None
    for (o, l, r, st, sp) in mms:
        ins = PE.e.matmul(o, lhsT=l, rhs=r, start=st, stop=sp)
        PE.nmm = getattr(PE, 'nmm', 0) + 1
    PE.cnt += 1
    ins.then_inc(PE.sem, 1)
    tok = Tok(PE.sem, PE.cnt)
    _commit(tok, reads, writes)
    return tok


class DQ:
    def __init__(self, e, sems):
        self.E = Eng(e, None)
        self.slots = [[s, 0] for s in sems]
        self.i = 0


def dma(Q, out, in_, reads=(), writes=()):
    E = Q.E
    _deps(E, reads, writes)
    sl = Q.slots[Q.i]
    Q.i = (Q.i + 1) % len(Q.slots)
    if sl[1] > 0:
        E.wait(Tok(sl[0], sl[1]))
    ins = E.e.dma_start(out=out, in_=in_)
    E.ndma = getattr(E, 'ndma', 0) + 1
    sl[1] += 16
    ins.then_inc(sl[0], 16)
    tok = Tok(sl[0], sl[1])
    _commit(tok, reads, writes)
    return tok


def barrier():
    toks = [Tok(E.sem, E.cnt) for E in (K.PE, K.ACT, K.DVE) if E.cnt > 0]
    for Q in (K.SP, K.GP):
        for s in Q.slots:
            if s[1] > 0:
                toks.append(Tok(s[0], s[1]))
    for E in (K.PE, K.ACT, K.DVE, K.SP.E, K.GP.E):
        for t in toks:
            E.wait(t)


class Arena:
    def __init__(self, nc, nbytes):
        self.f = nc.alloc_sbuf_tensor("arena", [128, nbytes // 4], F32)
        self.b = self.f.bitcast(BF16)
        self.off = 0
        self.cap = nbytes
        self.peak = 0

    def alloc(self, shape, dt):
        esz = 4 if dt == F32 else 2
        n = 1
        for s in shape[1:]:
            n *= s
        nb = (n * esz + 31) // 32 * 32
        off = self.off
        self.off += nb
        self.peak = max(self.peak, self.off)
        assert self.off <= self.cap, ("arena overflow", self.off, self.cap)
        if dt == F32:
            ap = self.f[0:shape[0], off // 4: off // 4 + n]
        else:
            ap = self.b[0:shape[0], off // 2: off // 2 + n]
        if len(shape) == 3:
            ap = ap.rearrange("p (a b) -> p a b", a=shape[1])
        return ap


class Pool:
    def __init__(self, shape, dt, n, name):
        self.items = [(K.A.alloc(shape, dt), Buf(name + str(i))) for i in range(n)]
        self.i = 0

    def get(self):
        it = self.items[self.i]
        self.i = (self.i + 1) % len(self.items)
        return it


def psbank(pin=False):
    while True:
        i = K.psi
        K.psi = (K.psi + 1) % len(K.PS)
        if i not in K.pinned:
            break
    if pin:
        K.pinned.add(i)
    return K.PS[i]


def loadw(pool, src):
    ap, b = pool.get()
    dma(K.GP, ap, src, writes=[b])
    return ap, b


def alt_eng():
    K.alt ^= 1
    return K.ACT if K.alt else K.DVE


def affine(E, out, in_, sc, bi, reads, writes):
    if E is K.ACT:
        op(E, lambda e: e.activation(out=out, in_=in_, func=AF.Identity, bias=bi, scale=sc), reads, writes)
    else:
        op(E, lambda e: e.tensor_scalar(out=out, in0=in_, scalar1=sc, scalar2=bi, op0=ALU.mult, op1=ALU.add),
           reads, writes)


def layernorm(zc, zbuf, pzb, tM, tV):
    ACT, DVE = K.ACT, K.DVE
    (S1, S1b) = psbank(pin=True)
    (S2, S2b) = psbank(pin=True)
    for c in range(16):
        zb, zbb = pzb.get()
        op(ACT, lambda e: e.activation(out=zb, in_=zc(c), func=AF.Copy), [zbuf], [zbb])
        mm([(S1[:, :], K.ones_bf[:, :], zb, c == 0, c == 15)], [zbb, K.cbuf], [S1b])
        zq, zqb = pzb.get()
        op(ACT, lambda e: e.activation(out=zq, in_=zc(c), func=AF.Square), [zbuf], [zqb])
        mm([(S2[:, :], K.ones_bf[:, :], zq, c == 0, c == 15)], [zqb, K.cbuf], [S2b])
    (M, Mb) = tM
    (V, Vb) = tV
    op(DVE, lambda e: e.tensor_scalar(out=M, in0=S1[:, :], scalar1=1.0 / 2048, scalar2=None, op0=ALU.mult), [S1b], [Mb])
    op(DVE, lambda e: e.tensor_tensor(out=V, in0=M, in1=M, op=ALU.mult), [Mb], [Vb])
    op(DVE, lambda e: e.scalar_tensor_tensor(out=V, in0=S2[:, :], scalar=1.0 / 2048, in1=V, op0=ALU.mult,
                                             op1=ALU.subtract), [S2b, Vb], [Vb])
    op(ACT, lambda e: e.activation(out=V, in_=V, func=AF.Sqrt, bias=1e-5, scale=1.0), [Vb], [Vb])
    op(DVE, lambda e: e.reciprocal(out=V, in_=V), [Vb], [Vb])
    for c in range(16):
        op(DVE, lambda e: e.tensor_tensor(out=zc(c), in0=zc(c), in1=M, op=ALU.subtract), [zbuf, Mb], [zbuf])
        op(DVE, lambda e: e.tensor_tensor(out=zc(c), in0=zc(c), in1=V, op=ALU.mult), [zbuf, Vb], [zbuf])
    K.pinned.clear()


def attention(ncomp, kchunks, score_fn, score_reads, Vt, Vb, scale, bias_fn, ppT, tmpf):
    ACT, DVE = K.ACT, K.DVE
    res = []
    for comp in range(ncomp):
        (O, Ob) = psbank(pin=True)
        (R, Rb) = psbank(pin=True)
        n = len(kchunks)
        sc = {}

        def emit_scores(i):
            (S, Sb) = psbank()
            pairs = score_fn(comp, kchunks[i])
            mm([(S[0:128, :], l, r, j == 0, j == len(pairs) - 1) for j, (l, r) in enumerate(pairs)],
               score_reads, [Sb])
            sc[i] = (S, Sb)

        emit_scores(0)
        for i in range(n):
            if i + 1 < n:
                emit_scores(i + 1)
            (S, Sb) = sc.pop(i)
            kc = kchunks[i]
            pT, pTb = ppT.get()
            if bias_fn is None:
                op(ACT, lambda e: e.activation(out=pT, in_=S[:, :], func=AF.Exp, scale=scale), [Sb], [pTb])
            else:
                bt, btb = bias_fn(kc)
                tf, tfb = tmpf.get()
                op(DVE, lambda e: e.scalar_tensor_tensor(out=tf, in0=S[:, :], scalar=scale, in1=bt, op0=ALU.mult,
                                                         op1=ALU.add), [Sb, btb], [tfb])
                op(ACT, lambda e: e.activation(out=pT, in_=tf, func=AF.Exp), [tfb], [pTb])
            mm([(O[:, :], Vt[:, kc, :], pT, i == 0, i == n - 1)], [pTb, Vb], [Ob])
            mm([(R[:, :], K.ones_bf[:, :], pT, i == 0, i == n - 1)], [pTb, K.cbuf], [Rb])
        res.append((O, Ob, R, Rb))
    return res


def build(nlayers=L, Lw=L, stop=None, moe_alloc=True, stop_layer=0, n_exp=8):
    nc = bass.Bass("TRN2", target_bir_lowering=False)

    def din(name, shape):
        return nc.dram_tensor(name, list(shape), F32, kind="ExternalInput").ap()

    def dout(name, shape):
        return nc.dram_tensor(name, list(shape), F32, kind="ExternalOutput").ap()

    xT = din("xT", [128, 16, NT])
    cond = din("cond", [128, 16])
    w_ada = din("w_ada", [Lw, 96, 128, 16, 128])
    b_ada = din("b_ada", [Lw, 128, 96])
    w64 = din("w64", [Lw, 34, 128, 16, 64])
    w128 = din("w128", [Lw, 22, 128, 16, 128])
    wuq_n = din("wuq_n", [Lw, 8, 128, 4, 128])
    wuq_r = din("wuq_r", [Lw, 8, 2, 128, 4, 64])
    wukv = din("wukv", [Lw, 8, 2, 128, 2, 128])
    w_out = din("w_out", [Lw, 16, 128, 16, 128])
    ffn_w1 = din("ffn_w1", [(Lw + 1) // 2, 43, 128, 16, 128])
    ffn_w3 = din("ffn_w3", [(Lw + 1) // 2, 43, 128, 16, 128])
    ffn_w2 = din("ffn_w2", [(Lw + 1) // 2, 43, 128, 2048])
    moe_w1 = din("moe_w1", [max(1, Lw // 2), n_exp, 22, 128, 16, 128] if moe_alloc else [1, 1, 1, 128, 16, 128])
    moe_w3 = din("moe_w3", [max(1, Lw // 2), n_exp, 22, 128, 16, 128] if moe_alloc else [1, 1, 1, 128, 16, 128])
    moe_w2 = din("moe_w2", [max(1, Lw // 2), n_exp, 22, 128, 2048] if moe_alloc else [1, 1, 1, 128, 2048])
    router = din("router", [max(1, Lw // 2), 128, 16, 8])
    lnp = din("lnp", [128, L * 4 * 16])
    gq = din("gq", [128, L * 4])
    gkv = din("gkv", [128, L * 2])
    subln = din("subln", [128, L])
    lamp = din("lamp", [64, 4 * L])
    ropec = din("ropec", [64, NT])
    ropes = din("ropes", [64, NT])
    kmask = din("kmask", [9, NKEY])
    qmask = din("qmask", [9, NT])
    c_dak = din("c_dak", [Lw, 4, 2, 64, 256])
    c_dav = din("c_dav", [Lw, 4, 128, 2, 128])
    c_ckv = din("c_ckv", [Lw, 128, 2, 256])
    c_kr = din("c_kr", [Lw, 64, 256])
    c_nak = din("c_nak", [Lw, 4, 128, 256])
    c_nav = din("c_nav", [Lw, 4, 128, 2, 128])
    nabias = din("nabias", [Lw, 4, NPAIR, 128, 512])
    ident_d = din("ident", [128, 128])

    yT = dout("yT", [128, 16, NT])
    o_dak = dout("o_dak", [L, 4, 2, 64, NT])
    o_dav = dout("o_dav", [L, 4, 128, 16, 128])
    o_ckv = dout("o_ckv", [L, 2, 128, NT])
    o_kr = dout("o_kr", [L, 64, NT])
    o_nak = dout("o_nak", [L, 4, 128, NT])
    o_nav = dout("o_nav", [L, 4, 128, 16, 128])
    tS = nc.dram_tensor("tS", [128, 16, NT], F32).ap()
    dbg1 = dout("dbg1", [128, 16, NT]) if stop is not None else None
    dbg2 = dout("dbg2", [128, 16, NT]) if stop is not None else None

    def finish():
        barrier()
        for c in range(16):
            dma(GP, dbg1[:, c, :], hT[:, c, :])
            dma(GP, dbg2[:, c, :], OT[:, c, :])
        barrier()
        return nc
    tSb = [Buf("tS%d" % i) for i in range(4)]

    sems = [nc.alloc_semaphore("s%d" % i) for i in range(3 + 16)]
    K.PE = Eng(nc.tensor, sems[0], is_pe=True)
    K.ACT = Eng(nc.scalar, sems[1])
    K.DVE = Eng(nc.vector, sems[2])
    K.SP = DQ(nc.sync, sems[3:11])
    K.GP = DQ(nc.gpsimd, sems[11:19])
    K.alt = 0
    PE, ACT, DVE, SP, GP = K.PE, K.ACT, K.DVE, K.SP, K.GP

    K.PS = []
    for i in range(8):
        t = nc.alloc_psum_tensor("ps%d" % i, [128, 512], F32)
        K.PS.append((t, Buf("ps%d" % i)))
    K.psi = 0
    K.pinned = set()

    A = Arena(nc, 203 * 1024)
    K.A = A
    hT = A.alloc([128, 16, NT], BF16)
    hTb = [Buf("hT%d" % i) for i in range(4)]
    R2 = A.off
    OT = A.alloc([128, 16, NT], BF16)
    OTb = [Buf("OT%d" % i) for i in range(4)]
    A.off = R2
    acc = A.alloc([128, 16, 1024], F32)
    accb = [Buf("acc%d" % i) for i in range(2)]
    K.cbuf = Buf("consts")
    cb = K.cbuf
    K.ones_bf = A.alloc([128, 128], BF16)
    ones_f = A.alloc([128, 128], F32)
    ident = A.alloc([128, 128], F32)
    mod = A.alloc([128, L * 96], F32)
    lnp_s = A.alloc([128, L * 64], F32)
    gq_s = A.alloc([128, L * 4], F32)
    gkv_s = A.alloc([128, L * 2], F32)
    sub_s = A.alloc([128, L], F32)
    nlam = A.alloc([128, L], F32)
    lam_t = A.alloc([128, 4 * L], F32)
    coef = A.alloc([128, 8 * 16], F32)
    coefb = Buf("coef")
    scond = A.alloc([128, 16], BF16)
    condf = A.alloc([128, 16], F32)
    gate_tm = A.alloc([128, 16 * 8], F32)
    gateb = Buf("gate")
    wr = A.alloc([128, 16 * 8], F32)
    wrA = A.alloc([128, 16 * 8], F32)
    wrB = A.alloc([128, 16 * 8], F32)
    wrb = Buf("wr")
    cstb = A.alloc([128, 8], F32)
    R3 = A.off

    op(DVE, lambda e: e.memset(K.ones_bf, 1.0), [], [cb])
    op(DVE, lambda e: e.memset(ones_f, 1.0), [], [cb])
    dma(SP, ident, ident_d, writes=[cb])
    dma(SP, lnp_s, lnp, writes=[cb])
    dma(SP, gq_s, gq, writes=[cb])
    dma(SP, gkv_s, gkv, writes=[cb])
    dma(SP, sub_s, subln, writes=[cb])
    dma(SP, condf, cond, writes=[cb])
    dma(SP, lam_t[0:64, :], lamp, writes=[cb])
    op(ACT, lambda e: e.activation(out=scond, in_=condf, func=AF.Silu), [cb], [cb])
    prods = A.alloc([128, 2 * L], F32)
    for l in range(L):
        for j in range(2):
            op(DVE, lambda e, l=l, j=j: e.tensor_tensor(out=prods[0:64, 2 * l + j:2 * l + j + 1],
                                                        in0=lam_t[0:64, 4 * l + 2 * j:4 * l + 2 * j + 1],
                                                        in1=lam_t[0:64, 4 * l + 2 * j + 1:4 * l + 2 * j + 2],
                                                        op=ALU.mult), [cb], [cb])
    (P0, P0b) = psbank()
    mm([(P0[:, 0:2 * L], ones_f[0:64, :], prods[0:64, :], True, True)], [cb], [P0b])
    op(ACT, lambda e: e.activation(out=prods, in_=P0[:, 0:2 * L], func=AF.Exp), [P0b], [cb])
    for l in range(L):
        op(DVE, lambda e, l=l: e.scalar_tensor_tensor(out=nlam[:, l:l + 1], in0=prods[:, 2 * l + 1:2 * l + 2],
                                                      scalar=-LAM_INIT[l], in1=prods[:, 2 * l:2 * l + 1],
                                                      op0=ALU.add, op1=ALU.subtract), [cb], [cb])
    m0 = A.off
    pada = Pool([128, 16, 128], BF16, 3, "wada")
    bada = A.alloc([128, 96], F32)
    for l in range(nlayers):
        (P1, P1b) = psbank()
        for j in range(96):
            wa, wab = loadw(pada, w_ada[l, j])
            mm([(P1[:, j:j + 1], wa[:, kc, :], scond[:, kc:kc + 1], kc == 0, kc == 15) for kc in range(16)],
               [wab, cb], [P1b])
        dma(SP, bada, b_ada[l], writes=[cb])
        op(DVE, lambda e, l=l: e.tensor_tensor(out=mod[:, l * 96:(l + 1) * 96], in0=P1[:, 0:96], in1=bada,
                                               op=ALU.add), [P1b, cb], [cb])
    barrier()
    A.off = m0

    def cvec(k):
        return coef[:, k * 16:(k + 1) * 16]

    def make_coefs(l, sub):
        base = l * 96 + (0 if sub == 0 else 48)
        sh = mod[:, base:base + 16]
        scl = mod[:, base + 16:base + 32]
        o0 = 0 if sub == 0 else 4
        if sub == 0 and l == 0:
            op(DVE, lambda e: e.tensor_scalar(out=cvec(0), in0=scl, scalar1=1.0, scalar2=None, op0=ALU.add),
               [cb, coefb], [coefb])
            op(DVE, lambda e: e.tensor_copy(out=cvec(1), in_=sh), [cb, coefb], [coefb])
            op(DVE, lambda e: e.memset(cvec(2), ALPHA), [coefb], [coefb])
            op(DVE, lambda e: e.memset(cvec(3), 0.0), [coefb], [coefb])
            return
        if sub == 0:
            Gp = lnp_s[:, (l - 1) * 64 + 32:(l - 1) * 64 + 48]
            Bp = lnp_s[:, (l - 1) * 64 + 48:(l - 1) * 64 + 64]
        else:
            Gp = lnp_s[:, l * 64:l * 64 + 16]
            Bp = lnp_s[:, l * 64 + 16:l * 64 + 32]
        op(DVE, lambda e: e.scalar_tensor_tensor(out=cvec(o0 + 0), in0=scl, scalar=1.0, in1=Gp, op0=ALU.add,
                                                 op1=ALU.mult), [cb, coefb], [coefb])
        op(DVE, lambda e: e.scalar_tensor_tensor(out=cvec(o0 + 1), in0=scl, scalar=1.0, in1=Bp, op0=ALU.add,
                                                 op1=ALU.mult), [cb, coefb], [coefb])
        op(DVE, lambda e: e.tensor_tensor(out=cvec(o0 + 1), in0=cvec(o0 + 1), in1=sh, op=ALU.add),
           [cb, coefb], [coefb])
        op(DVE, lambda e: e.tensor_scalar(out=cvec(o0 + 2), in0=Gp, scalar1=ALPHA, scalar2=None, op0=ALU.mult),
           [cb, coefb], [coefb])
        op(DVE, lambda e: e.tensor_scalar(out=cvec(o0 + 3), in0=Bp, scalar1=ALPHA, scalar2=None, op0=ALU.mult),
           [cb, coefb], [coefb])

    def col(v, c):
        return v[:, c:c + 1]

    for l in range(nlayers):
        is_moe = (l % 2 == 1)
        fi = l // 2
        src = xT if l == 0 else tS
        make_coefs(l, 0)
        make_coefs(l, 1)
        g1v = mod[:, l * 96 + 32:l * 96 + 48]
        g2v = mod[:, l * 96 + 80:l * 96 + 96]

        m0 = A.off
        tt = A.alloc([128, 16, 512], F32)
        ttb = Buf("tt")
        for qt in range(4):
            dma(SP, tt, src[:, :, qt * 512:(qt + 1) * 512], reads=[tSb[qt]], writes=[ttb])
            for c in range(16):
                affine(alt_eng(), hT[:, c, qt * 512:(qt + 1) * 512], tt[:, c, :], col(cvec(0), c), col(cvec(1), c),
                       [ttb, coefb], [hTb[qt]])
        barrier()
        A.off = m0
        if stop == "a" and l == stop_layer:
            return finish()

        m0 = A.off
        rc = A.alloc([64, 512], F32)
        rs = A.alloc([64, 512], F32)
        rcb = Buf("rope")
        Qc = [A.alloc([128, 512], BF16) for _ in range(2)]
        Qb = Buf("Q")
        Kc = [A.alloc([128, NKEY], BF16) for _ in range(2)]
        Kb = Buf("K")
        Vt = A.alloc([128, 18, 128], BF16)
        Vb = Buf("V")
        tA = A.alloc([128, 512], F32)
        tB = A.alloc([128, 512], F32)
        tC = A.alloc([128, 512], BF16)
        tAb, tBb, tCb = Buf("tA"), Buf("tB"), Buf("tC")
        pkf = Pool([128, 512], F32, 2, "kf")
        ppT = Pool([128, 512], BF16, 4, "pT")
        p64 = Pool([128, 16, 64], BF16, 3, "w64")
        p128 = Pool([128, 16, 128], BF16, 2, "w128")
        for c in range(2):
            dma(GP, Kc[c][64:73, :], kmask, writes=[Kb])
            dma(GP, Qc[c][64:73, :], qmask[:, 0:512], writes=[Qb])

        def rope_proj(l, blk, qt, rhs_of, nk, wsrc_plain, wsrc_rot, pool, M, out_bf, out_bf_b, out_f=None,
                      rhs_reads=()):
            wp, wpb = loadw(pool, wsrc_plain)
            wq, wqb = loadw(pool, wsrc_rot)
            (Pa, Pab) = psbank()
            (Pb, Pbb) = psbank()
            mm([(Pa[0:M, :], wp[:, kc, :], rhs_of(kc), kc == 0, kc == nk - 1) for kc in range(nk)],
               [wpb] + list(rhs_reads), [Pab])
            mm([(Pb[0:M, :], wq[:, kc, :], rhs_of(kc), kc == 0, kc == nk - 1) for kc in range(nk)],
               [wqb] + list(rhs_reads), [Pbb])
            op(DVE, lambda e: e.tensor_tensor(out=tA[0:M, :], in0=Pa[0:M, :], in1=rc[0:M, :], op=ALU.mult),
               [Pab, rcb], [tAb])
            op(DVE, lambda e: e.tensor_tensor(out=tB[0:M, :], in0=Pb[0:M, :], in1=rs[0:M, :], op=ALU.mult),
               [Pbb, rcb], [tBb])
            if out_f is None:
                op(DVE, lambda e: e.tensor_tensor(out=out_bf, in0=tA[0:M, :], in1=tB[0:M, :], op=ALU.add),
                   [tAb, tBb], [out_bf_b])
            else:
                (of, ofb) = out_f
                op(DVE, lambda e: e.tensor_tensor(out=of[0:M, :], in0=tA[0:M, :], in1=tB[0:M, :], op=ALU.add),
                   [tAb, tBb], [ofb])
                op(ACT, lambda e: e.activation(out=out_bf, in_=of[0:M, :], func=AF.Copy), [ofb], [out_bf_b])

        def load_rope(qt):
            dma(SP, rc, ropec[:, qt * 512:(qt + 1) * 512], writes=[rcb])
            dma(SP, rs, ropes[:, qt * 512:(qt + 1) * 512], writes=[rcb])

        def proj_v_tm(wv, wvb, lhs_of, nk, lhs_reads, hd, o_dst):
            for g in range(4):
                (Pv, Pvb) = psbank()
                mms = []
                for s in range(4):
                    tcn = g * 4 + s
                    for kc in range(nk):
                        mms.append((Pv[:, s * 128:(s + 1) * 128], lhs_of(kc, tcn), wv[:, kc, :], kc == 0, kc == nk - 1))
                mm(mms, [wvb] + list(lhs_reads), [Pvb])
                if o_dst is not None:
                    vo, vob = pkf.get()
                    op(DVE, lambda e: e.tensor_copy(out=vo, in_=Pv[:, :]), [Pvb], [vob])
                    dma(SP, o_dst[:, g * 4:(g + 1) * 4, :], vo.rearrange("p (a b) -> p a b", a=4), reads=[vob])
                if o_dst is not None:
                    op(ACT, lambda e: e.activation(out=Vt[:, g * 4:(g + 1) * 4, :],
                                                   in_=vo.rearrange("p (a b) -> p a b", a=4), func=AF.Copy),
                       [vob], [Vb])
                else:
                    op(ACT, lambda e: e.activation(out=Vt[:, g * 4:(g + 1) * 4, :],
                                                   in_=Pv[:, :].rearrange("p (a b) -> p a b", a=4), func=AF.Copy),
                       [Pvb], [Vb])

        for hd in range(4):
            for qt in range(4):
                load_rope(qt)
                for c in range(2):
                    blk = 16 + hd * 4 + c * 2
                    kf = pkf.get()
                    rope_proj(l, blk, qt, lambda kc, qt=qt: hT[:, kc, qt * 512:(qt + 1) * 512], 16,
                              w64[l, blk], w64[l, blk + 1], p64, 64,
                              Kc[c][0:64, qt * 512:(qt + 1) * 512], Kb, out_f=kf, rhs_reads=[hTb[qt]])
                    dma(SP, o_dak[l, hd, c, :, qt * 512:(qt + 1) * 512], kf[0][0:64, :], reads=[kf[1]])
            for c in range(2):
                dma(GP, Kc[c][0:64, 2048:NKEY], c_dak[l, hd, c], writes=[Kb])
            wv, wvb = loadw(p128, w128[l, 14 + hd])
            proj_v_tm(wv, wvb, lambda kc, tcn: hT[:, kc, tcn * 128:(tcn + 1) * 128], 16, hTb, hd, o_dav[l, hd])
            dma(GP, Vt[:, 16:18, :], c_dav[l, hd], writes=[Vb])
            for qt in range(4):
                load_rope(qt)
                for c in range(2):
                    blk = hd * 4 + c * 2
                    rope_proj(l, blk, qt, lambda kc, qt=qt: hT[:, kc, qt * 512:(qt + 1) * 512], 16,
                              w64[l, blk], w64[l, blk + 1], p64, 64, Qc[c][0:64, :], Qb, rhs_reads=[hTb[qt]])
                    if qt > 0 or hd > 0 or True:
                        dma(GP, Qc[c][64:73, :], qmask[:, qt * 512:(qt + 1) * 512], writes=[Qb])
                res = attention(2, list(range(18)),
                                lambda comp, kc: [(Kc[comp][0:73, kc * 128:(kc + 1) * 128], Qc[comp][0:73, :])],
                                [Kb, Qb], Vt, Vb, 0.125, None, ppT, None)
                (O1, O1b, R1, R1b), (O2, O2b, R2_, R2b) = res
                op(DVE, lambda e: e.reciprocal(out=tA, in_=R1[:, :]), [R1b], [tAb])
                op(DVE, lambda e: e.tensor_tensor(out=tA, in0=O1[:, :], in1=tA, op=ALU.mult), [O1b, tAb], [tAb])
                op(DVE, lambda e: e.reciprocal(out=tB, in_=R2_[:, :]), [R2b], [tBb])
                op(DVE, lambda e: e.tensor_tensor(out=tB, in0=O2[:, :], in1=tB, op=ALU.mult), [O2b, tBb], [tBb])
                op(DVE, lambda e: e.scalar_tensor_tensor(out=tA, in0=tB, scalar=nlam[:, l:l + 1], in1=tA,
                                                         op0=ALU.mult, op1=ALU.add), [tAb, tBb, cb], [tAb])
                op(DVE, lambda e: e.tensor_tensor(out=tC, in0=tA, in1=tA, op=ALU.mult), [tAb], [tCb])
                (X, Xb) = psbank()
                mm([(X[:, :], K.ones_bf[:, :], tC, True, True)], [tCb, cb], [Xb])
                op(ACT, lambda e: e.activation(out=tB, in_=X[:, :], func=AF.Sqrt, bias=1e-6, scale=1.0 / 128),
                   [Xb], [tBb])
                op(DVE, lambda e: e.reciprocal(out=tB, in_=tB), [tBb], [tBb])
                op(DVE, lambda e: e.tensor_tensor(out=tA, in0=tA, in1=tB, op=ALU.mult), [tAb, tBb], [tAb])
                op(DVE, lambda e: e.tensor_scalar(out=OT[:, hd, qt * 512:(qt + 1) * 512], in0=tA,
                                                  scalar1=sub_s[:, l:l + 1], scalar2=1.0 - LAM_INIT[l],
                                                  op0=ALU.mult, op1=ALU.mult), [tAb, cb], [OTb[qt]])
                K.pinned.clear()
        barrier()
        A.off = m0
        if stop == "da" and l == stop_layer:
            return finish()

        m0 = A.off
        cqn = A.alloc([128, 4, NT], BF16)
        cqnb = Buf("cqn")
        ckv_all = A.alloc([128, 2, NKEY], BF16)
        ckvb = Buf("ckv")
        kr_all = A.alloc([128, NKEY], BF16)
        krb = Buf("kr")
        rc = A.alloc([64, 512], F32)
        rs = A.alloc([64, 512], F32)
        rcb = Buf("rope")
        tA = A.alloc([128, 512], F32)
        tB = A.alloc([128, 512], F32)
        tC = A.alloc([128, 512], BF16)
        tAb, tBb, tCb = Buf("tA"), Buf("tB"), Buf("tC")
        m1 = A.off
        p128 = Pool([128, 16, 128], BF16, 2, "w128")
        p64 = Pool([128, 16, 64], BF16, 3, "w64")
        pkf = Pool([128, 512], F32, 2, "kf")
        dma(GP, kr_all[64:73, :], kmask, writes=[krb])
        dma(GP, ckv_all[:, :, 2048:NKEY], c_ckv[l], writes=[ckvb])
        dma(GP, kr_all[0:64, 2048:NKEY], c_kr[l], writes=[krb])

        def rms_blocks(nblk, blk0, qt, gcol, dst_of, dst_b, out_dst):
            banks = []
            for c in range(nblk):
                wp, wpb = loadw(p128, w128[l, blk0 + c])
                (Pq, Pqb) = psbank(pin=True)
                mm([(Pq[:, :], wp[:, kc, :], hT[:, kc, qt * 512:(qt + 1) * 512], kc == 0, kc == 15)
                    for kc in range(16)], [wpb, hTb[qt]], [Pqb])
                banks.append((Pq, Pqb))
            (X, Xb) = psbank()
            for c in range(nblk):
                op(ACT, lambda e, c=c: e.activation(out=tC, in_=banks[c][0][:, :], func=AF.Square), [banks[c][1]], [tCb])
                mm([(X[:, :], K.ones_bf[:, :], tC, c == 0, c == nblk - 1)], [tCb, cb], [Xb])
            op(ACT, lambda e: e.activation(out=tB, in_=X[:, :], func=AF.Sqrt, bias=1e-6, scale=1.0 / (128 * nblk)),
               [Xb], [tBb])
            op(DVE, lambda e: e.reciprocal(out=tB, in_=tB), [tBb], [tBb])
            for c in range(nblk):
                op(DVE, lambda e, c=c: e.tensor_tensor(out=tA, in0=banks[c][0][:, :], in1=tB, op=ALU.mult),
                   [banks[c][1], tBb], [tAb])
                if out_dst is None:
                    op(DVE, lambda e, c=c: e.tensor_scalar(out=dst_of(c), in0=tA, scalar1=gcol(c), scalar2=None,
                                                           op0=ALU.mult), [tAb, cb], [dst_b])
                else:
                    kf, kfb = pkf.get()
                    op(DVE, lambda e, c=c: e.tensor_scalar(out=kf, in0=tA, scalar1=gcol(c), scalar2=None,
                                                           op0=ALU.mult), [tAb, cb], [kfb])
                    dma(SP, out_dst(c), kf, reads=[kfb])
                    op(ACT, lambda e, c=c: e.activation(out=dst_of(c), in_=kf, func=AF.Copy), [kfb], [dst_b])
            K.pinned.clear()

        for qt in range(4):
            rms_blocks(4, 0, qt, lambda c: gq_s[:, l * 4 + c:l * 4 + c + 1],
                       lambda c, qt=qt: cqn[:, c, qt * 512:(qt + 1) * 512], cqnb, None)
            rms_blocks(2, 4, qt, lambda c: gkv_s[:, l * 2 + c:l * 2 + c + 1],
                       lambda c, qt=qt: ckv_all[:, c, qt * 512:(qt + 1) * 512], ckvb,
                       lambda c, qt=qt: o_ckv[l, c, :, qt * 512:(qt + 1) * 512])
            load_rope(qt)
            kf = pkf.get()
            rope_proj(l, 32, qt, lambda kc, qt=qt: hT[:, kc, qt * 512:(qt + 1) * 512], 16, w64[l, 32], w64[l, 33],
                      p64, 64, kr_all[0:64, qt * 512:(qt + 1) * 512], krb, out_f=kf, rhs_reads=[hTb[qt]])
            dma(SP, o_kr[l, :, qt * 512:(qt + 1) * 512], kf[0][0:64, :], reads=[kf[1]])
        barrier()
        A.off = m1
        kn = A.alloc([128, NKEY], BF16)
        knb = Buf("kn")
        Vt = A.alloc([128, 18, 128], BF16)
        Vb = Buf("V")
        qn = A.alloc([128, 512], BF16)
        qnb = Buf("qn")
        qr = A.alloc([128, 512], BF16)
        qrb = Buf("qr")
        ppT = Pool([128, 512], BF16, 4, "pT")
        pwn = Pool([128, 4, 128], BF16, 2, "wuqn")
        pwr = Pool([128, 4, 64], BF16, 2, "wuqr")
        pwk = Pool([128, 2, 128], BF16, 2, "wukv")
        for hd in range(8):
            wk, wkb = loadw(pwk, wukv[l, hd, 0])
            wv, wvb = loadw(pwk, wukv[l, hd, 1])
            for t5 in range(5):
                n = 512 if t5 < 4 else 256
                (Pk, Pkb) = psbank()
                mm([(Pk[:, 0:n], wk[:, fc, :], ckv_all[:, fc, t5 * 512:t5 * 512 + n], fc == 0, fc == 1)
                    for fc in range(2)], [wkb, ckvb], [Pkb])
                op(ACT, lambda e: e.activation(out=kn[:, t5 * 512:t5 * 512 + n], in_=Pk[:, 0:n], func=AF.Copy),
                   [Pkb], [knb])
            for g in range(5):
                ns = 4 if g < 4 else 2
                (Pv, Pvb) = psbank()
                mms = []
                for s in range(ns):
                    tcn = g * 4 + s
                    for fc in range(2):
                        mms.append((Pv[:, s * 128:(s + 1) * 128], ckv_all[:, fc, tcn * 128:(tcn + 1) * 128],
                                    wv[:, fc, :], fc == 0, fc == 1))
                mm(mms, [wvb, ckvb], [Pvb])
                op(ACT, lambda e: e.activation(out=Vt[:, g * 4:g * 4 + ns, :],
                                               in_=Pv[:, 0:ns * 128].rearrange("p (a b) -> p a b", a=ns),
                                               func=AF.Copy), [Pvb], [Vb])
            wn, wnb = loadw(pwn, wuq_n[l, hd])
            wrp, wrpb = pwr.get()
            dma(GP, wrp, wuq_r[l, hd, 0], writes=[wrpb])
            wrr, wrrb = pwr.get()
            dma(GP, wrr, wuq_r[l, hd, 1], writes=[wrrb])
            for qt in range(4):
                (Pq, Pqb) = psbank()
                mm([(Pq[:, :], wn[:, kc, :], cqn[:, kc, qt * 512:(qt + 1) * 512], kc == 0, kc == 3)
                    for kc in range(4)], [wnb, cqnb], [Pqb])
                op(ACT, lambda e: e.activation(out=qn, in_=Pq[:, :], func=AF.Copy), [Pqb], [qnb])
                load_rope(qt)
                (Pa, Pab) = psbank()
                (Pb, Pbb) = psbank()
                mm([(Pa[0:64, :], wrp[:, kc, :], cqn[:, kc, qt * 512:(qt + 1) * 512], kc == 0, kc == 3)
                    for kc in range(4)], [wrpb, cqnb], [Pab])
                mm([(Pb[0:64, :], wrr[:, kc, :], cqn[:, kc, qt * 512:(qt + 1) * 512], kc == 0, kc == 3)
                    for kc in range(4)], [wrrb, cqnb], [Pbb])
                op(DVE, lambda e: e.tensor_tensor(out=tA[0:64, :], in0=Pa[0:64, :], in1=rc, op=ALU.mult),
                   [Pab, rcb], [tAb])
                op(DVE, lambda e: e.tensor_tensor(out=tB[0:64, :], in0=Pb[0:64, :], in1=rs, op=ALU.mult),
                   [Pbb, rcb], [tBb])
                op(DVE, lambda e: e.tensor_tensor(out=qr[0:64, :], in0=tA[0:64, :], in1=tB[0:64, :], op=ALU.add),
                   [tAb, tBb], [qrb])
                dma(GP, qr[64:73, :], qmask[:, qt * 512:(qt + 1) * 512], writes=[qrb])
                res = attention(1, list(range(18)),
                                lambda comp, kc: [(kn[:, kc * 128:(kc + 1) * 128], qn[:, :]),
                                                  (kr_all[0:73, kc * 128:(kc + 1) * 128], qr[0:73, :])],
                                [knb, qnb, krb, qrb], Vt, Vb, 192.0 ** -0.5, None, ppT, None)
                (O1, O1b, R1, R1b) = res[0]
                op(DVE, lambda e: e.reciprocal(out=tA, in_=R1[:, :]), [R1b], [tAb])
                op(DVE, lambda e: e.tensor_tensor(out=OT[:, 4 + hd, qt * 512:(qt + 1) * 512], in0=O1[:, :], in1=tA,
                                                  op=ALU.mult), [O1b, tAb], [OTb[qt]])
                K.pinned.clear()
        barrier()
        A.off = m0
        if stop == "mla" and l == stop_layer:
            return finish()

        m0 = A.off
        qn = A.alloc([128, 512], BF16)
        qnb = Buf("qn")
        kn = A.alloc([128, NKEY], BF16)
        knb = Buf("kn")
        Vt = A.alloc([128, 18, 128], BF16)
        Vb = Buf("V")
        tA = A.alloc([128, 512], F32)
        tAb = Buf("tA")
        pbias = Pool([128, 512], F32, 2, "bias")
        ptf = Pool([128, 512], F32, 2, "tf")
        ppT = Pool([128, 512], BF16, 4, "pT")
        p128 = Pool([128, 16, 128], BF16, 2, "w128")
        pkf = Pool([128, 512], F32, 2, "kf")
        for hd in range(4):
            wk, wkb = loadw(p128, w128[l, 10 + hd])
            for qt in range(4):
                (Pk, Pkb) = psbank()
                mm([(Pk[:, :], wk[:, kc, :], hT[:, kc, qt * 512:(qt + 1) * 512], kc == 0, kc == 15)
                    for kc in range(16)], [wkb, hTb[qt]], [Pkb])
                kf, kfb = pkf.get()
                op(DVE, lambda e: e.tensor_copy(out=kf, in_=Pk[:, :]), [Pkb], [kfb])
                dma(SP, o_nak[l, hd, :, qt * 512:(qt + 1) * 512], kf, reads=[kfb])
                op(ACT, lambda e: e.activation(out=kn[:, qt * 512:(qt + 1) * 512], in_=kf, func=AF.Copy),
                   [kfb], [knb])
            dma(GP, kn[:, 2048:NKEY], c_nak[l, hd], writes=[knb])
            wv, wvb = loadw(p128, w128[l, 18 + hd])
            proj_v_tm(wv, wvb, lambda kc, tcn: hT[:, kc, tcn * 128:(tcn + 1) * 128], 16, hTb, hd, o_nav[l, hd])
            dma(GP, Vt[:, 16:18, :], c_nav[l, hd], writes=[Vb])
            wq, wqb = loadw(p128, w128[l, 6 + hd])
            for qt in range(4):
                (Pq, Pqb) = psbank()
                mm([(Pq[:, :], wq[:, kc, :], hT[:, kc, qt * 512:(qt + 1) * 512], kc == 0, kc == 15)
                    for kc in range(16)], [wqb, hTb[qt]], [Pqb])
                op(ACT, lambda e: e.activation(out=qn, in_=Pq[:, :], func=AF.Copy), [Pqb], [qnb])

                def bias_fn(kc, qt=qt, hd=hd):
                    bt, btb = pbias.get()
                    dma(SP, bt, nabias[l, hd, NA_PAIR[(qt, kc)]], writes=[btb])
                    return bt, btb

                res = attention(1, NA_CH[qt], lambda comp, kc: [(kn[:, kc * 128:(kc + 1) * 128], qn[:, :])],
                                [knb, qnb], Vt, Vb, 128.0 ** -0.5, bias_fn, ppT, ptf)
                (O1, O1b, R1, R1b) = res[0]
                op(DVE, lambda e: e.reciprocal(out=tA, in_=R1[:, :]), [R1b], [tAb])
                op(DVE, lambda e: e.tensor_tensor(out=OT[:, 12 + hd, qt * 512:(qt + 1) * 512], in0=O1[:, :], in1=tA,
                                                  op=ALU.mult), [O1b, tAb], [OTb[qt]])
                K.pinned.clear()
        barrier()
        A.off = m0
        if stop == "na" and l == stop_layer:
            return finish()

        m0 = A.off
        tt = A.alloc([128, 16, 512], F32)
        ttb = Buf("tt")
        pzb = Pool([128, 512], BF16, 4, "zb")
        tM = (A.alloc([128, 512], F32), Buf("M"))
        tV = (A.alloc([128, 512], F32), Buf("V"))
        p128 = Pool([128, 16, 128], BF16, 3, "wo")
        lg = A.alloc([128, 8], F32)
        lg2 = A.alloc([128, 8], F32)
        e1 = A.alloc([128, 8], F32)
        e2 = A.alloc([128, 8], F32)
        sm = A.alloc([128, 8], F32)
        smb = Buf("sm")
        if is_moe:
            dma(SP, wr, router[fi].rearrange("p a b -> p (a b)"), writes=[wrb])
            for c in range(16):
                op(DVE, lambda e, c=c: e.tensor_scalar(out=wrA[:, c * 8:(c + 1) * 8], in0=wr[:, c * 8:(c + 1) * 8],
                                                       scalar1=col(cvec(4), c), scalar2=None, op0=ALU.mult),
                   [wrb, coefb], [wrb])
                op(DVE, lambda e, c=c: e.tensor_scalar(out=wrB[:, c * 8:(c + 1) * 8], in0=wr[:, c * 8:(c + 1) * 8],
                                                       scalar1=col(cvec(5), c), scalar2=None, op0=ALU.mult),
                   [wrb, coefb], [wrb])
            (Pc, Pcb) = psbank()
            mm([(Pc[:, 0:8], ones_f[:, :], wrB[:, c * 8:(c + 1) * 8], c == 0, c == 15) for c in range(16)],
               [wrb, cb], [Pcb])
            op(DVE, lambda e: e.tensor_copy(out=cstb, in_=Pc[:, 0:8]), [Pcb], [wrb])
        for qt in range(4):
            dma(SP, tt, src[:, :, qt * 512:(qt + 1) * 512], reads=[tSb[qt]], writes=[ttb])
            for j in range(16):
                wo, wob = loadw(p128, w_out[l, j])
                (Pm, Pmb) = psbank()
                mm([(Pm[:, :], wo[:, kc, :], OT[:, kc, qt * 512:(qt + 1) * 512], kc == 0, kc == 15)
                    for kc in range(16)], [wob, OTb[qt]], [Pmb])
                affine(ACT, tt[:, j, :], tt[:, j, :], col(cvec(2), j), col(cvec(3), j), [ttb, coefb], [ttb])
                op(DVE, lambda e, j=j: e.scalar_tensor_tensor(out=tt[:, j, :], in0=Pm[:, :], scalar=col(g1v, j),
                                                              in1=tt[:, j, :], op0=ALU.mult, op1=ALU.add),
                   [Pmb, ttb, cb], [ttb])
            layernorm(lambda c: tt[:, c, :], ttb, pzb, tM, tV)
            for c in range(16):
                affine(alt_eng(), hT[:, c, qt * 512:(qt + 1) * 512], tt[:, c, :], col(cvec(4), c), col(cvec(5), c),
                       [ttb, coefb], [hTb[qt]])
            if is_moe:
                for sb in range(4):
                    (Pl, Plb) = psbank()
                    mm([(Pl[:, 0:8], tt[:, c, sb * 128:(sb + 1) * 128], wrA[:, c * 8:(c + 1) * 8], c == 0, c == 15)
                        for c in range(16)], [ttb, wrb], [Plb])
                    go = gate_tm[:, (qt * 4 + sb) * 8:(qt * 4 + sb + 1) * 8]
                    op(DVE, lambda e: e.tensor_tensor(out=lg, in0=Pl[:, 0:8], in1=cstb, op=ALU.add), [Plb, wrb], [smb])
                    op(DVE, lambda e: e.tensor_reduce(out=sm[:, 0:1], in_=lg, axis=mybir.AxisListType.X, op=ALU.max),
                       [smb], [smb])
                    op(DVE, lambda e: e.tensor_scalar(out=e1, in0=lg, scalar1=sm[:, 0:1], scalar2=None,
                                                      op0=ALU.is_equal), [smb], [smb])
                    op(DVE, lambda e: e.scalar_tensor_tensor(out=lg2, in0=e1, scalar=-1e30, in1=lg, op0=ALU.mult,
                                                             op1=ALU.add), [smb], [smb])
                    op(DVE, lambda e: e.tensor_reduce(out=sm[:, 1:2], in_=lg2, axis=mybir.AxisListType.X, op=ALU.max),
                       [smb], [smb])
                    op(DVE, lambda e: e.tensor_scalar(out=e2, in0=lg2, scalar1=sm[:, 1:2], scalar2=None,
                                                      op0=ALU.is_equal), [smb], [smb])
                    op(DVE, lambda e: e.tensor_tensor(out=sm[:, 2:3], in0=sm[:, 1:2], in1=sm[:, 0:1], op=ALU.subtract),
                       [smb], [smb])
                    op(ACT, lambda e: e.activation(out=sm[:, 3:4], in_=sm[:, 2:3], func=AF.Exp), [smb], [smb])
                    op(DVE, lambda e: e.tensor_scalar(out=sm[:, 4:5], in0=sm[:, 3:4], scalar1=1.0, scalar2=None,
                                                      op0=ALU.add), [smb], [smb])
                    op(DVE, lambda e: e.reciprocal(out=sm[:, 5:6], in_=sm[:, 4:5]), [smb], [smb])
                    op(DVE, lambda e: e.tensor_tensor(out=sm[:, 6:7], in0=sm[:, 3:4], in1=sm[:, 5:6], op=ALU.mult),
                       [smb], [smb])
                    op(DVE, lambda e: e.tensor_scalar(out=e1, in0=e1, scalar1=sm[:, 5:6], scalar2=None, op0=ALU.mult),
                       [smb], [smb])
                    op(DVE, lambda e: e.scalar_tensor_tensor(out=go, in0=e2, scalar=sm[:, 6:7], in1=e1, op0=ALU.mult,
                                                             op1=ALU.add), [smb, gateb], [gateb])
            for c in range(16):
                affine(alt_eng(), tt[:, c, :], tt[:, c, :], col(cvec(6), c), col(cvec(7), c), [ttb, coefb], [ttb])
            dma(SP, tS[:, :, qt * 512:(qt + 1) * 512], tt, reads=[ttb], writes=[tSb[qt]])
        barrier()
        A.off = m0
        if stop == "e" and l == stop_layer:
            return finish()

        m0 = A.off
        p13 = Pool([128, 16, 128], BF16, 4, "w13")
        p2 = Pool([128, 2048], BF16, GF + 1, "w2")
        pu = Pool([128, 1024], BF16, 2 * GF, "u")
        ptf = Pool([128, 512], F32, 2, "tf")
        gbc = A.alloc([128, 1024], F32)
        gbcb = Buf("gbc")
        grep = A.alloc([128, 128], F32)
        grepb = Buf("grep")
        if is_moe:
            chunks = [(e_, f) for e_ in range(n_exp) for f in range(22)]
        else:
            chunks = [(0, f) for f in range(43)]
        for ps_ in range(2):
            for tl in range(2):
                dma(SP, acc[:, :, tl * 512:(tl + 1) * 512], tS[:, :, ps_ * 1024 + tl * 512: ps_ * 1024 + (tl + 1) * 512],
                    reads=[tSb[ps_ * 2 + tl]], writes=[accb[tl]])
            group = []
            cur_e = -1
            for ci, (e_, f) in enumerate(chunks):
                if is_moe and e_ != cur_e:
                    cur_e = e_
                    for blk in range(8):
                        gb = (ps_ * 8 + blk) * 8 + e_
                        op(DVE, lambda e, gb=gb: e.tensor_copy(out=grep, in_=gate_tm[:, gb:gb + 1].to_broadcast([128, 128])),
                           [gateb], [grepb])
                        (Pg, Pgb) = psbank()
                        mm([(Pg[:, 0:128], grep, ident, True, True)], [grepb, cb], [Pgb])
                        op(ACT, lambda e, blk=blk: e.activation(out=gbc[:, blk * 128:(blk + 1) * 128], in_=Pg[:, 0:128],
                                                                func=AF.Copy), [Pgb], [gbcb])
                if is_moe:
                    s1, s3, s2 = moe_w1[fi, e_, f], moe_w3[fi, e_, f], moe_w2[fi, e_, f]
                else:
                    s1, s3, s2 = ffn_w1[fi, f], ffn_w3[fi, f], ffn_w2[fi, f]
                w1b, w1bb = loadw(p13, s1)
                w3b, w3bb = loadw(p13, s3)
                w2b, w2bb = loadw(p2, s2)
                u, ub = pu.get()
                for tl in range(2):
                    t0 = ps_ * 1024 + tl * 512
                    (G1, G1b) = psbank()
                    (G3, G3b) = psbank()
                    mm([(G1[:, :], w1b[:, kc, :], hT[:, kc, t0:t0 + 512], kc == 0, kc == 15) for kc in range(16)],
                       [w1bb, hTb[ps_ * 2 + tl]], [G1b])
                    mm([(G3[:, :], w3b[:, kc, :], hT[:, kc, t0:t0 + 512], kc == 0, kc == 15) for kc in range(16)],
                       [w3bb, hTb[ps_ * 2 + tl]], [G3b])
                    tf, tfb = ptf.get()
                    op(ACT, lambda e: e.activation(out=tf, in_=G1[:, :], func=AF.Silu), [G1b], [tfb])
                    if is_moe:
                        op(DVE, lambda e: e.tensor_tensor(out=tf, in0=G3[:, :], in1=tf, op=ALU.mult), [G3b, tfb], [tfb])
                        op(DVE, lambda e: e.tensor_tensor(out=u[:, tl * 512:(tl + 1) * 512], in0=tf,
                                                          in1=gbc[:, tl * 512:(tl + 1) * 512], op=ALU.mult),
                           [tfb, gbcb], [ub])
                    else:
                        op(DVE, lambda e: e.tensor_tensor(out=u[:, tl * 512:(tl + 1) * 512], in0=G3[:, :], in1=tf,
                                                          op=ALU.mult), [G3b, tfb], [ub])
                group.append((w2b, w2bb, u, ub))
                last_of_e = (ci + 1 == len(chunks)) or (chunks[ci + 1][0] != e_)
                if len(group) == GF or last_of_e:
                    for tl in range(2):
                        for j in range(16):
                            (Pw, Pwb) = psbank()
                            mm([(Pw[:, :], g_[0][:, j * 128:(j + 1) * 128], g_[2][:, tl * 512:(tl + 1) * 512],
                                 gi == 0, gi == len(group) - 1) for gi, g_ in enumerate(group)],
                               [g_[1] for g_ in group] + [g_[3] for g_ in group], [Pwb])
                            op(DVE, lambda e, j=j, tl=tl: e.scalar_tensor_tensor(
                                out=acc[:, j, tl * 512:(tl + 1) * 512], in0=Pw[:, :], scalar=col(g2v, j),
                                in1=acc[:, j, tl * 512:(tl + 1) * 512], op0=ALU.mult, op1=ALU.add),
                               [Pwb, accb[tl], cb], [accb[tl]])
                    group = []
            m2 = A.off
            pzb = Pool([128, 512], BF16, 4, "zb")
            tM = (A.alloc([128, 512], F32), Buf("M"))
            tV = (A.alloc([128, 512], F32), Buf("V"))
            for tl in range(2):
                qt = ps_ * 2 + tl
                layernorm(lambda c, tl=tl: acc[:, c, tl * 512:(tl + 1) * 512], accb[tl], pzb, tM, tV)
                if l == nlayers - 1:
                    for c in range(16):
                        affine(alt_eng(), acc[:, c, tl * 512:(tl + 1) * 512], acc[:, c, tl * 512:(tl + 1) * 512],
                               lnp_s[:, l * 64 + 32 + c:l * 64 + 33 + c], lnp_s[:, l * 64 + 48 + c:l * 64 + 49 + c],
                               [accb[tl], cb], [accb[tl]])
                    dma(SP, yT[:, :, qt * 512:(qt + 1) * 512], acc[:, :, tl * 512:(tl + 1) * 512], reads=[accb[tl]])
                else:
                    dma(SP, tS[:, :, qt * 512:(qt + 1) * 512], acc[:, :, tl * 512:(tl + 1) * 512], reads=[accb[tl]],
                        writes=[tSb[qt]])
            barrier()
            A.off = m2
        barrier()
        A.off = m0

    barrier()
    return nc


def _fm(x):
    T = x.shape[0]
    return np.ascontiguousarray(x.reshape(T, 16, 128).transpose(2, 1, 0))


def _pcol(v):
    return np.ascontiguousarray(v.reshape(-1, 128).T)


def _wblk(W, c0, m):
    Kd = W.shape[0]
    return np.ascontiguousarray(W[:, c0:c0 + m].reshape(Kd // 128, 128, m).transpose(1, 0, 2))


def _rot_perm():
    perm = np.zeros(64, np.int64)
    sign = np.zeros(64, np.float32)
    for a in range(2):
        for i in range(16):
            perm[a * 32 + i] = a * 32 + 16 + i
            sign[a * 32 + i] = -1.0
            perm[a * 32 + 16 + i] = a * 32 + i
            sign[a * 32 + 16 + i] = 1.0
    return perm, sign


def _rope_tables(n):
    t = np.arange(n)
    row = (t // 64).astype(np.float32)
    colv = (t % 64).astype(np.float32)
    inv = (10000.0 ** (-np.arange(0, 32, 2, dtype=np.float32) / 32)).astype(np.float32)
    ar = row[:, None] * inv[None, :]
    ac = colv[:, None] * inv[None, :]
    ang = np.concatenate([ar, ar, ac, ac], axis=-1)
    return np.cos(ang).astype(np.float32), np.sin(ang).astype(np.float32)


def _na_bias_sample(rpb_l):
    out = np.full((4, NPAIR, 128, 512), -30000.0, np.float32)
    r = np.arange(32)
    row_start = np.clip(r - 4, 0, 24)
    cq = np.arange(64)
    col_start = np.clip(cq - 8, 0, 48)
    for (qt, kc), pi in NA_PAIR.items():
        if kc >= 16:
            out[:, pi] = 0.0
            continue
        kk = kc * 128 + np.arange(128)
        qq = qt * 512 + np.arange(512)
        kr_, kcol = kk // 64, kk % 64
        qr_, qcol = qq // 64, qq % 64
        rs_ = row_start[qr_]
        cs_ = col_start[qcol]
        valid = ((kr_[:, None] >= rs_[None, :]) & (kr_[:, None] < rs_[None, :] + 8)
                 & (kcol[:, None] >= cs_[None, :]) & (kcol[:, None] < cs_[None, :] + 16))
        roff = np.clip(kr_[:, None] - qr_[None, :] + 7, 0, 14)
        coff = np.clip(kcol[:, None] - qcol[None, :], -15, 15) + 15
        g = rpb_l[:, roff, coff]
        out[:, pi] = np.where(valid[None], g, np.float32(-30000.0))
    return out


def _na_bias_prompt():
    out = np.full((NPAIR, 128, 512), -30000.0, np.float32)
    for (qt, kc), pi in NA_PAIR.items():
        if kc >= 16:
            continue
        kk = kc * 128 + np.arange(128)
        qq = qt * 512 + np.arange(512)
        out[pi] = np.where((kk[:, None] // 256) == (qq[None, :] // 256), np.float32(0.0), np.float32(-30000.0))
    return out


_NC_CACHE = {}


def prep(inp, Lw=L):
    f = lambda k: np.asarray(inp[k], dtype=np.float32)
    perm, sign = _rot_perm()
    w_in = f("w_in")
    shared = {}
    wa = f("w_ada")
    shared["w_ada"] = np.ascontiguousarray(wa.reshape(L, 16, 128, 96, 128).transpose(0, 3, 2, 1, 4))
    shared["b_ada"] = np.ascontiguousarray(f("b_ada").reshape(L, 96, 128).transpose(0, 2, 1))
    w64 = np.zeros((L, 34, 128, 16, 64), np.float32)
    w128 = np.zeros((L, 22, 128, 16, 128), np.float32)
    OFF = {"dq": 0, "dk": 512, "dv": 1024, "cq": 1536, "ckv": 2048, "kr": 2304, "nq": 2368, "nk": 2880, "nv": 3392}
    for l in range(L):
        W = w_in[l]
        for sec, b0 in (("dq", 0), ("dk", 16)):
            for hd in range(4):
                for c in range(2):
                    c0 = OFF[sec] + hd * 128 + c * 64
                    blkW = W[:, c0:c0 + 64]
                    w64[l, b0 + hd * 4 + c * 2] = blkW.reshape(16, 128, 64).transpose(1, 0, 2)
                    w64[l, b0 + hd * 4 + c * 2 + 1] = blkW[:, perm].reshape(16, 128, 64).transpose(1, 0, 2)
        blkW = W[:, OFF["kr"]:OFF["kr"] + 64]
        w64[l, 32] = blkW.reshape(16, 128, 64).transpose(1, 0, 2)
        w64[l, 33] = blkW[:, perm].reshape(16, 128, 64).transpose(1, 0, 2)
        for i in range(4):
            w128[l, i] = _wblk(W, OFF["cq"] + i * 128, 128)
            w128[l, 6 + i] = _wblk(W, OFF["nq"] + i * 128, 128)
            w128[l, 10 + i] = _wblk(W, OFF["nk"] + i * 128, 128)
            w128[l, 14 + i] = _wblk(W, OFF["dv"] + i * 128, 128)
            w128[l, 18 + i] = _wblk(W, OFF["nv"] + i * 128, 128)
        for i in range(2):
            w128[l, 4 + i] = _wblk(W, OFF["ckv"] + i * 128, 128)
    shared["w64"] = w64
    shared["w128"] = w128
    wuq = f("mla_wuq")
    wukv = f("mla_wukv")
    wuq_n = np.zeros((L, 8, 128, 4, 128), np.float32)
    wuq_r = np.zeros((L, 8, 2, 128, 4, 64), np.float32)
    wukv_r = np.zeros((L, 8, 2, 128, 2, 128), np.float32)
    for l in range(L):
        for hd in range(8):
            wuq_n[l, hd] = _wblk(wuq[l], hd * 192, 128)
            rb = wuq[l][:, hd * 192 + 128: hd * 192 + 192]
            wuq_r[l, hd, 0] = rb.reshape(4, 128, 64).transpose(1, 0, 2)
            wuq_r[l, hd, 1] = rb[:, perm].reshape(4, 128, 64).transpose(1, 0, 2)
            wukv_r[l, hd, 0] = _wblk(wukv[l], hd * 256, 128)
            wukv_r[l, hd, 1] = _wblk(wukv[l], hd * 256 + 128, 128)
    shared["wuq_n"] = wuq_n
    shared["wuq_r"] = wuq_r
    shared["wukv"] = wukv_r
    wo = f("w_out")
    shared["w_out"] = np.ascontiguousarray(wo.reshape(L, 16, 128, 16, 128).transpose(0, 3, 2, 1, 4))
    for nm in ("ffn_w1", "ffn_w3"):
        shared[nm] = np.ascontiguousarray(f(nm).reshape(2, 16, 128, 43, 128).transpose(0, 3, 2, 1, 4))
    shared["ffn_w2"] = np.ascontiguousarray(f("ffn_w2").reshape(2, 43, 128, 2048))
    for nm in ("moe_w1", "moe_w3"):
        shared[nm] = np.ascontiguousarray(f(nm).reshape(2, 8, 16, 128, 22, 128).transpose(0, 1, 4, 3, 2, 5))
    shared["moe_w2"] = np.ascontiguousarray(f("moe_w2").reshape(2, 8, 22, 128, 2048))
    shared["router"] = np.ascontiguousarray(f("moe_router").reshape(2, 16, 128, 8).transpose(0, 2, 1, 3))
    lnp = np.zeros((128, L * 64), np.float32)
    for l in range(L):
        for i, nm in enumerate(("ln1_g", "ln1_b", "ln2_g", "ln2_b")):
            lnp[:, l * 64 + i * 16:l * 64 + (i + 1) * 16] = _pcol(f(nm)[l])
    shared["lnp"] = lnp
    shared["gq"] = np.concatenate([_pcol(f("mla_gq")[l]) for l in range(L)], axis=1)
    shared["gkv"] = np.concatenate([_pcol(f("mla_gkv")[l]) for l in range(L)], axis=1)
    shared["subln"] = np.concatenate([_pcol(f("da_subln")[l]) for l in range(L)], axis=1)
    lamp = np.zeros((64, 4 * L), np.float32)
    for l in range(L):
        for i, nm in enumerate(("da_lq1", "da_lk1", "da_lq2", "da_lk2")):
            lamp[:, 4 * l + i] = f(nm)[l]
    shared["lamp"] = lamp
    shared["ident"] = np.eye(128, dtype=np.float32)

    cos, sin = _rope_tables(2048)
    ropec_s = np.ascontiguousarray(cos.T)
    ropes_s = np.ascontiguousarray((sin * sign[None, :]).T)
    ropec_p = np.ones((64, 2048), np.float32)
    ropes_p = np.zeros((64, 2048), np.float32)
    km_p = np.zeros((9, NKEY), np.float32)
    qm_p = np.zeros((9, NT), np.float32)
    for j in range(8):
        km_p[j, j * 256:(j + 1) * 256] = 1.0
        qm_p[j, :] = NBIG
        qm_p[j, j * 256:(j + 1) * 256] = 0.0
    km_p[8, 2048:] = 1.0
    qm_p[8, :] = NBIG
    km_s = np.zeros((9, NKEY), np.float32)
    km_s[0, :2048] = 1.0
    km_s[8, 2048:] = 1.0
    qm_s = np.zeros((9, NT), np.float32)
    nab_p1 = _na_bias_prompt()
    nab_p = np.ascontiguousarray(np.broadcast_to(nab_p1[None, None], (L, 4, NPAIR, 128, 512)))
    rpb = f("na_rpb")
    xp = f("x_prompt")
    xs = f("x_sample")
    c = f("c")
    cctx = f("c_ctx")
    zc = {
        "c_dak": np.zeros((L, 4, 2, 64, 256), np.float32), "c_dav": np.zeros((L, 4, 128, 2, 128), np.float32),
        "c_ckv": np.zeros((L, 128, 2, 256), np.float32), "c_kr": np.zeros((L, 64, 256), np.float32),
        "c_nak": np.zeros((L, 4, 128, 256), np.float32), "c_nav": np.zeros((L, 4, 128, 2, 128), np.float32),
    }
    in_maps = []
    for r in range(8):
        m = dict(shared)
        if r in (4, 5):
            b = r - 4
            m["xT"] = _fm(xs[b])
            m["cond"] = _pcol(c[b])
            m["ropec"], m["ropes"], m["kmask"], m["qmask"] = ropec_s, ropes_s, km_s, qm_s
            dk = f("cache_da_k")[b]
            m["c_dak"] = np.ascontiguousarray(dk.transpose(0, 2, 3, 4, 1))
            dv = f("cache_da_v")[b]
            m["c_dav"] = np.ascontiguousarray(dv.reshape(L, 2, 128, 4, 128).transpose(0, 3, 2, 1, 4))
            ck = f("cache_mla_ckv")[b]
            m["c_ckv"] = np.ascontiguousarray(ck.reshape(L, 256, 2, 128).transpose(0, 3, 2, 1))
            m["c_kr"] = np.ascontiguousarray(f("cache_mla_krope")[b].transpose(0, 2, 1))
            nk = f("cache_na_k")[b]
            m["c_nak"] = np.ascontiguousarray(nk.transpose(0, 2, 3, 1))
            nv = f("cache_na_v")[b]
            m["c_nav"] = np.ascontiguousarray(nv.reshape(L, 2, 128, 4, 128).transpose(0, 3, 2, 1, 4))
            m["nabias"] = np.stack([_na_bias_sample(rpb[l]) for l in range(L)], axis=0)
        else:
            rr = r if r < 4 else 0
            m["xT"] = _fm(xp[rr * 8:(rr + 1) * 8].reshape(2048, 2048))
            m["cond"] = _pcol(cctx)
            m["ropec"], m["ropes"], m["kmask"], m["qmask"] = ropec_p, ropes_p, km_p, qm_p
            m.update(zc)
            m["nabias"] = nab_p
        in_maps.append(m)

    return in_maps


def kernel(**inp):
    in_maps = prep(inp)
    if "nc" not in _NC_CACHE:
        _NC_CACHE["nc"] = build()
    nc = _NC_CACHE["nc"]
    res = run_bass_kernel_spmd(nc, in_maps, core_ids=list(range(8)))
    return post(res.results)


def post(R):

    def tm(yT):
        return yT.transpose(2, 1, 0).reshape(2048, 2048)

    y_p = np.concatenate([tm(R[r]["yT"]).reshape(8, 256, 2048) for r in range(4)], axis=0)
    y_s = np.stack([tm(R[4]["yT"]), tm(R[5]["yT"])], axis=0)
    dak = np.concatenate([R[r]["o_dak"].transpose(4, 0, 1, 2, 3).reshape(8, 256, L, 4, 2, 64).transpose(0, 2, 1, 3, 4, 5)
                          for r in range(4)], axis=0)
    dav = np.concatenate([R[r]["o_dav"].transpose(3, 2, 0, 1, 4).reshape(8, 256, L, 4, 128).transpose(0, 2, 1, 3, 4)
                          for r in range(4)], axis=0)
    ckv = np.concatenate([R[r]["o_ckv"].transpose(3, 0, 1, 2).reshape(8, 256, L, 256).transpose(0, 2, 1, 3)
                          for r in range(4)], axis=0)
    kr = np.concatenate([R[r]["o_kr"].transpose(2, 0, 1).reshape(8, 256, L, 64).transpose(0, 2, 1, 3)
                         for r in range(4)], axis=0)
    nak = np.concatenate([R[r]["o_nak"].transpose(3, 0, 1, 2).reshape(8, 256, L, 4, 128).transpose(0, 2, 1, 3, 4)
                          for r in range(4)], axis=0)
    nav = np.concatenate([R[r]["o_nav"].transpose(3, 2, 0, 1, 4).reshape(8, 256, L, 4, 128).transpose(0, 2, 1, 3, 4)
                          for r in range(4)], axis=0)
    outs = (y_p, y_s, dak, dav, ckv, kr, nak, nav)
    return tuple(np.ascontiguousarray(o, dtype=np.float32) for o in outs)
```

```python
import math
import numpy as np
import concourse.bass as bass
import concourse.mybir as mybir
from concourse.bass_utils import run_bass_kernel_spmd

F32 = mybir.dt.float32
BF16 = mybir.dt.bfloat16
AF = mybir.ActivationFunctionType
ALU = mybir.AluOpType

L = 4
NT = 2048
NKEY = 2304
ALPHA = 8.0 ** 0.25
LAM_INIT = [0.8 - 0.6 * math.exp(-0.3 * l) for l in range(L)]
NBIG = -32768.0
NA_CH = []
for _qt in range(4):
    NA_CH.append(list(range(max(0, 4 * _qt - 2), min(16, 4 * _qt + 6))) + [16, 17])
NA_PAIR = {}
for _qt in range(4):
    for _kc in NA_CH[_qt]:
        NA_PAIR[(_qt, _kc)] = len(NA_PAIR)
NPAIR = len(NA_PAIR)
GF = 3


class Tok:
    __slots__ = ("sem", "val")

    def __init__(self, sem, val):
        self.sem = sem
        self.val = val


class Buf:
    def __init__(self, name=""):
        self.name = name
        self.w = None
        self.r = {}


class Eng:
    def __init__(self, e, sem, is_pe=False):
        self.e = e
        self.sem = sem
        self.cnt = 0
        self.known = {}
        self.is_pe = is_pe

    def wait(self, tok):
        if tok is None:
            return
        if self.is_pe and tok.sem is self.sem:
            return
        k = id(tok.sem)
        if self.known.get(k, 0) >= tok.val:
            return
        self.e.wait_ge(tok.sem, tok.val)
        self.nwait = getattr(self, 'nwait', 0) + 1
        self.known[k] = tok.val


def _deps(E, reads, writes):
    for b in reads:
        E.wait(b.w)
    for b in writes:
        E.wait(b.w)
        for t in list(b.r.values()):
            E.wait(t)


def _commit(tok, reads, writes):
    for b in reads:
        b.r[id(tok.sem)] = tok
    for b in writes:
        b.w = tok
        b.r = {}


class K:
    pass


def op(E, fn, reads=(), writes=()):
    _deps(E, reads, writes)
    ins = fn(E.e)
    E.cnt += 1
    ins.then_inc(E.sem, 1)
    tok = Tok(E.sem, E.cnt)
    _commit(tok, reads, writes)
    return tok


def mm(mms, reads=(), writes=()):
    PE = K.PE
    _deps(PE, reads, writes)
    ins = None
    for (o, l, r, st, sp) in mms:
        ins = PE.e.matmul(o, lhsT=l, rhs=r, start=st, stop=sp)
        PE.nmm = getattr(PE, 'nmm', 0) + 1
    PE.cnt += 1
    ins.then_inc(PE.sem, 1)
    tok = Tok(PE.sem, PE.cnt)
    _commit(tok, reads, writes)
    return tok


class DQ:
    def __init__(self, e, sems):
        self.E = Eng(e, None)
        self.slots = [[s, 0] for s in sems]
        self.i = 0


def dma(Q, out, in_, reads=(), writes=()):
    E = Q.E
    _deps(E, reads, writes)
    sl = Q.slots[Q.i]
    Q.i = (Q.i + 1) % len(Q.slots)
    if sl[1] > 0:
        E.wait(Tok(sl[0], sl[1]))
    ins = E.e.dma_start(out=out, in_=in_)
    E.ndma = getattr(E, 'ndma', 0) + 1
    sl[1] += 16
    ins.then_inc(sl[0], 16)
    tok = Tok(sl[0], sl[1])
    _commit(tok, reads, writes)
    return tok


def barrier():
    toks = [Tok(E.sem, E.cnt) for E in (K.PE, K.ACT, K.DVE) if E.cnt > 0]
    for Q in (K.SP, K.GP):
        for s in Q.slots:
            if s[1] > 0:
                toks.append(Tok(s[0], s[1]))
    for E in (K.PE, K.ACT, K.DVE, K.SP.E, K.GP.E):
        for t in toks:
            E.wait(t)


class Arena:
    def __init__(self, nc, nbytes):
        self.f = nc.alloc_sbuf_tensor("arena", [128, nbytes // 4], F32)
        self.b = self.f.bitcast(BF16)
        self.off = 0
        self.cap = nbytes
        self.peak = 0

    def alloc(self, shape, dt):
        esz = 4 if dt == F32 else 2
        n = 1
        for s in shape[1:]:
            n *= s
        nb = (n * esz + 31) // 32 * 32
        off = self.off
        self.off += nb
        self.peak = max(self.peak, self.off)
        assert self.off <= self.cap, ("arena overflow", self.off, self.cap)
        if dt == F32:
            ap = self.f[0:shape[0], off // 4: off // 4 + n]
        else:
            ap = self.b[0:shape[0], off // 2: off // 2 + n]
        if len(shape) == 3:
            ap = ap.rearrange("p (a b) -> p a b", a=shape[1])
        return ap


class Pool:
    def __init__(self, shape, dt, n, name):
        self.items = [(K.A.alloc(shape, dt), Buf(name + str(i))) for i in range(n)]
        self.i = 0

    def get(self):
        it = self.items[self.i]
        self.i = (self.i + 1) % len(self.items)
        return it


def psbank(pin=False):
    while True:
        i = K.psi
        K.psi = (K.psi + 1) % len(K.PS)
        if i not in K.pinned:
            break
    if pin:
        K.pinned.add(i)
    return K.PS[i]


def loadw(pool, src):
    ap, b = pool.get()
    dma(K.GP, ap, src, writes=[b])
    return ap, b


def alt_eng():
    K.alt ^= 1
    return K.ACT if K.alt else K.DVE


def affine(E, out, in_, sc, bi, reads, writes):
    if E is K.ACT:
        op(E, lambda e: e.activation(out=out, in_=in_, func=AF.Identity, bias=bi, scale=sc), reads, writes)
    else:
        op(E, lambda e: e.tensor_scalar(out=out, in0=in_, scalar1=sc, scalar2=bi, op0=ALU.mult, op1=ALU.add),
           reads, writes)


def layernorm(zc, zbuf, pzb, tM, tV):
    ACT, DVE = K.ACT, K.DVE
    (S1, S1b) = psbank(pin=True)
    (S2, S2b) = psbank(pin=True)
    for c in range(16):
        zb, zbb = pzb.get()
        op(ACT, lambda e: e.activation(out=zb, in_=zc(c), func=AF.Copy), [zbuf], [zbb])
        mm([(S1[:, :], K.ones_bf[:, :], zb, c == 0, c == 15)], [zbb, K.cbuf], [S1b])
        zq, zqb = pzb.get()
        op(ACT, lambda e: e.activation(out=zq, in_=zc(c), func=AF.Square), [zbuf], [zqb])
        mm([(S2[:, :], K.ones_bf[:, :], zq, c == 0, c == 15)], [zqb, K.cbuf], [S2b])
    (M, Mb) = tM
    (V, Vb) = tV
    op(DVE, lambda e: e.tensor_scalar(out=M, in0=S1[:, :], scalar1=1.0 / 2048, scalar2=None, op0=ALU.mult), [S1b], [Mb])
    op(DVE, lambda e: e.tensor_tensor(out=V, in0=M, in1=M, op=ALU.mult), [Mb], [Vb])
    op(DVE, lambda e: e.scalar_tensor_tensor(out=V, in0=S2[:, :], scalar=1.0 / 2048, in1=V, op0=ALU.mult,
                                             op1=ALU.subtract), [S2b, Vb], [Vb])
    op(ACT, lambda e: e.activation(out=V, in_=V, func=AF.Sqrt, bias=1e-5, scale=1.0), [Vb], [Vb])
    op(DVE, lambda e: e.reciprocal(out=V, in_=V), [Vb], [Vb])
    for c in range(16):
        op(DVE, lambda e: e.tensor_tensor(out=zc(c), in0=zc(c), in1=M, op=ALU.subtract), [zbuf, Mb], [zbuf])
        op(DVE, lambda e: e.tensor_tensor(out=zc(c), in0=zc(c), in1=V, op=ALU.mult), [zbuf, Vb], [zbuf])
    K.pinned.clear()


def attention(ncomp, kchunks, score_fn, score_reads, Vt, Vb, scale, bias_fn, ppT, tmpf):
    ACT, DVE = K.ACT, K.DVE
    res = []
    for comp in range(ncomp):
        (O, Ob) = psbank(pin=True)
        (R, Rb) = psbank(pin=True)
        n = len(kchunks)
        sc = {}

        def emit_scores(i):
            (S, Sb) = psbank()
            pairs = score_fn(comp, kchunks[i])
            mm([(S[0:128, :], l, r, j == 0, j == len(pairs) - 1) for j, (l, r) in enumerate(pairs)],
               score_reads, [Sb])
            sc[i] = (S, Sb)

        emit_scores(0)
        if n > 1:
            emit_scores(1)
        for i in range(n):
            if i + 2 < n:
                emit_scores(i + 2)
            (S, Sb) = sc.pop(i)
            kc = kchunks[i]
            pT, pTb = ppT.get()
            if bias_fn is None:
                op(ACT, lambda e: e.activation(out=pT, in_=S[:, :], func=AF.Exp, scale=scale), [Sb], [pTb])
            else:
                bt, btb = bias_fn(kc)
                tf, tfb = tmpf.get()
                op(DVE, lambda e: e.scalar_tensor_tensor(out=tf, in0=S[:, :], scalar=scale, in1=bt, op0=ALU.mult,
                                                         op1=ALU.add), [Sb, btb], [tfb])
                op(ACT, lambda e: e.activation(out=pT, in_=tf, func=AF.Exp), [tfb], [pTb])
            mm([(O[:, :], Vt[:, kc, :], pT, i == 0, i == n - 1)], [pTb, Vb], [Ob])
            mm([(R[:, :], K.ones_bf[:, :], pT, i == 0, i == n - 1)], [pTb, K.cbuf], [Rb])
        res.append((O, Ob, R, Rb))
    return res


def build(nlayers=L, Lw=L, stop=None, moe_alloc=True, stop_layer=0, n_exp=8):
    nc = bass.Bass("TRN2", target_bir_lowering=False)

    def din(name, shape):
        return nc.dram_tensor(name, list(shape), F32, kind="ExternalInput").ap()

    def dout(name, shape):
        return nc.dram_tensor(name, list(shape), F32, kind="ExternalOutput").ap()

    xT = din("xT", [128, 16, NT])
    cond = din("cond", [128, 16])
    w_ada = din("w_ada", [Lw, 96, 128, 16, 128])
    b_ada = din("b_ada", [Lw, 128, 96])
    w64 = din("w64", [Lw, 34, 128, 16, 64])
    w128 = din("w128", [Lw, 22, 128, 16, 128])
    wuq_n = din("wuq_n", [Lw, 8, 128, 4, 128])
    wuq_r = din("wuq_r", [Lw, 8, 2, 128, 4, 64])
    wukv = din("wukv", [Lw, 8, 2, 128, 2, 128])
    w_out = din("w_out", [Lw, 16, 128, 16, 128])
    ffn_w1 = din("ffn_w1", [(Lw + 1) // 2, 43, 128, 16, 128])
    ffn_w3 = din("ffn_w3", [(Lw + 1) // 2, 43, 128, 16, 128])
    ffn_w2 = din("ffn_w2", [(Lw + 1) // 2, 43, 128, 2048])
    moe_w1 = din("moe_w1", [max(1, Lw // 2), n_exp, 22, 128, 16, 128] if moe_alloc else [1, 1, 1, 128, 16, 128])
    moe_w3 = din("moe_w3", [max(1, Lw // 2), n_exp, 22, 128, 16, 128] if moe_alloc else [1, 1, 1, 128, 16, 128])
    moe_w2 = din("moe_w2", [max(1, Lw // 2), n_exp, 22, 128, 2048] if moe_alloc else [1, 1, 1, 128, 2048])
    router = din("router", [max(1, Lw // 2), 128, 16, 8])
    lnp = din("lnp", [128, L * 4 * 16])
    gq = din("gq", [128, L * 4])
    gkv = din("gkv", [128, L * 2])
    subln = din("subln", [128, L])
    lamp = din("lamp", [64, 4 * L])
    ropec = din("ropec", [64, NT])
    ropes = din("ropes", [64, NT])
    kmask = din("kmask", [9, NKEY])
    qmask = din("qmask", [9, NT])
    c_dak = din("c_dak", [Lw, 4, 2, 64, 256])
    c_dav = din("c_dav", [Lw, 4, 128, 2, 128])
    c_ckv = din("c_ckv", [Lw, 128, 2, 256])
    c_kr = din("c_kr", [Lw, 64, 256])
    c_nak = din("c_nak", [Lw, 4, 128, 256])
    c_nav = din("c_nav", [Lw, 4, 128, 2, 128])
    nabias = din("nabias", [Lw, 4, NPAIR, 128, 512])
    ident_d = din("ident", [128, 128])

    yT = dout("yT", [128, 16, NT])
    o_dak = dout("o_dak", [L, 4, 2, 64, NT])
    o_dav = dout("o_dav", [L, 4, 128, 16, 128])
    o_ckv = dout("o_ckv", [L, 2, 128, NT])
    o_kr = dout("o_kr", [L, 64, NT])
    o_nak = dout("o_nak", [L, 4, 128, NT])
    o_nav = dout("o_nav", [L, 4, 128, 16, 128])
    tS = nc.dram_tensor("tS", [128, 16, NT], F32).ap()
    dbg1 = dout("dbg1", [128, 16, NT]) if stop is not None else None
    dbg2 = dout("dbg2", [128, 16, NT]) if stop is not None else None

    def finish():
        barrier()
        for c in range(16):
            dma(GP, dbg1[:, c, :], hT[:, c, :])
            dma(GP, dbg2[:, c, :], OT[:, c, :])
        barrier()
        return nc
    tSb = [Buf("tS%d" % i) for i in range(4)]

    sems = [nc.alloc_semaphore("s%d" % i) for i in range(3 + 16)]
    K.PE = Eng(nc.tensor, sems[0], is_pe=True)
    K.ACT = Eng(nc.scalar, sems[1])
    K.DVE = Eng(nc.vector, sems[2])
    K.SP = DQ(nc.sync, sems[3:11])
    K.GP = DQ(nc.gpsimd, sems[11:19])
    K.alt = 0
    PE, ACT, DVE, SP, GP = K.PE, K.ACT, K.DVE, K.SP, K.GP

    K.PS = []
    for i in range(8):
        t = nc.alloc_psum_tensor("ps%d" % i, [128, 512], F32)
        K.PS.append((t, Buf("ps%d" % i)))
    K.psi = 0
    K.pinned = set()

    A = Arena(nc, 203 * 1024)
    K.A = A
    hT = A.alloc([128, 16, NT], BF16)
    hTb = [Buf("hT%d" % i) for i in range(4)]
    R2 = A.off
    OT = A.alloc([128, 16, NT], BF16)
    OTb = [Buf("OT%d" % i) for i in range(4)]
    A.off = R2
    acc = A.alloc([128, 16, 1024], F32)
    accb = [Buf("acc%d" % i) for i in range(2)]
    K.cbuf = Buf("consts")
    cb = K.cbuf
    K.ones_bf = A.alloc([128, 128], BF16)
    ones_f = A.alloc([128, 128], F32)
    ident = A.alloc([128, 128], F32)
    mod = A.alloc([128, L * 96], F32)
    lnp_s = A.alloc([128, L * 64], F32)
    gq_s = A.alloc([128, L * 4], F32)
    gkv_s = A.alloc([128, L * 2], F32)
    sub_s = A.alloc([128, L], F32)
    nlam = A.alloc([128, L], F32)
    lam_t = A.alloc([128, 4 * L], F32)
    coef = A.alloc([128, 8 * 16], F32)
    coefb = Buf("coef")
    scond = A.alloc([128, 16], BF16)
    condf = A.alloc([128, 16], F32)
    gate_tm = A.alloc([128, 16 * 8], F32)
    gateb = Buf("gate")
    wr = A.alloc([128, 16 * 8], F32)
    wrA = A.alloc([128, 16 * 8], F32)
    wrB = A.alloc([128, 16 * 8], F32)
    wrb = Buf("wr")
    cstb = A.alloc([128, 8], F32)
    R3 = A.off

    op(DVE, lambda e: e.memset(K.ones_bf, 1.0), [], [cb])
    op(DVE, lambda e: e.memset(ones_f, 1.0), [], [cb])
    dma(SP, ident, ident_d, writes=[cb])
    dma(SP, lnp_s, lnp, writes=[cb])
    dma(SP, gq_s, gq, writes=[cb])
    dma(SP, gkv_s, gkv, writes=[cb])
    dma(SP, sub_s, subln, writes=[cb])
    dma(SP, condf, cond, writes=[cb])
    dma(SP, lam_t[0:64, :], lamp, writes=[cb])
    op(ACT, lambda e: e.activation(out=scond, in_=condf, func=AF.Silu), [cb], [cb])
    prods = A.alloc([128, 2 * L], F32)
    for l in range(L):
        for j in range(2):
            op(DVE, lambda e, l=l, j=j: e.tensor_tensor(out=prods[0:64, 2 * l + j:2 * l + j + 1],
                                                        in0=lam_t[0:64, 4 * l + 2 * j:4 * l + 2 * j + 1],
                                                        in1=lam_t[0:64, 4 * l + 2 * j + 1:4 * l + 2 * j + 2],
                                                        op=ALU.mult), [cb], [cb])
    (P0, P0b) = psbank()
    mm([(P0[:, 0:2 * L], ones_f[0:64, :], prods[0:64, :], True, True)], [cb], [P0b])
    op(ACT, lambda e: e.activation(out=prods, in_=P0[:, 0:2 * L], func=AF.Exp), [P0b], [cb])
    for l in range(L):
        op(DVE, lambda e, l=l: e.scalar_tensor_tensor(out=nlam[:, l:l + 1], in0=prods[:, 2 * l + 1:2 * l + 2],
                                                      scalar=-LAM_INIT[l], in1=prods[:, 2 * l:2 * l + 1],
                                                      op0=ALU.add, op1=ALU.subtract), [cb], [cb])
    m0 = A.off
    pada = Pool([128, 16, 128], BF16, 3, "wada")
    bada = A.alloc([128, 96], F32)
    for l in range(nlayers):
        (P1, P1b) = psbank()
        for j in range(96):
            wa, wab = loadw(pada, w_ada[l, j])
            mm([(P1[:, j:j + 1], wa[:, kc, :], scond[:, kc:kc + 1], kc == 0, kc == 15) for kc in range(16)],
               [wab, cb], [P1b])
        dma(SP, bada, b_ada[l], writes=[cb])
        op(DVE, lambda e, l=l: e.tensor_tensor(out=mod[:, l * 96:(l + 1) * 96], in0=P1[:, 0:96], in1=bada,
                                               op=ALU.add), [P1b, cb], [cb])
    barrier()
    A.off = m0

    def cvec(k):
        return coef[:, k * 16:(k + 1) * 16]

    def make_coefs(l, sub):
        base = l * 96 + (0 if sub == 0 else 48)
        sh = mod[:, base:base + 16]
        scl = mod[:, base + 16:base + 32]
        o0 = 0 if sub == 0 else 4
        if sub == 0 and l == 0:
            op(DVE, lambda e: e.tensor_scalar(out=cvec(0), in0=scl, scalar1=1.0, scalar2=None, op0=ALU.add),
               [cb, coefb], [coefb])
            op(DVE, lambda e: e.tensor_copy(out=cvec(1), in_=sh), [cb, coefb], [coefb])
            op(DVE, lambda e: e.memset(cvec(2), ALPHA), [coefb], [coefb])
            op(DVE, lambda e: e.memset(cvec(3), 0.0), [coefb], [coefb])
            return
        if sub == 0:
            Gp = lnp_s[:, (l - 1) * 64 + 32:(l - 1) * 64 + 48]
            Bp = lnp_s[:, (l - 1) * 64 + 48:(l - 1) * 64 + 64]
        else:
            Gp = lnp_s[:, l * 64:l * 64 + 16]
            Bp = lnp_s[:, l * 64 + 16:l * 64 + 32]
        op(DVE, lambda e: e.scalar_tensor_tensor(out=cvec(o0 + 0), in0=scl, scalar=1.0, in1=Gp, op0=ALU.add,
                                                 op1=ALU.mult), [cb, coefb], [coefb])
        op(DVE, lambda e: e.scalar_tensor_tensor(out=cvec(o0 + 1), in0=scl, scalar=1.0, in1=Bp, op0=ALU.add,
                                                 op1=ALU.mult), [cb, coefb], [coefb])
        op(DVE, lambda e: e.tensor_tensor(out=cvec(o0 + 1), in0=cvec(o0 + 1), in1=sh, op=ALU.add),
           [cb, coefb], [coefb])
        op(DVE, lambda e: e.tensor_scalar(out=cvec(o0 + 2), in0=Gp, scalar1=ALPHA, scalar2=None, op0=ALU.mult),
           [cb, coefb], [coefb])
        op(DVE, lambda e: e.tensor_scalar(out=cvec(o0 + 3), in0=Bp, scalar1=ALPHA, scalar2=None, op0=ALU.mult),
           [cb, coefb], [coefb])

    def col(v, c):
        return v[:, c:c + 1]

    for l in range(nlayers):
        is_moe = (l % 2 == 1)
        fi = l // 2
        src = xT if l == 0 else tS
        make_coefs(l, 0)
        make_coefs(l, 1)
        g1v = mod[:, l * 96 + 32:l * 96 + 48]
        g2v = mod[:, l * 96 + 80:l * 96 + 96]

        m0 = A.off
        tt = A.alloc([128, 16, 512], F32)
        ttb = Buf("tt")
        for qt in range(4):
            dma(SP, tt, src[:, :, qt * 512:(qt + 1) * 512], reads=[tSb[qt]], writes=[ttb])
            for c in range(16):
                affine(alt_eng(), hT[:, c, qt * 512:(qt + 1) * 512], tt[:, c, :], col(cvec(0), c), col(cvec(1), c),
                       [ttb, coefb], [hTb[qt]])
        barrier()
        A.off = m0
        if stop == "a" and l == stop_layer:
            return finish()

        m0 = A.off
        rc = A.alloc([64, 512], F32)
        rs = A.alloc([64, 512], F32)
        rcb = Buf("rope")
        Qc = [A.alloc([128, 512], BF16) for _ in range(2)]
        Qb = Buf("Q")
        Kc = [A.alloc([128, NKEY], BF16) for _ in range(2)]
        Kb = Buf("K")
        Vt = A.alloc([128, 18, 128], BF16)
        Vb = Buf("V")
        tA = A.alloc([128, 512], F32)
        tB = A.alloc([128, 512], F32)
        tC = A.alloc([128, 512], BF16)
        tAb, tBb, tCb = Buf("tA"), Buf("tB"), Buf("tC")
        pkf = Pool([128, 512], F32, 2, "kf")
        ppT = Pool([128, 512], BF16, 4, "pT")
        p64 = Pool([128, 16, 64], BF16, 3, "w64")
        p128 = Pool([128, 16, 128], BF16, 2, "w128")
        for c in range(2):
            dma(GP, Kc[c][64:73, :], kmask, writes=[Kb])
            dma(GP, Qc[c][64:73, :], qmask[:, 0:512], writes=[Qb])

        def rope_proj(l, blk, qt, rhs_of, nk, wsrc_plain, wsrc_rot, pool, M, out_bf, out_bf_b, out_f=None,
                      rhs_reads=()):
            wp, wpb = loadw(pool, wsrc_plain)
            wq, wqb = loadw(pool, wsrc_rot)
            (Pa, Pab) = psbank()
            (Pb, Pbb) = psbank()
            mm([(Pa[0:M, :], wp[:, kc, :], rhs_of(kc), kc == 0, kc == nk - 1) for kc in range(nk)],
               [wpb] + list(rhs_reads), [Pab])
            mm([(Pb[0:M, :], wq[:, kc, :], rhs_of(kc), kc == 0, kc == nk - 1) for kc in range(nk)],
               [wqb] + list(rhs_reads), [Pbb])
            op(DVE, lambda e: e.tensor_tensor(out=tA[0:M, :], in0=Pa[0:M, :], in1=rc[0:M, :], op=ALU.mult),
               [Pab, rcb], [tAb])
            op(DVE, lambda e: e.tensor_tensor(out=tB[0:M, :], in0=Pb[0:M, :], in1=rs[0:M, :], op=ALU.mult),
               [Pbb, rcb], [tBb])
            if out_f is None:
                op(DVE, lambda e: e.tensor_tensor(out=out_bf, in0=tA[0:M, :], in1=tB[0:M, :], op=ALU.add),
                   [tAb, tBb], [out_bf_b])
            else:
                (of, ofb) = out_f
                op(DVE, lambda e: e.tensor_tensor(out=of[0:M, :], in0=tA[0:M, :], in1=tB[0:M, :], op=ALU.add),
                   [tAb, tBb], [ofb])
                op(ACT, lambda e: e.activation(out=out_bf, in_=of[0:M, :], func=AF.Copy), [ofb], [out_bf_b])

        def load_rope(qt):
            dma(SP, rc, ropec[:, qt * 512:(qt + 1) * 512], writes=[rcb])
            dma(SP, rs, ropes[:, qt * 512:(qt + 1) * 512], writes=[rcb])

        def proj_v_tm(wv, wvb, lhs_of, nk, lhs_reads, hd, o_dst):
            for g in range(4):
                (Pv, Pvb) = psbank()
                mms = []
                for s in range(4):
                    tcn = g * 4 + s
                    for kc in range(nk):
                        mms.append((Pv[:, s * 128:(s + 1) * 128], lhs_of(kc, tcn), wv[:, kc, :], kc == 0, kc == nk - 1))
                mm(mms, [wvb] + list(lhs_reads), [Pvb])
                if o_dst is not None:
                    vo, vob = pkf.get()
                    op(DVE, lambda e: e.tensor_copy(out=vo, in_=Pv[:, :]), [Pvb], [vob])
                    dma(SP, o_dst[:, g * 4:(g + 1) * 4, :], vo.rearrange("p (a b) -> p a b", a=4), reads=[vob])
                if o_dst is not None:
                    op(ACT, lambda e: e.activation(out=Vt[:, g * 4:(g + 1) * 4, :],
                                                   in_=vo.rearrange("p (a b) -> p a b", a=4), func=AF.Copy),
                       [vob], [Vb])
                else:
                    op(ACT, lambda e: e.activation(out=Vt[:, g * 4:(g + 1) * 4, :],
                                                   in_=Pv[:, :].rearrange("p (a b) -> p a b", a=4), func=AF.Copy),
                       [Pvb], [Vb])

        for hd in range(4):
            for qt in range(4):
                load_rope(qt)
                for c in range(2):
                    blk = 16 + hd * 4 + c * 2
                    kf = pkf.get()
                    rope_proj(l, blk, qt, lambda kc, qt=qt: hT[:, kc, qt * 512:(qt + 1) * 512], 16,
                              w64[l, blk], w64[l, blk + 1], p64, 64,
                              Kc[c][0:64, qt * 512:(qt + 1) * 512], Kb, out_f=kf, rhs_reads=[hTb[qt]])
                    dma(SP, o_dak[l, hd, c, :, qt * 512:(qt + 1) * 512], kf[0][0:64, :], reads=[kf[1]])
            for c in range(2):
                dma(GP, Kc[c][0:64, 2048:NKEY], c_dak[l, hd, c], writes=[Kb])
            wv, wvb = loadw(p128, w128[l, 14 + hd])
            proj_v_tm(wv, wvb, lambda kc, tcn: hT[:, kc, tcn * 128:(tcn + 1) * 128], 16, hTb, hd, o_dav[l, hd])
            dma(GP, Vt[:, 16:18, :], c_dav[l, hd], writes=[Vb])
            for qt in range(4):
                load_rope(qt)
                for c in range(2):
                    blk = hd * 4 + c * 2
                    rope_proj(l, blk, qt, lambda kc, qt=qt: hT[:, kc, qt * 512:(qt + 1) * 512], 16,
                              w64[l, blk], w64[l, blk + 1], p64, 64, Qc[c][0:64, :], Qb, rhs_reads=[hTb[qt]])
                    if qt > 0 or hd > 0 or True:
                        dma(GP, Qc[c][64:73, :], qmask[:, qt * 512:(qt + 1) * 512], writes=[Qb])
                res = attention(2, list(range(18)),
                                lambda comp, kc: [(Kc[comp][0:73, kc * 128:(kc + 1) * 128], Qc[comp][0:73, :])],
                                [Kb, Qb], Vt, Vb, 0.125, None, ppT, None)
                (O1, O1b, R1, R1b), (O2, O2b, R2_, R2b) = res
                op(DVE, lambda e: e.reciprocal(out=tA, in_=R1[:, :]), [R1b], [tAb])
                op(DVE, lambda e: e.tensor_tensor(out=tA, in0=O1[:, :], in1=tA, op=ALU.mult), [O1b, tAb], [tAb])
                op(DVE, lambda e: e.reciprocal(out=tB, in_=R2_[:, :]), [R2b], [tBb])
                op(DVE, lambda e: e.tensor_tensor(out=tB, in0=O2[:, :], in1=tB, op=ALU.mult), [O2b, tBb], [tBb])
                op(DVE, lambda e: e.scalar_tensor_tensor(out=tA, in0=tB, scalar=nlam[:, l:l + 1], in1=tA,
                                                         op0=ALU.mult, op1=ALU.add), [tAb, tBb, cb], [tAb])
                op(DVE, lambda e: e.tensor_tensor(out=tC, in0=tA, in1=tA, op=ALU.mult), [tAb], [tCb])
                (X, Xb) = psbank()
                mm([(X[:, :], K.ones_bf[:, :], tC, True, True)], [tCb, cb], [Xb])
                op(ACT, lambda e: e.activation(out=tB, in_=X[:, :], func=AF.Sqrt, bias=1e-6, scale=1.0 / 128),
                   [Xb], [tBb])
                op(DVE, lambda e: e.reciprocal(out=tB, in_=tB), [tBb], [tBb])
                op(DVE, lambda e: e.tensor_tensor(out=tA, in0=tA, in1=tB, op=ALU.mult), [tAb, tBb], [tAb])
                op(DVE, lambda e: e.tensor_scalar(out=OT[:, hd, qt * 512:(qt + 1) * 512], in0=tA,
                                                  scalar1=sub_s[:, l:l + 1], scalar2=1.0 - LAM_INIT[l],
                                                  op0=ALU.mult, op1=ALU.mult), [tAb, cb], [OTb[qt]])
                K.pinned.clear()
        barrier()
        A.off = m0
        if stop == "da" and l == stop_layer:
            return finish()

        m0 = A.off
        cqn = A.alloc([128, 4, NT], BF16)
        cqnb = Buf("cqn")
        ckv_all = A.alloc([128, 2, NKEY], BF16)
        ckvb = Buf("ckv")
        kr_all = A.alloc([128, NKEY], BF16)
        krb = Buf("kr")
        rc = A.alloc([64, 512], F32)
        rs = A.alloc([64, 512], F32)
        rcb = Buf("rope")
        tA = A.alloc([128, 512], F32)
        tB = A.alloc([128, 512], F32)
        tC = A.alloc([128, 512], BF16)
        tAb, tBb, tCb = Buf("tA"), Buf("tB"), Buf("tC")
        m1 = A.off
        p128 = Pool([128, 16, 128], BF16, 2, "w128")
        p64 = Pool([128, 16, 64], BF16, 3, "w64")
        pkf = Pool([128, 512], F32, 2, "kf")
        dma(GP, kr_all[64:73, :], kmask, writes=[krb])
        dma(GP, ckv_all[:, :, 2048:NKEY], c_ckv[l], writes=[ckvb])
        dma(GP, kr_all[0:64, 2048:NKEY], c_kr[l], writes=[krb])

        def rms_blocks(nblk, blk0, qt, gcol, dst_of, dst_b, out_dst):
            banks = []
            for c in range(nblk):
                wp, wpb = loadw(p128, w128[l, blk0 + c])
                (Pq, Pqb) = psbank(pin=True)
                mm([(Pq[:, :], wp[:, kc, :], hT[:, kc, qt * 512:(qt + 1) * 512], kc == 0, kc == 15)
                    for kc in range(16)], [wpb, hTb[qt]], [Pqb])
                banks.append((Pq, Pqb))
            (X, Xb) = psbank()
            for c in range(nblk):
                op(ACT, lambda e, c=c: e.activation(out=tC, in_=banks[c][0][:, :], func=AF.Square), [banks[c][1]], [tCb])
                mm([(X[:, :], K.ones_bf[:, :], tC, c == 0, c == nblk - 1)], [tCb, cb], [Xb])
            op(ACT, lambda e: e.activation(out=tB, in_=X[:, :], func=AF.Sqrt, bias=1e-6, scale=1.0 / (128 * nblk)),
               [Xb], [tBb])
            op(DVE, lambda e: e.reciprocal(out=tB, in_=tB), [tBb], [tBb])
            for c in range(nblk):
                op(DVE, lambda e, c=c: e.tensor_tensor(out=tA, in0=banks[c][0][:, :], in1=tB, op=ALU.mult),
                   [banks[c][1], tBb], [tAb])
                if out_dst is None:
                    op(DVE, lambda e, c=c: e.tensor_scalar(out=dst_of(c), in0=tA, scalar1=gcol(c), scalar2=None,
                                                           op0=ALU.mult), [tAb, cb], [dst_b])
                else:
                    kf, kfb = pkf.get()
                    op(DVE, lambda e, c=c: e.tensor_scalar(out=kf, in0=tA, scalar1=gcol(c), scalar2=None,
                                                           op0=ALU.mult), [tAb, cb], [kfb])
                    dma(SP, out_dst(c), kf, reads=[kfb])
                    op(ACT, lambda e, c=c: e.activation(out=dst_of(c), in_=kf, func=AF.Copy), [kfb], [dst_b])
            K.pinned.clear()

        for qt in range(4):
            rms_blocks(4, 0, qt, lambda c: gq_s[:, l * 4 + c:l * 4 + c + 1],
                       lambda c, qt=qt: cqn[:, c, qt * 512:(qt + 1) * 512], cqnb, None)
            rms_blocks(2, 4, qt, lambda c: gkv_s[:, l * 2 + c:l * 2 + c + 1],
                       lambda c, qt=qt: ckv_all[:, c, qt * 512:(qt + 1) * 512], ckvb,
                       lambda c, qt=qt: o_ckv[l, c, :, qt * 512:(qt + 1) * 512])
            load_rope(qt)
            kf = pkf.get()
            rope_proj(l, 32, qt, lambda kc, qt=qt: hT[:, kc, qt * 512:(qt + 1) * 512], 16, w64[l, 32], w64[l, 33],
                      p64, 64, kr_all[0:64, qt * 512:(qt + 1) * 512], krb, out_f=kf, rhs_reads=[hTb[qt]])
            dma(SP, o_kr[l, :, qt * 512:(qt + 1) * 512], kf[0][0:64, :], reads=[kf[1]])
        barrier()
        A.off = m1
        kn = A.alloc([128, NKEY], BF16)
        knb = Buf("kn")
        Vt = A.alloc([128, 18, 128], BF16)
        Vb = Buf("V")
        qn = A.alloc([128, 512], BF16)
        qnb = Buf("qn")
        qr = A.alloc([128, 512], BF16)
        qrb = Buf("qr")
        ppT = Pool([128, 512], BF16, 4, "pT")
        pwn = Pool([128, 4, 128], BF16, 2, "wuqn")
        pwr = Pool([128, 4, 64], BF16, 2, "wuqr")
        pwk = Pool([128, 2, 128], BF16, 2, "wukv")
        for hd in range(8):
            wk, wkb = loadw(pwk, wukv[l, hd, 0])
            wv, wvb = loadw(pwk, wukv[l, hd, 1])
            for t5 in range(5):
                n = 512 if t5 < 4 else 256
                (Pk, Pkb) = psbank()
                mm([(Pk[:, 0:n], wk[:, fc, :], ckv_all[:, fc, t5 * 512:t5 * 512 + n], fc == 0, fc == 1)
                    for fc in range(2)], [wkb, ckvb], [Pkb])
                op(ACT, lambda e: e.activation(out=kn[:, t5 * 512:t5 * 512 + n], in_=Pk[:, 0:n], func=AF.Copy),
                   [Pkb], [knb])
            for g in range(5):
                ns = 4 if g < 4 else 2
                (Pv, Pvb) = psbank()
                mms = []
                for s in range(ns):
                    tcn = g * 4 + s
                    for fc in range(2):
                        mms.append((Pv[:, s * 128:(s + 1) * 128], ckv_all[:, fc, tcn * 128:(tcn + 1) * 128],
                                    wv[:, fc, :], fc == 0, fc == 1))
                mm(mms, [wvb, ckvb], [Pvb])
                op(ACT, lambda e: e.activation(out=Vt[:, g * 4:g * 4 + ns, :],
                                               in_=Pv[:, 0:ns * 128].rearrange("p (a b) -> p a b", a=ns),
                                               func=AF.Copy), [Pvb], [Vb])
            wn, wnb = loadw(pwn, wuq_n[l, hd])
            wrp, wrpb = pwr.get()
            dma(GP, wrp, wuq_r[l, hd, 0], writes=[wrpb])
            wrr, wrrb = pwr.get()
            dma(GP, wrr, wuq_r[l, hd, 1], writes=[wrrb])
            for qt in range(4):
                (Pq, Pqb) = psbank()
                mm([(Pq[:, :], wn[:, kc, :], cqn[:, kc, qt * 512:(qt + 1) * 512], kc == 0, kc == 3)
                    for kc in range(4)], [wnb, cqnb], [Pqb])
                op(ACT, lambda e: e.activation(out=qn, in_=Pq[:, :], func=AF.Copy), [Pqb], [qnb])
                load_rope(qt)
                (Pa, Pab) = psbank()
                (Pb, Pbb) = psbank()
                mm([(Pa[0:64, :], wrp[:, kc, :], cqn[:, kc, qt * 512:(qt + 1) * 512], kc == 0, kc == 3)
                    for kc in range(4)], [wrpb, cqnb], [Pab])
                mm([(Pb[0:64, :], wrr[:, kc, :], cqn[:, kc, qt * 512:(qt + 1) * 512], kc == 0, kc == 3)
                    for kc in range(4)], [wrrb, cqnb], [Pbb])
                op(DVE, lambda e: e.tensor_tensor(out=tA[0:64, :], in0=Pa[0:64, :], in1=rc, op=ALU.mult),
                   [Pab, rcb], [tAb])
                op(DVE, lambda e: e.tensor_tensor(out=tB[0:64, :], in0=Pb[0:64, :], in1=rs, op=ALU.mult),
                   [Pbb, rcb], [tBb])
                op(DVE, lambda e: e.tensor_tensor(out=qr[0:64, :], in0=tA[0:64, :], in1=tB[0:64, :], op=ALU.add),
                   [tAb, tBb], [qrb])
                dma(GP, qr[64:73, :], qmask[:, qt * 512:(qt + 1) * 512], writes=[qrb])
                res = attention(1, list(range(18)),
                                lambda comp, kc: [(kn[:, kc * 128:(kc + 1) * 128], qn[:, :]),
                                                  (kr_all[0:73, kc * 128:(kc + 1) * 128], qr[0:73, :])],
                                [knb, qnb, krb, qrb], Vt, Vb, 192.0 ** -0.5, None, ppT, None)
                (O1, O1b, R1, R1b) = res[0]
                op(DVE, lambda e: e.reciprocal(out=tA, in_=R1[:, :]), [R1b], [tAb])
                op(DVE, lambda e: e.tensor_tensor(out=OT[:, 4 + hd, qt * 512:(qt + 1) * 512], in0=O1[:, :], in1=tA,
                                                  op=ALU.mult), [O1b, tAb], [OTb[qt]])
                K.pinned.clear()
        barrier()
        A.off = m0
        if stop == "mla" and l == stop_layer:
            return finish()

        m0 = A.off
        qn = A.alloc([128, 512], BF16)
        qnb = Buf("qn")
        kn = A.alloc([128, NKEY], BF16)
        knb = Buf("kn")
        Vt = A.alloc([128, 18, 128], BF16)
        Vb = Buf("V")
        tA = A.alloc([128, 512], F32)
        tAb = Buf("tA")
        pbias = Pool([128, 512], F32, 2, "bias")
        ptf = Pool([128, 512], F32, 2, "tf")
        ppT = Pool([128, 512], BF16, 4, "pT")
        p128 = Pool([128, 16, 128], BF16, 2, "w128")
        pkf = Pool([128, 512], F32, 2, "kf")
        for hd in range(4):
            wk, wkb = loadw(p128, w128[l, 10 + hd])
            for qt in range(4):
                (Pk, Pkb) = psbank()
                mm([(Pk[:, :], wk[:, kc, :], hT[:, kc, qt * 512:(qt + 1) * 512], kc == 0, kc == 15)
                    for kc in range(16)], [wkb, hTb[qt]], [Pkb])
                kf, kfb = pkf.get()
                op(DVE, lambda e: e.tensor_copy(out=kf, in_=Pk[:, :]), [Pkb], [kfb])
                dma(SP, o_nak[l, hd, :, qt * 512:(qt + 1) * 512], kf, reads=[kfb])
                op(ACT, lambda e: e.activation(out=kn[:, qt * 512:(qt + 1) * 512], in_=kf, func=AF.Copy),
                   [kfb], [knb])
            dma(GP, kn[:, 2048:NKEY], c_nak[l, hd], writes=[knb])
            wv, wvb = loadw(p128, w128[l, 18 + hd])
            proj_v_tm(wv, wvb, lambda kc, tcn: hT[:, kc, tcn * 128:(tcn + 1) * 128], 16, hTb, hd, o_nav[l, hd])
            dma(GP, Vt[:, 16:18, :], c_nav[l, hd], writes=[Vb])
            wq, wqb = loadw(p128, w128[l, 6 + hd])
            for qt in range(4):
                (Pq, Pqb) = psbank()
                mm([(Pq[:, :], wq[:, kc, :], hT[:, kc, qt * 512:(qt + 1) * 512], kc == 0, kc == 15)
                    for kc in range(16)], [wqb, hTb[qt]], [Pqb])
                op(ACT, lambda e: e.activation(out=qn, in_=Pq[:, :], func=AF.Copy), [Pqb], [qnb])

                def bias_fn(kc, qt=qt, hd=hd):
                    bt, btb = pbias.get()
                    dma(SP, bt, nabias[l, hd, NA_PAIR[(qt, kc)]], writes=[btb])
                    return bt, btb

                res = attention(1, NA_CH[qt], lambda comp, kc: [(kn[:, kc * 128:(kc + 1) * 128], qn[:, :])],
                                [knb, qnb], Vt, Vb, 128.0 ** -0.5, bias_fn, ppT, ptf)
                (O1, O1b, R1, R1b) = res[0]
                op(DVE, lambda e: e.reciprocal(out=tA, in_=R1[:, :]), [R1b], [tAb])
                op(DVE, lambda e: e.tensor_tensor(out=OT[:, 12 + hd, qt * 512:(qt + 1) * 512], in0=O1[:, :], in1=tA,
                                                  op=ALU.mult), [O1b, tAb], [OTb[qt]])
                K.pinned.clear()
        barrier()
        A.off = m0
        if stop == "na" and l == stop_layer:
            return finish()

        m0 = A.off
        tt = A.alloc([128, 16, 512], F32)
        ttb = Buf("tt")
        pzb = Pool([128, 512], BF16, 4, "zb")
        tM = (A.alloc([128, 512], F32), Buf("M"))
        tV = (A.alloc([128, 512], F32), Buf("V"))
        p128 = Pool([128, 16, 128], BF16, 3, "wo")
        lg = A.alloc([128, 8], F32)
        lg2 = A.alloc([128, 8], F32)
        e1 = A.alloc([128, 8], F32)
        e2 = A.alloc([128, 8], F32)
        sm = A.alloc([128, 8], F32)
        smb = Buf("sm")
        if is_moe:
            dma(SP, wr, router[fi].rearrange("p a b -> p (a b)"), writes=[wrb])
            for c in range(16):
                op(DVE, lambda e, c=c: e.tensor_scalar(out=wrA[:, c * 8:(c + 1) * 8], in0=wr[:, c * 8:(c + 1) * 8],
                                                       scalar1=col(cvec(4), c), scalar2=None, op0=ALU.mult),
                   [wrb, coefb], [wrb])
                op(DVE, lambda e, c=c: e.tensor_scalar(out=wrB[:, c * 8:(c + 1) * 8], in0=wr[:, c * 8:(c + 1) * 8],
                                                       scalar1=col(cvec(5), c), scalar2=None, op0=ALU.mult),
                   [wrb, coefb], [wrb])
            (Pc, Pcb) = psbank()
            mm([(Pc[:, 0:8], ones_f[:, :], wrB[:, c * 8:(c + 1) * 8], c == 0, c == 15) for c in range(16)],
               [wrb, cb], [Pcb])
            op(DVE, lambda e: e.tensor_copy(out=cstb, in_=Pc[:, 0:8]), [Pcb], [wrb])
        for qt in range(4):
            dma(SP, tt, src[:, :, qt * 512:(qt + 1) * 512], reads=[tSb[qt]], writes=[ttb])
            for j in range(16):
                wo, wob = loadw(p128, w_out[l, j])
                (Pm, Pmb) = psbank()
                mm([(Pm[:, :], wo[:, kc, :], OT[:, kc, qt * 512:(qt + 1) * 512], kc == 0, kc == 15)
                    for kc in range(16)], [wob, OTb[qt]], [Pmb])
                affine(ACT, tt[:, j, :], tt[:, j, :], col(cvec(2), j), col(cvec(3), j), [ttb, coefb], [ttb])
                op(DVE, lambda e, j=j: e.scalar_tensor_tensor(out=tt[:, j, :], in0=Pm[:, :], scalar=col(g1v, j),
                                                              in1=tt[:, j, :], op0=ALU.mult, op1=ALU.add),
                   [Pmb, ttb, cb], [ttb])
            layernorm(lambda c: tt[:, c, :], ttb, pzb, tM, tV)
            for c in range(16):
                affine(alt_eng(), hT[:, c, qt * 512:(qt + 1) * 512], tt[:, c, :], col(cvec(4), c), col(cvec(5), c),
                       [ttb, coefb], [hTb[qt]])
            if is_moe:
                for sb in range(4):
                    (Pl, Plb) = psbank()
                    mm([(Pl[:, 0:8], tt[:, c, sb * 128:(sb + 1) * 128], wrA[:, c * 8:(c + 1) * 8], c == 0, c == 15)
                        for c in range(16)], [ttb, wrb], [Plb])
                    go = gate_tm[:, (qt * 4 + sb) * 8:(qt * 4 + sb + 1) * 8]
                    op(DVE, lambda e: e.tensor_tensor(out=lg, in0=Pl[:, 0:8], in1=cstb, op=ALU.add), [Plb, wrb], [smb])
                    op(DVE, lambda e: e.tensor_reduce(out=sm[:, 0:1], in_=lg, axis=mybir.AxisListType.X, op=ALU.max),
                       [smb], [smb])
                    op(DVE, lambda e: e.tensor_scalar(out=e1, in0=lg, scalar1=sm[:, 0:1], scalar2=None,
                                                      op0=ALU.is_equal), [smb], [smb])
                    op(DVE, lambda e: e.scalar_tensor_tensor(out=lg2, in0=e1, scalar=-1e30, in1=lg, op0=ALU.mult,
                                                             op1=ALU.add), [smb], [smb])
                    op(DVE, lambda e: e.tensor_reduce(out=sm[:, 1:2], in_=lg2, axis=mybir.AxisListType.X, op=ALU.max),
                       [smb], [smb])
                    op(DVE, lambda e: e.tensor_scalar(out=e2, in0=lg2, scalar1=sm[:, 1:2], scalar2=None,
                                                      op0=ALU.is_equal), [smb], [smb])
                    op(DVE, lambda e: e.tensor_tensor(out=sm[:, 2:3], in0=sm[:, 1:2], in1=sm[:, 0:1], op=ALU.subtract),
                       [smb], [smb])
                    op(ACT, lambda e: e.activation(out=sm[:, 3:4], in_=sm[:, 2:3], func=AF.Exp), [smb], [smb])
                    op(DVE, lambda e: e.tensor_scalar(out=sm[:, 4:5], in0=sm[:, 3:4], scalar1=1.0, scalar2=None,
                                                      op0=ALU.add), [smb], [smb])
                    op(DVE, lambda e: e.reciprocal(out=sm[:, 5:6], in_=sm[:, 4:5]), [smb], [smb])
                    op(DVE, lambda e: e.tensor_tensor(out=sm[:, 6:7], in0=sm[:, 3:4], in1=sm[:, 5:6], op=ALU.mult),
                       [smb], [smb])
                    op(DVE, lambda e: e.tensor_scalar(out=e1, in0=e1, scalar1=sm[:, 5:6], scalar2=None, op0=ALU.mult),
                       [smb], [smb])
                    op(DVE, lambda e: e.scalar_tensor_tensor(out=go, in0=e2, scalar=sm[:, 6:7], in1=e1, op0=ALU.mult,
                                                             op1=ALU.add), [smb, gateb], [gateb])
            for c in range(16):
                affine(alt_eng(), tt[:, c, :], tt[:, c, :], col(cvec(6), c), col(cvec(7), c), [ttb, coefb], [ttb])
            dma(SP, tS[:, :, qt * 512:(qt + 1) * 512], tt, reads=[ttb], writes=[tSb[qt]])
        barrier()
        A.off = m0
        if stop == "e" and l == stop_layer:
            return finish()

        m0 = A.off
        p13 = Pool([128, 16, 128], BF16, 4, "w13")
        p2 = Pool([128, 2048], BF16, GF + 1, "w2")
        pu = Pool([128, 1024], BF16, 2 * GF, "u")
        ptf = Pool([128, 512], F32, 2, "tf")
        gbc = A.alloc([128, 1024], F32)
        gbcb = Buf("gbc")
        grep = A.alloc([128, 128], F32)
        grepb = Buf("grep")
        if is_moe:
            chunks = [(e_, f) for e_ in range(n_exp) for f in range(22)]
        else:
            chunks = [(0, f) for f in range(43)]
        for ps_ in range(2):
            for tl in range(2):
                dma(SP, acc[:, :, tl * 512:(tl + 1) * 512], tS[:, :, ps_ * 1024 + tl * 512: ps_ * 1024 + (tl + 1) * 512],
                    reads=[tSb[ps_ * 2 + tl]], writes=[accb[tl]])
            group = []
            cur_e = -1
            for ci, (e_, f) in enumerate(chunks):
                if is_moe and e_ != cur_e:
                    cur_e = e_
                    for blk in range(8):
                        gb = (ps_ * 8 + blk) * 8 + e_
                        op(DVE, lambda e, gb=gb: e.tensor_copy(out=grep, in_=gate_tm[:, gb:gb + 1].to_broadcast([128, 128])),
                           [gateb], [grepb])
                        (Pg, Pgb) = psbank()
                        mm([(Pg[:, 0:128], grep, ident, True, True)], [grepb, cb], [Pgb])
                        op(ACT, lambda e, blk=blk: e.activation(out=gbc[:, blk * 128:(blk + 1) * 128], in_=Pg[:, 0:128],
                                                                func=AF.Copy), [Pgb], [gbcb])
                if is_moe:
                    s1, s3, s2 = moe_w1[fi, e_, f], moe_w3[fi, e_, f], moe_w2[fi, e_, f]
                else:
                    s1, s3, s2 = ffn_w1[fi, f], ffn_w3[fi, f], ffn_w2[fi, f]
                w1b, w1bb = loadw(p13, s1)
                w3b, w3bb = loadw(p13, s3)
                w2b, w2bb = loadw(p2, s2)
                u, ub = pu.get()
                for tl in range(2):
                    t0 = ps_ * 1024 + tl * 512
                    (G1, G1b) = psbank()
                    (G3, G3b) = psbank()
                    mm([(G1[:, :], w1b[:, kc, :], hT[:, kc, t0:t0 + 512], kc == 0, kc == 15) for kc in range(16)],
                       [w1bb, hTb[ps_ * 2 + tl]], [G1b])
                    mm([(G3[:, :], w3b[:, kc, :], hT[:, kc, t0:t0 + 512], kc == 0, kc == 15) for kc in range(16)],
                       [w3bb, hTb[ps_ * 2 + tl]], [G3b])
                    tf, tfb = ptf.get()
                    op(ACT, lambda e: e.activation(out=tf, in_=G1[:, :], func=AF.Silu), [G1b], [tfb])
                    if is_moe:
                        op(DVE, lambda e: e.tensor_tensor(out=tf, in0=G3[:, :], in1=tf, op=ALU.mult), [G3b, tfb], [tfb])
                        op(DVE, lambda e: e.tensor_tensor(out=u[:, tl * 512:(tl + 1) * 512], in0=tf,
                                                          in1=gbc[:, tl * 512:(tl + 1) * 512], op=ALU.mult),
                           [tfb, gbcb], [ub])
                    else:
                        op(DVE, lambda e: e.tensor_tensor(out=u[:, tl * 512:(tl + 1) * 512], in0=G3[:, :], in1=tf,
                                                          op=ALU.mult), [G3b, tfb], [ub])
                group.append((w2b, w2bb, u, ub))
                last_of_e = (ci + 1 == len(chunks)) or (chunks[ci + 1][0] != e_)
                if len(group) == GF or last_of_e:
                    for tl in range(2):
                        for j in range(16):
                            (Pw, Pwb) = psbank()
                            mm([(Pw[:, :], g_[0][:, j * 128:(j + 1) * 128], g_[2][:, tl * 512:(tl + 1) * 512],
                                 gi == 0, gi == len(group) - 1) for gi, g_ in enumerate(group)],
                               [g_[1] for g_ in group] + [g_[3] for g_ in group], [Pwb])
                            op(DVE, lambda e, j=j, tl=tl: e.scalar_tensor_tensor(
                                out=acc[:, j, tl * 512:(tl + 1) * 512], in0=Pw[:, :], scalar=col(g2v, j),
                                in1=acc[:, j, tl * 512:(tl + 1) * 512], op0=ALU.mult, op1=ALU.add),
                               [Pwb, accb[tl], cb], [accb[tl]])
                    group = []
            m2 = A.off
            pzb = Pool([128, 512], BF16, 4, "zb")
            tM = (A.alloc([128, 512], F32), Buf("M"))
            tV = (A.alloc([128, 512], F32), Buf("V"))
            for tl in range(2):
                qt = ps_ * 2 + tl
                layernorm(lambda c, tl=tl: acc[:, c, tl * 512:(tl + 1) * 512], accb[tl], pzb, tM, tV)
                if l == nlayers - 1:
                    for c in range(16):
                        affine(alt_eng(), acc[:, c, tl * 512:(tl + 1) * 512], acc[:, c, tl * 512:(tl + 1) * 512],
                               lnp_s[:, l * 64 + 32 + c:l * 64 + 33 + c], lnp_s[:, l * 64 + 48 + c:l * 64 + 49 + c],
                               [accb[tl], cb], [accb[tl]])
                    dma(SP, yT[:, :, qt * 512:(qt + 1) * 512], acc[:, :, tl * 512:(tl + 1) * 512], reads=[accb[tl]])
                else:
                    dma(SP, tS[:, :, qt * 512:(qt + 1) * 512], acc[:, :, tl * 512:(tl + 1) * 512], reads=[accb[tl]],
                        writes=[tSb[qt]])
            barrier()
            A.off = m2
        barrier()
        A.off = m0

    barrier()
    return nc


def _fm(x):
    T = x.shape[0]
    return np.ascontiguousarray(x.reshape(T, 16, 128).transpose(2, 1, 0))


def _pcol(v):
    return np.ascontiguousarray(v.reshape(-1, 128).T)


def _wblk(W, c0, m):
    Kd = W.shape[0]
    return np.ascontiguousarray(W[:, c0:c0 + m].reshape(Kd // 128, 128, m).transpose(1, 0, 2))


def _rot_perm():
    perm = np.zeros(64, np.int64)
    sign = np.zeros(64, np.float32)
    for a in range(2):
        for i in range(16):
            perm[a * 32 + i] = a * 32 + 16 + i
            sign[a * 32 + i] = -1.0
            perm[a * 32 + 16 + i] = a * 32 + i
            sign[a * 32 + 16 + i] = 1.0
    return perm, sign


def _rope_tables(n):
    t = np.arange(n)
    row = (t // 64).astype(np.float32)
    colv = (t % 64).astype(np.float32)
    inv = (10000.0 ** (-np.arange(0, 32, 2, dtype=np.float32) / 32)).astype(np.float32)
    ar = row[:, None] * inv[None, :]
    ac = colv[:, None] * inv[None, :]
    ang = np.concatenate([ar, ar, ac, ac], axis=-1)
    return np.cos(ang).astype(np.float32), np.sin(ang).astype(np.float32)


def _na_bias_sample(rpb_l):
    out = np.full((4, NPAIR, 128, 512), -30000.0, np.float32)
    r = np.arange(32)
    row_start = np.clip(r - 4, 0, 24)
    cq = np.arange(64)
    col_start = np.clip(cq - 8, 0, 48)
    for (qt, kc), pi in NA_PAIR.items():
        if kc >= 16:
            out[:, pi] = 0.0
            continue
        kk = kc * 128 + np.arange(128)
        qq = qt * 512 + np.arange(512)
        kr_, kcol = kk // 64, kk % 64
        qr_, qcol = qq // 64, qq % 64
        rs_ = row_start[qr_]
        cs_ = col_start[qcol]
        valid = ((kr_[:, None] >= rs_[None, :]) & (kr_[:, None] < rs_[None, :] + 8)
                 & (kcol[:, None] >= cs_[None, :]) & (kcol[:, None] < cs_[None, :] + 16))
        roff = np.clip(kr_[:, None] - qr_[None, :] + 7, 0, 14)
        coff = np.clip(kcol[:, None] - qcol[None, :], -15, 15) + 15
        g = rpb_l[:, roff, coff]
        out[:, pi] = np.where(valid[None], g, np.float32(-30000.0))
    return out


def _na_bias_prompt():
    out = np.full((NPAIR, 128, 512), -30000.0, np.float32)
    for (qt, kc), pi in NA_PAIR.items():
        if kc >= 16:
            continue
        kk = kc * 128 + np.arange(128)
        qq = qt * 512 + np.arange(512)
        out[pi] = np.where((kk[:, None] // 256) == (qq[None, :] // 256), np.float32(0.0), np.float32(-30000.0))
    return out


_NC_CACHE = {}


def prep(inp, Lw=L):
    f = lambda k: np.asarray(inp[k], dtype=np.float32)
    perm, sign = _rot_perm()
    w_in = f("w_in")
    shared = {}
    wa = f("w_ada")
    shared["w_ada"] = np.ascontiguousarray(wa.reshape(L, 16, 128, 96, 128).transpose(0, 3, 2, 1, 4))
    shared["b_ada"] = np.ascontiguousarray(f("b_ada").reshape(L, 96, 128).transpose(0, 2, 1))
    w64 = np.zeros((L, 34, 128, 16, 64), np.float32)
    w128 = np.zeros((L, 22, 128, 16, 128), np.float32)
    OFF = {"dq": 0, "dk": 512, "dv": 1024, "cq": 1536, "ckv": 2048, "kr": 2304, "nq": 2368, "nk": 2880, "nv": 3392}
    for l in range(L):
        W = w_in[l]
        for sec, b0 in (("dq", 0), ("dk", 16)):
            for hd in range(4):
                for c in range(2):
                    c0 = OFF[sec] + hd * 128 + c * 64
                    blkW = W[:, c0:c0 + 64]
                    w64[l, b0 + hd * 4 + c * 2] = blkW.reshape(16, 128, 64).transpose(1, 0, 2)
                    w64[l, b0 + hd * 4 + c * 2 + 1] = blkW[:, perm].reshape(16, 128, 64).transpose(1, 0, 2)
        blkW = W[:, OFF["kr"]:OFF["kr"] + 64]
        w64[l, 32] = blkW.reshape(16, 128, 64).transpose(1, 0, 2)
        w64[l, 33] = blkW[:, perm].reshape(16, 128, 64).transpose(1, 0, 2)
        for i in range(4):
            w128[l, i] = _wblk(W, OFF["cq"] + i * 128, 128)
            w128[l, 6 + i] = _wblk(W, OFF["nq"] + i * 128, 128)
            w128[l, 10 + i] = _wblk(W, OFF["nk"] + i * 128, 128)
            w128[l, 14 + i] = _wblk(W, OFF["dv"] + i * 128, 128)
            w128[l, 18 + i] = _wblk(W, OFF["nv"] + i * 128, 128)
        for i in range(2):
            w128[l, 4 + i] = _wblk(W, OFF["ckv"] + i * 128, 128)
    shared["w64"] = w64
    shared["w128"] = w128
    wuq = f("mla_wuq")
    wukv = f("mla_wukv")
    wuq_n = np.zeros((L, 8, 128, 4, 128), np.float32)
    wuq_r = np.zeros((L, 8, 2, 128, 4, 64), np.float32)
    wukv_r = np.zeros((L, 8, 2, 128, 2, 128), np.float32)
    for l in range(L):
        for hd in range(8):
            wuq_n[l, hd] = _wblk(wuq[l], hd * 192, 128)
            rb = wuq[l][:, hd * 192 + 128: hd * 192 + 192]
            wuq_r[l, hd, 0] = rb.reshape(4, 128, 64).transpose(1, 0, 2)
            wuq_r[l, hd, 1] = rb[:, perm].reshape(4, 128, 64).transpose(1, 0, 2)
            wukv_r[l, hd, 0] = _wblk(wukv[l], hd * 256, 128)
            wukv_r[l, hd, 1] = _wblk(wukv[l], hd * 256 + 128, 128)
    shared["wuq_n"] = wuq_n
    shared["wuq_r"] = wuq_r
    shared["wukv"] = wukv_r
    wo = f("w_out")
    shared["w_out"] = np.ascontiguousarray(wo.reshape(L, 16, 128, 16, 128).transpose(0, 3, 2, 1, 4))
    for nm in ("ffn_w1", "ffn_w3"):
        shared[nm] = np.ascontiguousarray(f(nm).reshape(2, 16, 128, 43, 128).transpose(0, 3, 2, 1, 4))
    shared["ffn_w2"] = np.ascontiguousarray(f("ffn_w2").reshape(2, 43, 128, 2048))
    for nm in ("moe_w1", "moe_w3"):
        shared[nm] = np.ascontiguousarray(f(nm).reshape(2, 8, 16, 128, 22, 128).transpose(0, 1, 4, 3, 2, 5))
    shared["moe_w2"] = np.ascontiguousarray(f("moe_w2").reshape(2, 8, 22, 128, 2048))
    shared["router"] = np.ascontiguousarray(f("moe_router").reshape(2, 16, 128, 8).transpose(0, 2, 1, 3))
    lnp = np.zeros((128, L * 64), np.float32)
    for l in range(L):
        for i, nm in enumerate(("ln1_g", "ln1_b", "ln2_g", "ln2_b")):
            lnp[:, l * 64 + i * 16:l * 64 + (i + 1) * 16] = _pcol(f(nm)[l])
    shared["lnp"] = lnp
    shared["gq"] = np.concatenate([_pcol(f("mla_gq")[l]) for l in range(L)], axis=1)
    shared["gkv"] = np.concatenate([_pcol(f("mla_gkv")[l]) for l in range(L)], axis=1)
    shared["subln"] = np.concatenate([_pcol(f("da_subln")[l]) for l in range(L)], axis=1)
    lamp = np.zeros((64, 4 * L), np.float32)
    for l in range(L):
        for i, nm in enumerate(("da_lq1", "da_lk1", "da_lq2", "da_lk2")):
            lamp[:, 4 * l + i] = f(nm)[l]
    shared["lamp"] = lamp
    shared["ident"] = np.eye(128, dtype=np.float32)

    cos, sin = _rope_tables(2048)
    ropec_s = np.ascontiguousarray(cos.T)
    ropes_s = np.ascontiguousarray((sin * sign[None, :]).T)
    ropec_p = np.ones((64, 2048), np.float32)
    ropes_p = np.zeros((64, 2048), np.float32)
    km_p = np.zeros((9, NKEY), np.float32)
    qm_p = np.zeros((9, NT), np.float32)
    for j in range(8):
        km_p[j, j * 256:(j + 1) * 256] = 1.0
        qm_p[j, :] = NBIG
        qm_p[j, j * 256:(j + 1) * 256] = 0.0
    km_p[8, 2048:] = 1.0
    qm_p[8, :] = NBIG
    km_s = np.zeros((9, NKEY), np.float32)
    km_s[0, :2048] = 1.0
    km_s[8, 2048:] = 1.0
    qm_s = np.zeros((9, NT), np.float32)
    nab_p1 = _na_bias_prompt()
    nab_p = np.ascontiguousarray(np.broadcast_to(nab_p1[None, None], (L, 4, NPAIR, 128, 512)))
    rpb = f("na_rpb")
    xp = f("x_prompt")
    xs = f("x_sample")
    c = f("c")
    cctx = f("c_ctx")
    zc = {
        "c_dak": np.zeros((L, 4, 2, 64, 256), np.float32), "c_dav": np.zeros((L, 4, 128, 2, 128), np.float32),
        "c_ckv": np.zeros((L, 128, 2, 256), np.float32), "c_kr": np.zeros((L, 64, 256), np.float32),
        "c_nak": np.zeros((L, 4, 128, 256), np.float32), "c_nav": np.zeros((L, 4, 128, 2, 128), np.float32),
    }
    in_maps = []
    for r in range(8):
        m = dict(shared)
        if r in (4, 5):
            b = r - 4
            m["xT"] = _fm(xs[b])
            m["cond"] = _pcol(c[b])
            m["ropec"], m["ropes"], m["kmask"], m["qmask"] = ropec_s, ropes_s, km_s, qm_s
            dk = f("cache_da_k")[b]
            m["c_dak"] = np.ascontiguousarray(dk.transpose(0, 2, 3, 4, 1))
            dv = f("cache_da_v")[b]
            m["c_dav"] = np.ascontiguousarray(dv.reshape(L, 2, 128, 4, 128).transpose(0, 3, 2, 1, 4))
            ck = f("cache_mla_ckv")[b]
            m["c_ckv"] = np.ascontiguousarray(ck.reshape(L, 256, 2, 128).transpose(0, 3, 2, 1))
            m["c_kr"] = np.ascontiguousarray(f("cache_mla_krope")[b].transpose(0, 2, 1))
            nk = f("cache_na_k")[b]
            m["c_nak"] = np.ascontiguousarray(nk.transpose(0, 2, 3, 1))
            nv = f("cache_na_v")[b]
            m["c_nav"] = np.ascontiguousarray(nv.reshape(L, 2, 128, 4, 128).transpose(0, 3, 2, 1, 4))
            m["nabias"] = np.stack([_na_bias_sample(rpb[l]) for l in range(L)], axis=0)
        else:
            rr = r if r < 4 else 0
            m["xT"] = _fm(xp[rr * 8:(rr + 1) * 8].reshape(2048, 2048))
            m["cond"] = _pcol(cctx)
            m["ropec"], m["ropes"], m["kmask"], m["qmask"] = ropec_p, ropes_p, km_p, qm_p
            m.update(zc)
            m["nabias"] = nab_p
        in_maps.append(m)

    return in_maps


def kernel(**inp):
    in_maps = prep(inp)
    if "nc" not in _NC_CACHE:
        _NC_CACHE["nc"] = build()
    nc = _NC_CACHE["nc"]
    res = run_bass_kernel_spmd(nc, in_maps, core_ids=list(range(8)))
    return post(res.results)


def post(R):

    def tm(yT):
        return yT.transpose(2, 1, 0).reshape(2048, 2048)

    y_p = np.concatenate([tm(R[r]["yT"]).reshape(8, 256, 2048) for r in range(4)], axis=0)
    y_s = np.stack([tm(R[4]["yT"]), tm(R[5]["yT"])], axis=0)
    dak = np.concatenate([R[r]["o_dak"].transpose(4, 0, 1, 2, 3).reshape(8, 256, L, 4, 2, 64).transpose(0, 2, 1, 3, 4, 5)
                          for r in range(4)], axis=0)
    dav = np.concatenate([R[r]["o_dav"].transpose(3, 2, 0, 1, 4).reshape(8, 256, L, 4, 128).transpose(0, 2, 1, 3, 4)
                          for r in range(4)], axis=0)
    ckv = np.concatenate([R[r]["o_ckv"].transpose(3, 0, 1, 2).reshape(8, 256, L, 256).transpose(0, 2, 1, 3)
                          for r in range(4)], axis=0)
    kr = np.concatenate([R[r]["o_kr"].transpose(2, 0, 1).reshape(8, 256, L, 64).transpose(0, 2, 1, 3)
                         for r in range(4)], axis=0)
    nak = np.concatenate([R[r]["o_nak"].transpose(3, 0, 1, 2).reshape(8, 256, L, 4, 128).transpose(0, 2, 1, 3, 4)
                          for r in range(4)], axis=0)
    nav = np.concatenate([R[r]["o_nav"].transpose(3, 2, 0, 1, 4).reshape(8, 256, L, 4, 128).transpose(0, 2, 1, 3, 4)
                          for r in range(4)], axis=0)
    outs = (y_p, y_s, dak, dav, ckv, kr, nak, nav)
    return tuple(np.ascontiguousarray(o, dtype=np.float32) for o in outs)
```

```python
import math
import numpy as np
import concourse.bass as bass
import concourse.mybir as mybir
from concourse.bass_utils import run_bass_kernel_spmd

F32 = mybir.dt.float32
BF16 = mybir.dt.bfloat16
AF = mybir.ActivationFunctionType
ALU = mybir.AluOpType

L = 4
NT = 2048
NKEY = 2304
ALPHA = 8.0 ** 0.25
LAM_INIT = [0.8 - 0.6 * math.exp(-0.3 * l) for l in range(L)]
NBIG = -32768.0
NA_CH = []
for _qt in range(4):
    NA_CH.append(list(range(max(0, 4 * _qt - 2), min(16, 4 * _qt + 6))) + [16, 17])
NA_PAIR = {}
for _qt in range(4):
    for _kc in NA_CH[_qt]:
        NA_PAIR[(_qt, _kc)] = len(NA_PAIR)
NPAIR = len(NA_PAIR)
GF = 3


class Tok:
    __slots__ = ("sem", "val")

    def __init__(self, sem, val):
        self.sem = sem
        self.val = val


class Buf:
    def __init__(self, name=""):
        self.name = name
        self.w = None
        self.r = {}


class Eng:
    def __init__(self, e, sem, is_pe=False):
        self.e = e
        self.sem = sem
        self.cnt = 0
        self.known = {}
        self.is_pe = is_pe

    def wait(self, tok):
        if tok is None:
            return
        if self.is_pe and tok.sem is self.sem:
            return
        k = id(tok.sem)
        if self.known.get(k, 0) >= tok.val:
            return
        self.e.wait_ge(tok.sem, tok.val)
        self.nwait = getattr(self, 'nwait', 0) + 1
        self.known[k] = tok.val


def _deps(E, reads, writes):
    for b in reads:
        E.wait(b.w)
    for b in writes:
        E.wait(b.w)
        for t in list(b.r.values()):
            E.wait(t)


def _commit(tok, reads, writes):
    for b in reads:
        b.r[id(tok.sem)] = tok
    for b in writes:
        b.w = tok
        b.r = {}


class K:
    pass


def op(E, fn, reads=(), writes=()):
    _deps(E, reads, writes)
    ins = fn(E.e)
    E.cnt += 1
    ins.then_inc(E.sem, 1)
    tok = Tok(E.sem, E.cnt)
    _commit(tok, reads, writes)
    return tok


def mm(mms, reads=(), writes=()):
    PE = K.PE
    _deps(PE, reads, writes)
    ins = None
    for (o, l, r, st, sp) in mms:
        ins = PE.e.matmul(o, lhsT=l, rhs=r, start=st, stop=sp)
        PE.nmm = getattr(PE, 'nmm', 0) + 1
    PE.cnt += 1
    ins.then_inc(PE.sem, 1)
    tok = Tok(PE.sem, PE.cnt)
    _commit(tok, reads, writes)
    return tok


class DQ:
    def __init__(self, e, sems):
        self.E = Eng(e, None)
        self.slots = [[s, 0] for s in sems]
        self.i = 0


def dma(Q, out, in_, reads=(), writes=()):
    E = Q.E
    _deps(E, reads, writes)
    sl = Q.slots[Q.i]
    Q.i = (Q.i + 1) % len(Q.slots)
    if sl[1] > 0:
        E.wait(Tok(sl[0], sl[1]))
    ins = E.e.dma_start(out=out, in_=in_)
    E.ndma = getattr(E, 'ndma', 0) + 1
    sl[1] += 16
    ins.then_inc(sl[0], 16)
    tok = Tok(sl[0], sl[1])
    _commit(tok, reads, writes)
    return tok


def barrier():
    toks = [Tok(E.sem, E.cnt) for E in (K.PE, K.ACT, K.DVE) if E.cnt > 0]
    for Q in (K.SP, K.GP):
        for s in Q.slots:
            if s[1] > 0:
                toks.append(Tok(s[0], s[1]))
    for E in (K.PE, K.ACT, K.DVE, K.SP.E, K.GP.E):
        for t in toks:
            E.wait(t)


class Arena:
    def __init__(self, nc, nbytes):
        self.f = nc.alloc_sbuf_tensor("arena", [128, nbytes // 4], F32)
        self.b = self.f.bitcast(BF16)
        self.off = 0
        self.cap = nbytes
        self.peak = 0

    def alloc(self, shape, dt):
        esz = 4 if dt == F32 else 2
        n = 1
        for s in shape[1:]:
            n *= s
        nb = (n * esz + 31) // 32 * 32
        off = self.off
        self.off += nb
        self.peak = max(self.peak, self.off)
        assert self.off <= self.cap, ("arena overflow", self.off, self.cap)
        if dt == F32:
            ap = self.f[0:shape[0], off // 4: off // 4 + n]
        else:
            ap = self.b[0:shape[0], off // 2: off // 2 + n]
        if len(shape) == 3:
            ap = ap.rearrange("p (a b) -> p a b", a=shape[1])
        return ap


class Pool:
    def __init__(self, shape, dt, n, name):
        self.items = [(K.A.alloc(shape, dt), Buf(name + str(i))) for i in range(n)]
        self.i = 0

    def get(self):
        it = self.items[self.i]
        self.i = (self.i + 1) % len(self.items)
        return it


def psbank(pin=False):
    while True:
        i = K.psi
        K.psi = (K.psi + 1) % len(K.PS)
        if i not in K.pinned:
            break
    if pin:
        K.pinned.add(i)
    return K.PS[i]


def loadw(pool, src):
    ap, b = pool.get()
    dma(K.GP, ap, src, writes=[b])
    return ap, b


def alt_eng():
    K.alt ^= 1
    return K.ACT if K.alt else K.DVE


def affine(E, out, in_, sc, bi, reads, writes):
    if E is K.ACT:
        op(E, lambda e: e.activation(out=out, in_=in_, func=AF.Identity, bias=bi, scale=sc), reads, writes)
    else:
        op(E, lambda e: e.tensor_scalar(out=out, in0=in_, scalar1=sc, scalar2=bi, op0=ALU.mult, op1=ALU.add),
           reads, writes)


def layernorm(zc, zbuf, pzb, tM, tV):
    ACT, DVE = K.ACT, K.DVE
    (S1, S1b) = psbank(pin=True)
    (S2, S2b) = psbank(pin=True)
    for c in range(16):
        zb, zbb = pzb.get()
        op(ACT, lambda e: e.activation(out=zb, in_=zc(c), func=AF.Copy), [zbuf], [zbb])
        mm([(S1[:, :], K.ones_bf[:, :], zb, c == 0, c == 15)], [zbb, K.cbuf], [S1b])
        zq, zqb = pzb.get()
        op(ACT, lambda e: e.activation(out=zq, in_=zc(c), func=AF.Square), [zbuf], [zqb])
        mm([(S2[:, :], K.ones_bf[:, :], zq, c == 0, c == 15)], [zqb, K.cbuf], [S2b])
    (M, Mb) = tM
    (V, Vb) = tV
    op(DVE, lambda e: e.tensor_scalar(out=M, in0=S1[:, :], scalar1=1.0 / 2048, scalar2=None, op0=ALU.mult), [S1b], [Mb])
    op(DVE, lambda e: e.tensor_tensor(out=V, in0=M, in1=M, op=ALU.mult), [Mb], [Vb])
    op(DVE, lambda e: e.scalar_tensor_tensor(out=V, in0=S2[:, :], scalar=1.0 / 2048, in1=V, op0=ALU.mult,
                                             op1=ALU.subtract), [S2b, Vb], [Vb])
    op(ACT, lambda e: e.activation(out=V, in_=V, func=AF.Sqrt, bias=1e-5, scale=1.0), [Vb], [Vb])
    op(DVE, lambda e: e.reciprocal(out=V, in_=V), [Vb], [Vb])
    for c in range(16):
        op(DVE, lambda e: e.tensor_tensor(out=zc(c), in0=zc(c), in1=M, op=ALU.subtract), [zbuf, Mb], [zbuf])
        op(DVE, lambda e: e.tensor_tensor(out=zc(c), in0=zc(c), in1=V, op=ALU.mult), [zbuf, Vb], [zbuf])
    K.pinned.clear()


def attention(ncomp, kchunks, score_fn, score_reads, Vt, Vb, scale, bias_fn, ppT, tmpf):
    ACT, DVE = K.ACT, K.DVE
    res = []
    for comp in range(ncomp):
        (O, Ob) = psbank(pin=True)
        (R, Rb) = psbank(pin=True)
        n = len(kchunks)
        sc = {}

        def emit_scores(i):
            (S, Sb) = psbank()
            pairs = score_fn(comp, kchunks[i])
            mm([(S[0:128, :], l, r, j == 0, j == len(pairs) - 1) for j, (l, r) in enumerate(pairs)],
               score_reads, [Sb])
            sc[i] = (S, Sb)

        emit_scores(0)
        if n > 1:
            emit_scores(1)
        for i in range(n):
            if i + 2 < n:
                emit_scores(i + 2)
            (S, Sb) = sc.pop(i)
            kc = kchunks[i]
            pT, pTb = ppT.get()
            if bias_fn is None:
                op(ACT, lambda e: e.activation(out=pT, in_=S[:, :], func=AF.Exp, scale=scale), [Sb], [pTb])
            else:
                bt, btb = bias_fn(kc)
                tf, tfb = tmpf.get()
                op(DVE, lambda e: e.scalar_tensor_tensor(out=tf, in0=S[:, :], scalar=scale, in1=bt, op0=ALU.mult,
                                                         op1=ALU.add), [Sb, btb], [tfb])
                op(ACT, lambda e: e.activation(out=pT, in_=tf, func=AF.Exp), [tfb], [pTb])
            mm([(O[:, :], Vt[:, kc, :], pT, i == 0, i == n - 1)], [pTb, Vb], [Ob])
            mm([(R[:, :], K.ones_bf[:, :], pT, i == 0, i == n - 1)], [pTb, K.cbuf], [Rb])
        res.append((O, Ob, R, Rb))
    return res


def build(nlayers=L, Lw=L, stop=None, moe_alloc=True, stop_layer=0, n_exp=8):
    nc = bass.Bass("TRN2", target_bir_lowering=False)

    def din(name, shape):
        return nc.dram_tensor(name, list(shape), F32, kind="ExternalInput").ap()

    def dout(name, shape):
        return nc.dram_tensor(name, list(shape), F32, kind="ExternalOutput").ap()

    xT = din("xT", [128, 16, NT])
    cond = din("cond", [128, 16])
    w_ada = din("w_ada", [Lw, 96, 128, 16, 128])
    b_ada = din("b_ada", [Lw, 128, 96])
    w64 = din("w64", [Lw, 34, 128, 16, 64])
    w128 = din("w128", [Lw, 22, 128, 16, 128])
    wuq_n = din("wuq_n", [Lw, 8, 128, 4, 128])
    wuq_r = din("wuq_r", [Lw, 8, 2, 128, 4, 64])
    wukv = din("wukv", [Lw, 8, 2, 128, 2, 128])
    w_out = din("w_out", [Lw, 16, 128, 16, 128])
    ffn_w1 = din("ffn_w1", [(Lw + 1) // 2, 43, 128, 16, 128])
    ffn_w3 = din("ffn_w3", [(Lw + 1) // 2, 43, 128, 16, 128])
    ffn_w2 = din("ffn_w2", [(Lw + 1) // 2, 43, 128, 2048])
    moe_w1 = din("moe_w1", [max(1, Lw // 2), n_exp, 22, 128, 16, 128] if moe_alloc else [1, 1, 1, 128, 16, 128])
    moe_w3 = din("moe_w3", [max(1, Lw // 2), n_exp, 22, 128, 16, 128] if moe_alloc else [1, 1, 1, 128, 16, 128])
    moe_w2 = din("moe_w2", [max(1, Lw // 2), n_exp, 22, 128, 2048] if moe_alloc else [1, 1, 1, 128, 2048])
    router = din("router", [max(1, Lw // 2), 128, 16, 8])
    lnp = din("lnp", [128, L * 4 * 16])
    gq = din("gq", [128, L * 4])
    gkv = din("gkv", [128, L * 2])
    subln = din("subln", [128, L])
    lamp = din("lamp", [64, 4 * L])
    ropec = din("ropec", [64, NT])
    ropes = din("ropes", [64, NT])
    kmask = din("kmask", [9, NKEY])
    qmask = din("qmask", [9, NT])
    c_dak = din("c_dak", [Lw, 4, 2, 64, 256])
    c_dav = din("c_dav", [Lw, 4, 128, 2, 128])
    c_ckv = din("c_ckv", [Lw, 128, 2, 256])
    c_kr = din("c_kr", [Lw, 64, 256])
    c_nak = din("c_nak", [Lw, 4, 128, 256])
    c_nav = din("c_nav", [Lw, 4, 128, 2, 128])
    nabias = din("nabias", [Lw, 4, NPAIR, 128, 512])
    ident_d = din("ident", [128, 128])

    yT = dout("yT", [128, 16, NT])
    o_dak = dout("o_dak", [L, 4, 2, 64, NT])
    o_dav = dout("o_dav", [L, 4, 128, 16, 128])
    o_ckv = dout("o_ckv", [L, 2, 128, NT])
    o_kr = dout("o_kr", [L, 64, NT])
    o_nak = dout("o_nak", [L, 4, 128, NT])
    o_nav = dout("o_nav", [L, 4, 128, 16, 128])
    tS = nc.dram_tensor("tS", [128, 16, NT], F32).ap()
    dbg1 = dout("dbg1", [128, 16, NT]) if stop is not None else None
    dbg2 = dout("dbg2", [128, 16, NT]) if stop is not None else None

    def finish():
        barrier()
        for c in range(16):
            dma(GP, dbg1[:, c, :], hT[:, c, :])
            dma(GP, dbg2[:, c, :], OT[:, c, :])
        barrier()
        return nc
    tSb = [Buf("tS%d" % i) for i in range(4)]

    sems = [nc.alloc_semaphore("s%d" % i) for i in range(3 + 16)]
    K.PE = Eng(nc.tensor, sems[0], is_pe=True)
    K.ACT = Eng(nc.scalar, sems[1])
    K.DVE = Eng(nc.vector, sems[2])
    K.SP = DQ(nc.sync, sems[3:11])
    K.GP = DQ(nc.gpsimd, sems[11:19])
    K.alt = 0
    PE, ACT, DVE, SP, GP = K.PE, K.ACT, K.DVE, K.SP, K.GP

    K.PS = []
    for i in range(8):
        t = nc.alloc_psum_tensor("ps%d" % i, [128, 512], F32)
        K.PS.append((t, Buf("ps%d" % i)))
    K.psi = 0
    K.pinned = set()

    A = Arena(nc, 203 * 1024)
    K.A = A
    hT = A.alloc([128, 16, NT], BF16)
    hTb = [Buf("hT%d" % i) for i in range(4)]
    R2 = A.off
    OT = A.alloc([128, 16, NT], BF16)
    OTb = [Buf("OT%d" % i) for i in range(4)]
    A.off = R2
    acc = A.alloc([128, 16, 1024], F32)
    accb = [Buf("acc%d" % i) for i in range(2)]
    K.cbuf = Buf("consts")
    cb = K.cbuf
    K.ones_bf = A.alloc([128, 128], BF16)
    ones_f = A.alloc([128, 128], F32)
    ident = A.alloc([128, 128], F32)
    mod = A.alloc([128, L * 96], F32)
    lnp_s = A.alloc([128, L * 64], F32)
    gq_s = A.alloc([128, L * 4], F32)
    gkv_s = A.alloc([128, L * 2], F32)
    sub_s = A.alloc([128, L], F32)
    nlam = A.alloc([128, L], F32)
    lam_t = A.alloc([128, 4 * L], F32)
    coef = A.alloc([128, 8 * 16], F32)
    coefb = Buf("coef")
    scond = A.alloc([128, 16], BF16)
    condf = A.alloc([128, 16], F32)
    gate_tm = A.alloc([128, 16 * 8], F32)
    gateb = Buf("gate")
    wr = A.alloc([128, 16 * 8], F32)
    wrA = A.alloc([128, 16 * 8], F32)
    wrB = A.alloc([128, 16 * 8], F32)
    wrb = Buf("wr")
    cstb = A.alloc([128, 8], F32)
    R3 = A.off

    op(DVE, lambda e: e.memset(K.ones_bf, 1.0), [], [cb])
    op(DVE, lambda e: e.memset(ones_f, 1.0), [], [cb])
    dma(SP, ident, ident_d, writes=[cb])
    dma(SP, lnp_s, lnp, writes=[cb])
    dma(SP, gq_s, gq, writes=[cb])
    dma(SP, gkv_s, gkv, writes=[cb])
    dma(SP, sub_s, subln, writes=[cb])
    dma(SP, condf, cond, writes=[cb])
    dma(SP, lam_t[0:64, :], lamp, writes=[cb])
    op(ACT, lambda e: e.activation(out=scond, in_=condf, func=AF.Silu), [cb], [cb])
    prods = A.alloc([128, 2 * L], F32)
    for l in range(L):
        for j in range(2):
            op(DVE, lambda e, l=l, j=j: e.tensor_tensor(out=prods[0:64, 2 * l + j:2 * l + j + 1],
                                                        in0=lam_t[0:64, 4 * l + 2 * j:4 * l + 2 * j + 1],
                                                        in1=lam_t[0:64, 4 * l + 2 * j + 1:4 * l + 2 * j + 2],
                                                        op=ALU.mult), [cb], [cb])
    (P0, P0b) = psbank()
    mm([(P0[:, 0:2 * L], ones_f[0:64, :], prods[0:64, :], True, True)], [cb], [P0b])
    op(ACT, lambda e: e.activation(out=prods, in_=P0[:, 0:2 * L], func=AF.Exp), [P0b], [cb])
    for l in range(L):
        op(DVE, lambda e, l=l: e.scalar_tensor_tensor(out=nlam[:, l:l + 1], in0=prods[:, 2 * l + 1:2 * l + 2],
                                                      scalar=-LAM_INIT[l], in1=prods[:, 2 * l:2 * l + 1],
                                                      op0=ALU.add, op1=ALU.subtract), [cb], [cb])
    m0 = A.off
    pada = Pool([128, 16, 128], BF16, 3, "wada")
    bada = A.alloc([128, 96], F32)
    for l in range(nlayers):
        (P1, P1b) = psbank()
        for j in range(96):
            wa, wab = loadw(pada, w_ada[l, j])
            mm([(P1[:, j:j + 1], wa[:, kc, :], scond[:, kc:kc + 1], kc == 0, kc == 15) for kc in range(16)],
               [wab, cb], [P1b])
        dma(SP, bada, b_ada[l], writes=[cb])
        op(DVE, lambda e, l=l: e.tensor_tensor(out=mod[:, l * 96:(l + 1) * 96], in0=P1[:, 0:96], in1=bada,
                                               op=ALU.add), [P1b, cb], [cb])
    barrier()
    A.off = m0

    def cvec(k):
        return coef[:, k * 16:(k + 1) * 16]

    def make_coefs(l, sub):
        base = l * 96 + (0 if sub == 0 else 48)
        sh = mod[:, base:base + 16]
        scl = mod[:, base + 16:base + 32]
        o0 = 0 if sub == 0 else 4
        if sub == 0 and l == 0:
            op(DVE, lambda e: e.tensor_scalar(out=cvec(0), in0=scl, scalar1=1.0, scalar2=None, op0=ALU.add),
               [cb, coefb], [coefb])
            op(DVE, lambda e: e.tensor_copy(out=cvec(1), in_=sh), [cb, coefb], [coefb])
            op(DVE, lambda e: e.memset(cvec(2), ALPHA), [coefb], [coefb])
            op(DVE, lambda e: e.memset(cvec(3), 0.0), [coefb], [coefb])
            return
        if sub == 0:
            Gp = lnp_s[:, (l - 1) * 64 + 32:(l - 1) * 64 + 48]
            Bp = lnp_s[:, (l - 1) * 64 + 48:(l - 1) * 64 + 64]
        else:
            Gp = lnp_s[:, l * 64:l * 64 + 16]
            Bp = lnp_s[:, l * 64 + 16:l * 64 + 32]
        op(DVE, lambda e: e.scalar_tensor_tensor(out=cvec(o0 + 0), in0=scl, scalar=1.0, in1=Gp, op0=ALU.add,
                                                 op1=ALU.mult), [cb, coefb], [coefb])
        op(DVE, lambda e: e.scalar_tensor_tensor(out=cvec(o0 + 1), in0=scl, scalar=1.0, in1=Bp, op0=ALU.add,
                                                 op1=ALU.mult), [cb, coefb], [coefb])
        op(DVE, lambda e: e.tensor_tensor(out=cvec(o0 + 1), in0=cvec(o0 + 1), in1=sh, op=ALU.add),
           [cb, coefb], [coefb])
        op(DVE, lambda e: e.tensor_scalar(out=cvec(o0 + 2), in0=Gp, scalar1=ALPHA, scalar2=None, op0=ALU.mult),
           [cb, coefb], [coefb])
        op(DVE, lambda e: e.tensor_scalar(out=cvec(o0 + 3), in0=Bp, scalar1=ALPHA, scalar2=None, op0=ALU.mult),
           [cb, coefb], [coefb])

    def col(v, c):
        return v[:, c:c + 1]

    for l in range(nlayers):
        is_moe = (l % 2 == 1)
        fi = l // 2
        src = xT if l == 0 else tS
        make_coefs(l, 0)
        make_coefs(l, 1)
        g1v = mod[:, l * 96 + 32:l * 96 + 48]
        g2v = mod[:, l * 96 + 80:l * 96 + 96]

        m0 = A.off
        tt = A.alloc([128, 16, 512], F32)
        ttb = Buf("tt")
        for qt in range(4):
            dma(SP, tt, src[:, :, qt * 512:(qt + 1) * 512], reads=[tSb[qt]], writes=[ttb])
            for c in range(16):
                affine(alt_eng(), hT[:, c, qt * 512:(qt + 1) * 512], tt[:, c, :], col(cvec(0), c), col(cvec(1), c),
                       [ttb, coefb], [hTb[qt]])
        barrier()
        A.off = m0
        if stop == "a" and l == stop_layer:
            return finish()

        m0 = A.off
        rc = A.alloc([64, 512], F32)
        rs = A.alloc([64, 512], F32)
        rcb = Buf("rope")
        Qc = [A.alloc([128, NT], BF16) for _ in range(2)]
        Qb = [Buf("Q%d" % i) for i in range(4)]
        Kc = [A.alloc([128, NKEY], BF16) for _ in range(2)]
        Kb = Buf("K")
        Vt = A.alloc([128, 18, 128], BF16)
        Vb = Buf("V")
        tA = A.alloc([128, 512], F32)
        tB = A.alloc([128, 512], F32)
        tC = A.alloc([128, 512], BF16)
        tAb, tBb, tCb = Buf("tA"), Buf("tB"), Buf("tC")
        pkf = Pool([128, 512], F32, 2, "kf")
        ppT = Pool([128, 512], BF16, 4, "pT")
        p64 = Pool([128, 16, 64], BF16, 3, "w64")
        p128 = Pool([128, 16, 128], BF16, 2, "w128")
        for c in range(2):
            dma(GP, Kc[c][64:73, :], kmask, writes=[Kb])
            dma(GP, Qc[c][64:73, :], qmask, writes=Qb)

        def rope_proj(l, blk, qt, rhs_of, nk, wsrc_plain, wsrc_rot, pool, M, out_bf, out_bf_b, out_f=None,
                      rhs_reads=()):
            wp, wpb = loadw(pool, wsrc_plain)
            wq, wqb = loadw(pool, wsrc_rot)
            (Pa, Pab) = psbank()
            (Pb, Pbb) = psbank()
            mm([(Pa[0:M, :], wp[:, kc, :], rhs_of(kc), kc == 0, kc == nk - 1) for kc in range(nk)],
               [wpb] + list(rhs_reads), [Pab])
            mm([(Pb[0:M, :], wq[:, kc, :], rhs_of(kc), kc == 0, kc == nk - 1) for kc in range(nk)],
               [wqb] + list(rhs_reads), [Pbb])
            op(DVE, lambda e: e.tensor_tensor(out=tA[0:M, :], in0=Pa[0:M, :], in1=rc[0:M, :], op=ALU.mult),
               [Pab, rcb], [tAb])
            op(DVE, lambda e: e.tensor_tensor(out=tB[0:M, :], in0=Pb[0:M, :], in1=rs[0:M, :], op=ALU.mult),
               [Pbb, rcb], [tBb])
            if out_f is None:
                op(DVE, lambda e: e.tensor_tensor(out=out_bf, in0=tA[0:M, :], in1=tB[0:M, :], op=ALU.add),
                   [tAb, tBb], [out_bf_b])
            else:
                (of, ofb) = out_f
                op(DVE, lambda e: e.tensor_tensor(out=of[0:M, :], in0=tA[0:M, :], in1=tB[0:M, :], op=ALU.add),
                   [tAb, tBb], [ofb])
                op(ACT, lambda e: e.activation(out=out_bf, in_=of[0:M, :], func=AF.Copy), [ofb], [out_bf_b])

        def load_rope(qt):
            dma(SP, rc, ropec[:, qt * 512:(qt + 1) * 512], writes=[rcb])
            dma(SP, rs, ropes[:, qt * 512:(qt + 1) * 512], writes=[rcb])

        def proj_v_tm(wv, wvb, lhs_of, nk, lhs_reads, hd, o_dst):
            for g in range(4):
                (Pv, Pvb) = psbank()
                mms = []
                for s in range(4):
                    tcn = g * 4 + s
                    for kc in range(nk):
                        mms.append((Pv[:, s * 128:(s + 1) * 128], lhs_of(kc, tcn), wv[:, kc, :], kc == 0, kc == nk - 1))
                mm(mms, [wvb] + list(lhs_reads), [Pvb])
                if o_dst is not None:
                    vo, vob = pkf.get()
                    op(DVE, lambda e: e.tensor_copy(out=vo, in_=Pv[:, :]), [Pvb], [vob])
                    dma(SP, o_dst[:, g * 4:(g + 1) * 4, :], vo.rearrange("p (a b) -> p a b", a=4), reads=[vob])
                if o_dst is not None:
                    op(ACT, lambda e: e.activation(out=Vt[:, g * 4:(g + 1) * 4, :],
                                                   in_=vo.rearrange("p (a b) -> p a b", a=4), func=AF.Copy),
                       [vob], [Vb])
                else:
                    op(ACT, lambda e: e.activation(out=Vt[:, g * 4:(g + 1) * 4, :],
                                                   in_=Pv[:, :].rearrange("p (a b) -> p a b", a=4), func=AF.Copy),
                       [Pvb], [Vb])

        for hd in range(4):
            for qt in range(4):
                load_rope(qt)
                for c in range(2):
                    blk = 16 + hd * 4 + c * 2
                    kf = pkf.get()
                    rope_proj(l, blk, qt, lambda kc, qt=qt: hT[:, kc, qt * 512:(qt + 1) * 512], 16,
                              w64[l, blk], w64[l, blk + 1], p64, 64,
                              Kc[c][0:64, qt * 512:(qt + 1) * 512], Kb, out_f=kf, rhs_reads=[hTb[qt]])
                    dma(SP, o_dak[l, hd, c, :, qt * 512:(qt + 1) * 512], kf[0][0:64, :], reads=[kf[1]])
            for c in range(2):
                dma(GP, Kc[c][0:64, 2048:NKEY], c_dak[l, hd, c], writes=[Kb])
            wv, wvb = loadw(p128, w128[l, 14 + hd])
            proj_v_tm(wv, wvb, lambda kc, tcn: hT[:, kc, tcn * 128:(tcn + 1) * 128], 16, hTb, hd, o_dav[l, hd])
            dma(GP, Vt[:, 16:18, :], c_dav[l, hd], writes=[Vb])
            for qt in range(4):
                load_rope(qt)
                for c in range(2):
                    blk = hd * 4 + c * 2
                    rope_proj(l, blk, qt, lambda kc, qt=qt: hT[:, kc, qt * 512:(qt + 1) * 512], 16,
                              w64[l, blk], w64[l, blk + 1], p64, 64, Qc[c][0:64, qt * 512:(qt + 1) * 512], Qb[qt],
                              rhs_reads=[hTb[qt]])
                res = attention(2, list(range(18)),
                                lambda comp, kc, qt=qt: [(Kc[comp][0:73, kc * 128:(kc + 1) * 128],
                                                          Qc[comp][0:73, qt * 512:(qt + 1) * 512])],
                                [Kb, Qb[qt]], Vt, Vb, 0.125, None, ppT, None)
                (O1, O1b, R1, R1b), (O2, O2b, R2_, R2b) = res
                op(DVE, lambda e: e.reciprocal(out=tA, in_=R1[:, :]), [R1b], [tAb])
                op(DVE, lambda e: e.tensor_tensor(out=tA, in0=O1[:, :], in1=tA, op=ALU.mult), [O1b, tAb], [tAb])
                op(DVE, lambda e: e.reciprocal(out=tB, in_=R2_[:, :]), [R2b], [tBb])
                op(DVE, lambda e: e.tensor_tensor(out=tB, in0=O2[:, :], in1=tB, op=ALU.mult), [O2b, tBb], [tBb])
                op(DVE, lambda e: e.scalar_tensor_tensor(out=tA, in0=tB, scalar=nlam[:, l:l + 1], in1=tA,
                                                         op0=ALU.mult, op1=ALU.add), [tAb, tBb, cb], [tAb])
                op(DVE, lambda e: e.tensor_tensor(out=tC, in0=tA, in1=tA, op=ALU.mult), [tAb], [tCb])
                (X, Xb) = psbank()
                mm([(X[:, :], K.ones_bf[:, :], tC, True, True)], [tCb, cb], [Xb])
                op(ACT, lambda e: e.activation(out=tB, in_=X[:, :], func=AF.Sqrt, bias=1e-6, scale=1.0 / 128),
                   [Xb], [tBb])
                op(DVE, lambda e: e.reciprocal(out=tB, in_=tB), [tBb], [tBb])
                op(DVE, lambda e: e.tensor_tensor(out=tA, in0=tA, in1=tB, op=ALU.mult), [tAb, tBb], [tAb])
                op(DVE, lambda e: e.tensor_scalar(out=OT[:, hd, qt * 512:(qt + 1) * 512], in0=tA,
                                                  scalar1=sub_s[:, l:l + 1], scalar2=1.0 - LAM_INIT[l],
                                                  op0=ALU.mult, op1=ALU.mult), [tAb, cb], [OTb[qt]])
                K.pinned.clear()
        barrier()
        A.off = m0
        if stop == "da" and l == stop_layer:
            return finish()

        m0 = A.off
        cqn = A.alloc([128, 4, NT], BF16)
        cqnb = Buf("cqn")
        ckv_all = A.alloc([128, 2, NKEY], BF16)
        ckvb = Buf("ckv")
        kr_all = A.alloc([128, NKEY], BF16)
        krb = Buf("kr")
        rc = A.alloc([64, 512], F32)
        rs = A.alloc([64, 512], F32)
        rcb = Buf("rope")
        tA = A.alloc([128, 512], F32)
        tB = A.alloc([128, 512], F32)
        tC = A.alloc([128, 512], BF16)
        tAb, tBb, tCb = Buf("tA"), Buf("tB"), Buf("tC")
        m1 = A.off
        p128 = Pool([128, 16, 128], BF16, 2, "w128")
        p64 = Pool([128, 16, 64], BF16, 3, "w64")
        pkf = Pool([128, 512], F32, 2, "kf")
        dma(GP, kr_all[64:73, :], kmask, writes=[krb])
        dma(GP, ckv_all[:, :, 2048:NKEY], c_ckv[l], writes=[ckvb])
        dma(GP, kr_all[0:64, 2048:NKEY], c_kr[l], writes=[krb])

        def rms_blocks(nblk, blk0, qt, gcol, dst_of, dst_b, out_dst):
            banks = []
            for c in range(nblk):
                wp, wpb = loadw(p128, w128[l, blk0 + c])
                (Pq, Pqb) = psbank(pin=True)
                mm([(Pq[:, :], wp[:, kc, :], hT[:, kc, qt * 512:(qt + 1) * 512], kc == 0, kc == 15)
                    for kc in range(16)], [wpb, hTb[qt]], [Pqb])
                banks.append((Pq, Pqb))
            (X, Xb) = psbank()
            for c in range(nblk):
                op(ACT, lambda e, c=c: e.activation(out=tC, in_=banks[c][0][:, :], func=AF.Square), [banks[c][1]], [tCb])
                mm([(X[:, :], K.ones_bf[:, :], tC, c == 0, c == nblk - 1)], [tCb, cb], [Xb])
            op(ACT, lambda e: e.activation(out=tB, in_=X[:, :], func=AF.Sqrt, bias=1e-6, scale=1.0 / (128 * nblk)),
               [Xb], [tBb])
            op(DVE, lambda e: e.reciprocal(out=tB, in_=tB), [tBb], [tBb])
            for c in range(nblk):
                op(DVE, lambda e, c=c: e.tensor_tensor(out=tA, in0=banks[c][0][:, :], in1=tB, op=ALU.mult),
                   [banks[c][1], tBb], [tAb])
                if out_dst is None:
                    op(DVE, lambda e, c=c: e.tensor_scalar(out=dst_of(c), in0=tA, scalar1=gcol(c), scalar2=None,
                                                           op0=ALU.mult), [tAb, cb], [dst_b])
                else:
                    kf, kfb = pkf.get()
                    op(DVE, lambda e, c=c: e.tensor_scalar(out=kf, in0=tA, scalar1=gcol(c), scalar2=None,
                                                           op0=ALU.mult), [tAb, cb], [kfb])
                    dma(SP, out_dst(c), kf, reads=[kfb])
                    op(ACT, lambda e, c=c: e.activation(out=dst_of(c), in_=kf, func=AF.Copy), [kfb], [dst_b])
            K.pinned.clear()

        for qt in range(4):
            rms_blocks(4, 0, qt, lambda c: gq_s[:, l * 4 + c:l * 4 + c + 1],
                       lambda c, qt=qt: cqn[:, c, qt * 512:(qt + 1) * 512], cqnb, None)
            rms_blocks(2, 4, qt, lambda c: gkv_s[:, l * 2 + c:l * 2 + c + 1],
                       lambda c, qt=qt: ckv_all[:, c, qt * 512:(qt + 1) * 512], ckvb,
                       lambda c, qt=qt: o_ckv[l, c, :, qt * 512:(qt + 1) * 512])
            load_rope(qt)
            kf = pkf.get()
            rope_proj(l, 32, qt, lambda kc, qt=qt: hT[:, kc, qt * 512:(qt + 1) * 512], 16, w64[l, 32], w64[l, 33],
                      p64, 64, kr_all[0:64, qt * 512:(qt + 1) * 512], krb, out_f=kf, rhs_reads=[hTb[qt]])
            dma(SP, o_kr[l, :, qt * 512:(qt + 1) * 512], kf[0][0:64, :], reads=[kf[1]])
        barrier()
        A.off = m1
        kn = A.alloc([128, NKEY], BF16)
        knb = Buf("kn")
        Vt = A.alloc([128, 18, 128], BF16)
        Vb = Buf("V")
        qn = A.alloc([128, NT], BF16)
        qnb = [Buf("qn%d" % i) for i in range(4)]
        qr = A.alloc([128, NT], BF16)
        qrb = [Buf("qr%d" % i) for i in range(4)]
        dma(GP, qr[64:73, :], qmask, writes=qrb)
        ppT = Pool([128, 512], BF16, 4, "pT")
        pwn = Pool([128, 4, 128], BF16, 2, "wuqn")
        pwr = Pool([128, 4, 64], BF16, 2, "wuqr")
        pwk = Pool([128, 2, 128], BF16, 2, "wukv")
        for hd in range(8):
            wk, wkb = loadw(pwk, wukv[l, hd, 0])
            wv, wvb = loadw(pwk, wukv[l, hd, 1])
            for t5 in range(5):
                n = 512 if t5 < 4 else 256
                (Pk, Pkb) = psbank()
                mm([(Pk[:, 0:n], wk[:, fc, :], ckv_all[:, fc, t5 * 512:t5 * 512 + n], fc == 0, fc == 1)
                    for fc in range(2)], [wkb, ckvb], [Pkb])
                op(ACT, lambda e: e.activation(out=kn[:, t5 * 512:t5 * 512 + n], in_=Pk[:, 0:n], func=AF.Copy),
                   [Pkb], [knb])
            for g in range(5):
                ns = 4 if g < 4 else 2
                (Pv, Pvb) = psbank()
                mms = []
                for s in range(ns):
                    tcn = g * 4 + s
                    for fc in range(2):
                        mms.append((Pv[:, s * 128:(s + 1) * 128], ckv_all[:, fc, tcn * 128:(tcn + 1) * 128],
                                    wv[:, fc, :], fc == 0, fc == 1))
                mm(mms, [wvb, ckvb], [Pvb])
                op(ACT, lambda e: e.activation(out=Vt[:, g * 4:g * 4 + ns, :],
                                               in_=Pv[:, 0:ns * 128].rearrange("p (a b) -> p a b", a=ns),
                                               func=AF.Copy), [Pvb], [Vb])
            wn, wnb = loadw(pwn, wuq_n[l, hd])
            wrp, wrpb = pwr.get()
            dma(GP, wrp, wuq_r[l, hd, 0], writes=[wrpb])
            wrr, wrrb = pwr.get()
            dma(GP, wrr, wuq_r[l, hd, 1], writes=[wrrb])
            for qt in range(4):
                (Pq, Pqb) = psbank()
                mm([(Pq[:, :], wn[:, kc, :], cqn[:, kc, qt * 512:(qt + 1) * 512], kc == 0, kc == 3)
                    for kc in range(4)], [wnb, cqnb], [Pqb])
                op(ACT, lambda e: e.activation(out=qn[:, qt * 512:(qt + 1) * 512], in_=Pq[:, :], func=AF.Copy),
                   [Pqb], [qnb[qt]])
                load_rope(qt)
                (Pa, Pab) = psbank()
                (Pb, Pbb) = psbank()
                mm([(Pa[0:64, :], wrp[:, kc, :], cqn[:, kc, qt * 512:(qt + 1) * 512], kc == 0, kc == 3)
                    for kc in range(4)], [wrpb, cqnb], [Pab])
                mm([(Pb[0:64, :], wrr[:, kc, :], cqn[:, kc, qt * 512:(qt + 1) * 512], kc == 0, kc == 3)
                    for kc in range(4)], [wrrb, cqnb], [Pbb])
                op(DVE, lambda e: e.tensor_tensor(out=tA[0:64, :], in0=Pa[0:64, :], in1=rc, op=ALU.mult),
                   [Pab, rcb], [tAb])
                op(DVE, lambda e: e.tensor_tensor(out=tB[0:64, :], in0=Pb[0:64, :], in1=rs, op=ALU.mult),
                   [Pbb, rcb], [tBb])
                op(DVE, lambda e: e.tensor_tensor(out=qr[0:64, qt * 512:(qt + 1) * 512], in0=tA[0:64, :],
                                                  in1=tB[0:64, :], op=ALU.add), [tAb, tBb], [qrb[qt]])
                res = attention(1, list(range(18)),
                                lambda comp, kc, qt=qt: [(kn[:, kc * 128:(kc + 1) * 128],
                                                          qn[:, qt * 512:(qt + 1) * 512]),
                                                         (kr_all[0:73, kc * 128:(kc + 1) * 128],
                                                          qr[0:73, qt * 512:(qt + 1) * 512])],
                                [knb, qnb[qt], krb, qrb[qt]], Vt, Vb, 192.0 ** -0.5, None, ppT, None)
                (O1, O1b, R1, R1b) = res[0]
                op(DVE, lambda e: e.reciprocal(out=tA, in_=R1[:, :]), [R1b], [tAb])
                op(DVE, lambda e: e.tensor_tensor(out=OT[:, 4 + hd, qt * 512:(qt + 1) * 512], in0=O1[:, :], in1=tA,
                                                  op=ALU.mult), [O1b, tAb], [OTb[qt]])
                K.pinned.clear()
        barrier()
        A.off = m0
        if stop == "mla" and l == stop_layer:
            return finish()

        m0 = A.off
        qn = A.alloc([128, 512], BF16)
        qnb = Buf("qn")
        kn = A.alloc([128, NKEY], BF16)
        knb = Buf("kn")
        Vt = A.alloc([128, 18, 128], BF16)
        Vb = Buf("V")
        tA = A.alloc([128, 512], F32)
        tAb = Buf("tA")
        pbias = Pool([128, 512], F32, 2, "bias")
        ptf = Pool([128, 512], F32, 2, "tf")
        ppT = Pool([128, 512], BF16, 4, "pT")
        p128 = Pool([128, 16, 128], BF16, 2, "w128")
        pkf = Pool([128, 512], F32, 2, "kf")
        for hd in range(4):
            wk, wkb = loadw(p128, w128[l, 10 + hd])
            for qt in range(4):
                (Pk, Pkb) = psbank()
                mm([(Pk[:, :], wk[:, kc, :], hT[:, kc, qt * 512:(qt + 1) * 512], kc == 0, kc == 15)
                    for kc in range(16)], [wkb, hTb[qt]], [Pkb])
                kf, kfb = pkf.get()
                op(DVE, lambda e: e.tensor_copy(out=kf, in_=Pk[:, :]), [Pkb], [kfb])
                dma(SP, o_nak[l, hd, :, qt * 512:(qt + 1) * 512], kf, reads=[kfb])
                op(ACT, lambda e: e.activation(out=kn[:, qt * 512:(qt + 1) * 512], in_=kf, func=AF.Copy),
                   [kfb], [knb])
            dma(GP, kn[:, 2048:NKEY], c_nak[l, hd], writes=[knb])
            wv, wvb = loadw(p128, w128[l, 18 + hd])
            proj_v_tm(wv, wvb, lambda kc, tcn: hT[:, kc, tcn * 128:(tcn + 1) * 128], 16, hTb, hd, o_nav[l, hd])
            dma(GP, Vt[:, 16:18, :], c_nav[l, hd], writes=[Vb])
            wq, wqb = loadw(p128, w128[l, 6 + hd])
            for qt in range(4):
                (Pq, Pqb) = psbank()
                mm([(Pq[:, :], wq[:, kc, :], hT[:, kc, qt * 512:(qt + 1) * 512], kc == 0, kc == 15)
                    for kc in range(16)], [wqb, hTb[qt]], [Pqb])
                op(ACT, lambda e: e.activation(out=qn, in_=Pq[:, :], func=AF.Copy), [Pqb], [qnb])

                def bias_fn(kc, qt=qt, hd=hd):
                    bt, btb = pbias.get()
                    dma(SP, bt, nabias[l, hd, NA_PAIR[(qt, kc)]], writes=[btb])
                    return bt, btb

                res = attention(1, NA_CH[qt], lambda comp, kc: [(kn[:, kc * 128:(kc + 1) * 128], qn[:, :])],
                                [knb, qnb], Vt, Vb, 128.0 ** -0.5, bias_fn, ppT, ptf)
                (O1, O1b, R1, R1b) = res[0]
                op(DVE, lambda e: e.reciprocal(out=tA, in_=R1[:, :]), [R1b], [tAb])
                op(DVE, lambda e: e.tensor_tensor(out=OT[:, 12 + hd, qt * 512:(qt + 1) * 512], in0=O1[:, :], in1=tA,
                                                  op=ALU.mult), [O1b, tAb], [OTb[qt]])
                K.pinned.clear()
        barrier()
        A.off = m0
        if stop == "na" and l == stop_layer:
            return finish()

        m0 = A.off
        tt = A.alloc([128, 16, 512], F32)
        ttb = Buf("tt")
        pzb = Pool([128, 512], BF16, 4, "zb")
        tM = (A.alloc([128, 512], F32), Buf("M"))
        tV = (A.alloc([128, 512], F32), Buf("V"))
        p128 = Pool([128, 16, 128], BF16, 3, "wo")
        lg = A.alloc([128, 8], F32)
        lg2 = A.alloc([128, 8], F32)
        e1 = A.alloc([128, 8], F32)
        e2 = A.alloc([128, 8], F32)
        sm = A.alloc([128, 8], F32)
        smb = Buf("sm")
        if is_moe:
            dma(SP, wr, router[fi].rearrange("p a b -> p (a b)"), writes=[wrb])
            for c in range(16):
                op(DVE, lambda e, c=c: e.tensor_scalar(out=wrA[:, c * 8:(c + 1) * 8], in0=wr[:, c * 8:(c + 1) * 8],
                                                       scalar1=col(cvec(4), c), scalar2=None, op0=ALU.mult),
                   [wrb, coefb], [wrb])
                op(DVE, lambda e, c=c: e.tensor_scalar(out=wrB[:, c * 8:(c + 1) * 8], in0=wr[:, c * 8:(c + 1) * 8],
                                                       scalar1=col(cvec(5), c), scalar2=None, op0=ALU.mult),
                   [wrb, coefb], [wrb])
            (Pc, Pcb) = psbank()
            mm([(Pc[:, 0:8], ones_f[:, :], wrB[:, c * 8:(c + 1) * 8], c == 0, c == 15) for c in range(16)],
               [wrb, cb], [Pcb])
            op(DVE, lambda e: e.tensor_copy(out=cstb, in_=Pc[:, 0:8]), [Pcb], [wrb])
        for qt in range(4):
            dma(SP, tt, src[:, :, qt * 512:(qt + 1) * 512], reads=[tSb[qt]], writes=[ttb])
            for j in range(16):
                wo, wob = loadw(p128, w_out[l, j])
                (Pm, Pmb) = psbank()
                mm([(Pm[:, :], wo[:, kc, :], OT[:, kc, qt * 512:(qt + 1) * 512], kc == 0, kc == 15)
                    for kc in range(16)], [wob, OTb[qt]], [Pmb])
                affine(ACT, tt[:, j, :], tt[:, j, :], col(cvec(2), j), col(cvec(3), j), [ttb, coefb], [ttb])
                op(DVE, lambda e, j=j: e.scalar_tensor_tensor(out=tt[:, j, :], in0=Pm[:, :], scalar=col(g1v, j),
                                                              in1=tt[:, j, :], op0=ALU.mult, op1=ALU.add),
                   [Pmb, ttb, cb], [ttb])
            layernorm(lambda c: tt[:, c, :], ttb, pzb, tM, tV)
            for c in range(16):
                affine(alt_eng(), hT[:, c, qt * 512:(qt + 1) * 512], tt[:, c, :], col(cvec(4), c), col(cvec(5), c),
                       [ttb, coefb], [hTb[qt]])
            if is_moe:
                for sb in range(4):
                    (Pl, Plb) = psbank()
                    mm([(Pl[:, 0:8], tt[:, c, sb * 128:(sb + 1) * 128], wrA[:, c * 8:(c + 1) * 8], c == 0, c == 15)
                        for c in range(16)], [ttb, wrb], [Plb])
                    go = gate_tm[:, (qt * 4 + sb) * 8:(qt * 4 + sb + 1) * 8]
                    op(DVE, lambda e: e.tensor_tensor(out=lg, in0=Pl[:, 0:8], in1=cstb, op=ALU.add), [Plb, wrb], [smb])
                    op(DVE, lambda e: e.tensor_reduce(out=sm[:, 0:1], in_=lg, axis=mybir.AxisListType.X, op=ALU.max),
                       [smb], [smb])
                    op(DVE, lambda e: e.tensor_scalar(out=e1, in0=lg, scalar1=sm[:, 0:1], scalar2=None,
                                                      op0=ALU.is_equal), [smb], [smb])
                    op(DVE, lambda e: e.scalar_tensor_tensor(out=lg2, in0=e1, scalar=-1e30, in1=lg, op0=ALU.mult,
                                                             op1=ALU.add), [smb], [smb])
                    op(DVE, lambda e: e.tensor_reduce(out=sm[:, 1:2], in_=lg2, axis=mybir.AxisListType.X, op=ALU.max),
                       [smb], [smb])
                    op(DVE, lambda e: e.tensor_scalar(out=e2, in0=lg2, scalar1=sm[:, 1:2], scalar2=None,
                                                      op0=ALU.is_equal), [smb], [smb])
                    op(DVE, lambda e: e.tensor_tensor(out=sm[:, 2:3], in0=sm[:, 1:2], in1=sm[:, 0:1], op=ALU.subtract),
                       [smb], [smb])
                    op(ACT, lambda e: e.activation(out=sm[:, 3:4], in_=sm[:, 2:3], func=AF.Exp), [smb], [smb])
                    op(DVE, lambda e: e.tensor_scalar(out=sm[:, 4:5], in0=sm[:, 3:4], scalar1=1.0, scalar2=None,
                                                      op0=ALU.add), [smb], [smb])
                    op(DVE, lambda e: e.reciprocal(out=sm[:, 5:6], in_=sm[:, 4:5]), [smb], [smb])
                    op(DVE, lambda e: e.tensor_tensor(out=sm[:, 6:7], in0=sm[:, 3:4], in1=sm[:, 5:6], op=ALU.mult),
                       [smb], [smb])
                    op(DVE, lambda e: e.tensor_scalar(out=e1, in0=e1, scalar1=sm[:, 5:6], scalar2=None, op0=ALU.mult),
                       [smb], [smb])
                    op(DVE, lambda e: e.scalar_tensor_tensor(out=go, in0=e2, scalar=sm[:, 6:7], in1=e1, op0=ALU.mult,
                                                             op1=ALU.add), [smb, gateb], [gateb])
            for c in range(16):
                affine(alt_eng(), tt[:, c, :], tt[:, c, :], col(cvec(6), c), col(cvec(7), c), [ttb, coefb], [ttb])
            dma(SP, tS[:, :, qt * 512:(qt + 1) * 512], tt, reads=[ttb], writes=[tSb[qt]])
        barrier()
        A.off = m0
        if stop == "e" and l == stop_layer:
            return finish()

        m0 = A.off
        p13 = Pool([128, 16, 128], BF16, 4, "w13")
        p2 = Pool([128, 2048], BF16, GF + 1, "w2")
        pu = Pool([128, 1024], BF16, 2 * GF, "u")
        ptf = Pool([128, 512], F32, 2, "tf")
        gbc = A.alloc([128, 1024], F32)
        gbcb = Buf("gbc")
        grep = A.alloc([128, 128], F32)
        grepb = Buf("grep")
        if is_moe:
            chunks = [(e_, f) for e_ in range(n_exp) for f in range(22)]
        else:
            chunks = [(0, f) for f in range(43)]
        for ps_ in range(2):
            for tl in range(2):
                dma(SP, acc[:, :, tl * 512:(tl + 1) * 512], tS[:, :, ps_ * 1024 + tl * 512: ps_ * 1024 + (tl + 1) * 512],
                    reads=[tSb[ps_ * 2 + tl]], writes=[accb[tl]])
            group = []
            cur_e = -1
            for ci, (e_, f) in enumerate(chunks):
                if is_moe and e_ != cur_e:
                    cur_e = e_
                    for blk in range(8):
                        gb = (ps_ * 8 + blk) * 8 + e_
                        op(DVE, lambda e, gb=gb: e.tensor_copy(out=grep, in_=gate_tm[:, gb:gb + 1].to_broadcast([128, 128])),
                           [gateb], [grepb])
                        (Pg, Pgb) = psbank()
                        mm([(Pg[:, 0:128], grep, ident, True, True)], [grepb, cb], [Pgb])
                        op(ACT, lambda e, blk=blk: e.activation(out=gbc[:, blk * 128:(blk + 1) * 128], in_=Pg[:, 0:128],
                                                                func=AF.Copy), [Pgb], [gbcb])
                if is_moe:
                    s1, s3, s2 = moe_w1[fi, e_, f], moe_w3[fi, e_, f], moe_w2[fi, e_, f]
                else:
                    s1, s3, s2 = ffn_w1[fi, f], ffn_w3[fi, f], ffn_w2[fi, f]
                w1b, w1bb = loadw(p13, s1)
                w3b, w3bb = loadw(p13, s3)
                w2b, w2bb = loadw(p2, s2)
                u, ub = pu.get()
                for tl in range(2):
                    t0 = ps_ * 1024 + tl * 512
                    (G1, G1b) = psbank()
                    (G3, G3b) = psbank()
                    mm([(G1[:, :], w1b[:, kc, :], hT[:, kc, t0:t0 + 512], kc == 0, kc == 15) for kc in range(16)],
                       [w1bb, hTb[ps_ * 2 + tl]], [G1b])
                    mm([(G3[:, :], w3b[:, kc, :], hT[:, kc, t0:t0 + 512], kc == 0, kc == 15) for kc in range(16)],
                       [w3bb, hTb[ps_ * 2 + tl]], [G3b])
                    tf, tfb = ptf.get()
                    op(ACT, lambda e: e.activation(out=tf, in_=G1[:, :], func=AF.Silu), [G1b], [tfb])
                    if is_moe:
                        op(DVE, lambda e: e.tensor_tensor(out=tf, in0=G3[:, :], in1=tf, op=ALU.mult), [G3b, tfb], [tfb])
                        op(DVE, lambda e: e.tensor_tensor(out=u[:, tl * 512:(tl + 1) * 512], in0=tf,
                                                          in1=gbc[:, tl * 512:(tl + 1) * 512], op=ALU.mult),
                           [tfb, gbcb], [ub])
                    else:
                        op(DVE, lambda e: e.tensor_tensor(out=u[:, tl * 512:(tl + 1) * 512], in0=G3[:, :], in1=tf,
                                                          op=ALU.mult), [G3b, tfb], [ub])
                group.append((w2b, w2bb, u, ub))
                last_of_e = (ci + 1 == len(chunks)) or (chunks[ci + 1][0] != e_)
                if len(group) == GF or last_of_e:
                    for tl in range(2):
                        for j in range(16):
                            (Pw, Pwb) = psbank()
                            mm([(Pw[:, :], g_[0][:, j * 128:(j + 1) * 128], g_[2][:, tl * 512:(tl + 1) * 512],
                                 gi == 0, gi == len(group) - 1) for gi, g_ in enumerate(group)],
                               [g_[1] for g_ in group] + [g_[3] for g_ in group], [Pwb])
                            op(DVE, lambda e, j=j, tl=tl: e.scalar_tensor_tensor(
                                out=acc[:, j, tl * 512:(tl + 1) * 512], in0=Pw[:, :], scalar=col(g2v, j),
                                in1=acc[:, j, tl * 512:(tl + 1) * 512], op0=ALU.mult, op1=ALU.add),
                               [Pwb, accb[tl], cb], [accb[tl]])
                    group = []
            m2 = A.off
            pzb = Pool([128, 512], BF16, 4, "zb")
            tM = (A.alloc([128, 512], F32), Buf("M"))
            tV = (A.alloc([128, 512], F32), Buf("V"))
            for tl in range(2):
                qt = ps_ * 2 + tl
                layernorm(lambda c, tl=tl: acc[:, c, tl * 512:(tl + 1) * 512], accb[tl], pzb, tM, tV)
                if l == nlayers - 1:
                    for c in range(16):
                        affine(alt_eng(), acc[:, c, tl * 512:(tl + 1) * 512], acc[:, c, tl * 512:(tl + 1) * 512],
                               lnp_s[:, l * 64 + 32 + c:l * 64 + 33 + c], lnp_s[:, l * 64 + 48 + c:l * 64 + 49 + c],
                               [accb[tl], cb], [accb[tl]])
                    dma(SP, yT[:, :, qt * 512:(qt + 1) * 512], acc[:, :, tl * 512:(tl + 1) * 512], reads=[accb[tl]])
                else:
                    dma(SP, tS[:, :, qt * 512:(qt + 1) * 512], acc[:, :, tl * 512:(tl + 1) * 512], reads=[accb[tl]],
                        writes=[tSb[qt]])
            barrier()
            A.off = m2
        barrier()
        A.off = m0

    barrier()
    return nc


def _fm(x):
    T = x.shape[0]
    return np.ascontiguousarray(x.reshape(T, 16, 128).transpose(2, 1, 0))


def _pcol(v):
    return np.ascontiguousarray(v.reshape(-1, 128).T)


def _wblk(W, c0, m):
    Kd = W.shape[0]
    return np.ascontiguousarray(W[:, c0:c0 + m].reshape(Kd // 128, 128, m).transpose(1, 0, 2))


def _rot_perm():
    perm = np.zeros(64, np.int64)
    sign = np.zeros(64, np.float32)
    for a in range(2):
        for i in range(16):
            perm[a * 32 + i] = a * 32 + 16 + i
            sign[a * 32 + i] = -1.0
            perm[a * 32 + 16 + i] = a * 32 + i
            sign[a * 32 + 16 + i] = 1.0
    return perm, sign


def _rope_tables(n):
    t = np.arange(n)
    row = (t // 64).astype(np.float32)
    colv = (t % 64).astype(np.float32)
    inv = (10000.0 ** (-np.arange(0, 32, 2, dtype=np.float32) / 32)).astype(np.float32)
    ar = row[:, None] * inv[None, :]
    ac = colv[:, None] * inv[None, :]
    ang = np.concatenate([ar, ar, ac, ac], axis=-1)
    return np.cos(ang).astype(np.float32), np.sin(ang).astype(np.float32)


def _na_bias_sample(rpb_l):
    out = np.full((4, NPAIR, 128, 512), -30000.0, np.float32)
    r = np.arange(32)
    row_start = np.clip(r - 4, 0, 24)
    cq = np.arange(64)
    col_start = np.clip(cq - 8, 0, 48)
    for (qt, kc), pi in NA_PAIR.items():
        if kc >= 16:
            out[:, pi] = 0.0
            continue
        kk = kc * 128 + np.arange(128)
        qq = qt * 512 + np.arange(512)
        kr_, kcol = kk // 64, kk % 64
        qr_, qcol = qq // 64, qq % 64
        rs_ = row_start[qr_]
        cs_ = col_start[qcol]
        valid = ((kr_[:, None] >= rs_[None, :]) & (kr_[:, None] < rs_[None, :] + 8)
                 & (kcol[:, None] >= cs_[None, :]) & (kcol[:, None] < cs_[None, :] + 16))
        roff = np.clip(kr_[:, None] - qr_[None, :] + 7, 0, 14)
        coff = np.clip(kcol[:, None] - qcol[None, :], -15, 15) + 15
        g = rpb_l[:, roff, coff]
        out[:, pi] = np.where(valid[None], g, np.float32(-30000.0))
    return out


def _na_bias_prompt():
    out = np.full((NPAIR, 128, 512), -30000.0, np.float32)
    for (qt, kc), pi in NA_PAIR.items():
        if kc >= 16:
            continue
        kk = kc * 128 + np.arange(128)
        qq = qt * 512 + np.arange(512)
        out[pi] = np.where((kk[:, None] // 256) == (qq[None, :] // 256), np.float32(0.0), np.float32(-30000.0))
    return out


_NC_CACHE = {}


def prep(inp, Lw=L):
    f = lambda k: np.asarray(inp[k], dtype=np.float32)
    perm, sign = _rot_perm()
    w_in = f("w_in")
    shared = {}
    wa = f("w_ada")
    shared["w_ada"] = np.ascontiguousarray(wa.reshape(L, 16, 128, 96, 128).transpose(0, 3, 2, 1, 4))
    shared["b_ada"] = np.ascontiguousarray(f("b_ada").reshape(L, 96, 128).transpose(0, 2, 1))
    w64 = np.zeros((L, 34, 128, 16, 64), np.float32)
    w128 = np.zeros((L, 22, 128, 16, 128), np.float32)
    OFF = {"dq": 0, "dk": 512, "dv": 1024, "cq": 1536, "ckv": 2048, "kr": 2304, "nq": 2368, "nk": 2880, "nv": 3392}
    for l in range(L):
        W = w_in[l]
        for sec, b0 in (("dq", 0), ("dk", 16)):
            for hd in range(4):
                for c in range(2):
                    c0 = OFF[sec] + hd * 128 + c * 64
                    blkW = W[:, c0:c0 + 64]
                    w64[l, b0 + hd * 4 + c * 2] = blkW.reshape(16, 128, 64).transpose(1, 0, 2)
                    w64[l, b0 + hd * 4 + c * 2 + 1] = blkW[:, perm].reshape(16, 128, 64).transpose(1, 0, 2)
        blkW = W[:, OFF["kr"]:OFF["kr"] + 64]
        w64[l, 32] = blkW.reshape(16, 128, 64).transpose(1, 0, 2)
        w64[l, 33] = blkW[:, perm].reshape(16, 128, 64).transpose(1, 0, 2)
        for i in range(4):
            w128[l, i] = _wblk(W, OFF["cq"] + i * 128, 128)
            w128[l, 6 + i] = _wblk(W, OFF["nq"] + i * 128, 128)
            w128[l, 10 + i] = _wblk(W, OFF["nk"] + i * 128, 128)
            w128[l, 14 + i] = _wblk(W, OFF["dv"] + i * 128, 128)
            w128[l, 18 + i] = _wblk(W, OFF["nv"] + i * 128, 128)
        for i in range(2):
            w128[l, 4 + i] = _wblk(W, OFF["ckv"] + i * 128, 128)
    shared["w64"] = w64
    shared["w128"] = w128
    wuq = f("mla_wuq")
    wukv = f("mla_wukv")
    wuq_n = np.zeros((L, 8, 128, 4, 128), np.float32)
    wuq_r = np.zeros((L, 8, 2, 128, 4, 64), np.float32)
    wukv_r = np.zeros((L, 8, 2, 128, 2, 128), np.float32)
    for l in range(L):
        for hd in range(8):
            wuq_n[l, hd] = _wblk(wuq[l], hd * 192, 128)
            rb = wuq[l][:, hd * 192 + 128: hd * 192 + 192]
            wuq_r[l, hd, 0] = rb.reshape(4, 128, 64).transpose(1, 0, 2)
            wuq_r[l, hd, 1] = rb[:, perm].reshape(4, 128, 64).transpose(1, 0, 2)
            wukv_r[l, hd, 0] = _wblk(wukv[l], hd * 256, 128)
            wukv_r[l, hd, 1] = _wblk(wukv[l], hd * 256 + 128, 128)
    shared["wuq_n"] = wuq_n
    shared["wuq_r"] = wuq_r
    shared["wukv"] = wukv_r
    wo = f("w_out")
    shared["w_out"] = np.ascontiguousarray(wo.reshape(L, 16, 128, 16, 128).transpose(0, 3, 2, 1, 4))
    for nm in ("ffn_w1", "ffn_w3"):
        shared[nm] = np.ascontiguousarray(f(nm).reshape(2, 16, 128, 43, 128).transpose(0, 3, 2, 1, 4))
    shared["ffn_w2"] = np.ascontiguousarray(f("ffn_w2").reshape(2, 43, 128, 2048))
    for nm in ("moe_w1", "moe_w3"):
        shared[nm] = np.ascontiguousarray(f(nm).reshape(2, 8, 16, 128, 22, 128).transpose(0, 1, 4, 3, 2, 5))
    shared["moe_w2"] = np.ascontiguousarray(f("moe_w2").reshape(2, 8, 22, 128, 2048))
    shared["router"] = np.ascontiguousarray(f("moe_router").reshape(2, 16, 128, 8).transpose(0, 2, 1, 3))
    lnp = np.zeros((128, L * 64), np.float32)
    for l in range(L):
        for i, nm in enumerate(("ln1_g", "ln1_b", "ln2_g", "ln2_b")):
            lnp[:, l * 64 + i * 16:l * 64 + (i + 1) * 16] = _pcol(f(nm)[l])
    shared["lnp"] = lnp
    shared["gq"] = np.concatenate([_pcol(f("mla_gq")[l]) for l in range(L)], axis=1)
    shared["gkv"] = np.concatenate([_pcol(f("mla_gkv")[l]) for l in range(L)], axis=1)
    shared["subln"] = np.concatenate([_pcol(f("da_subln")[l]) for l in range(L)], axis=1)
    lamp = np.zeros((64, 4 * L), np.float32)
    for l in range(L):
        for i, nm in enumerate(("da_lq1", "da_lk1", "da_lq2", "da_lk2")):
            lamp[:, 4 * l + i] = f(nm)[l]
    shared["lamp"] = lamp
    shared["ident"] = np.eye(128, dtype=np.float32)

    cos, sin = _rope_tables(2048)
    ropec_s = np.ascontiguousarray(cos.T)
    ropes_s = np.ascontiguousarray((sin * sign[None, :]).T)
    ropec_p = np.ones((64, 2048), np.float32)
    ropes_p = np.zeros((64, 2048), np.float32)
    km_p = np.zeros((9, NKEY), np.float32)
    qm_p = np.zeros((9, NT), np.float32)
    for j in range(8):
        km_p[j, j * 256:(j + 1) * 256] = 1.0
        qm_p[j, :] = NBIG
        qm_p[j, j * 256:(j + 1) * 256] = 0.0
    km_p[8, 2048:] = 1.0
    qm_p[8, :] = NBIG
    km_s = np.zeros((9, NKEY), np.float32)
    km_s[0, :2048] = 1.0
    km_s[8, 2048:] = 1.0
    qm_s = np.zeros((9, NT), np.float32)
    nab_p1 = _na_bias_prompt()
    nab_p = np.ascontiguousarray(np.broadcast_to(nab_p1[None, None], (L, 4, NPAIR, 128, 512)))
    rpb = f("na_rpb")
    xp = f("x_prompt")
    xs = f("x_sample")
    c = f("c")
    cctx = f("c_ctx")
    zc = {
        "c_dak": np.zeros((L, 4, 2, 64, 256), np.float32), "c_dav": np.zeros((L, 4, 128, 2, 128), np.float32),
        "c_ckv": np.zeros((L, 128, 2, 256), np.float32), "c_kr": np.zeros((L, 64, 256), np.float32),
        "c_nak": np.zeros((L, 4, 128, 256), np.float32), "c_nav": np.zeros((L, 4, 128, 2, 128), np.float32),
    }
    in_maps = []
    for r in range(8):
        m = dict(shared)
        if r in (4, 5):
            b = r - 4
            m["xT"] = _fm(xs[b])
            m["cond"] = _pcol(c[b])
            m["ropec"], m["ropes"], m["kmask"], m["qmask"] = ropec_s, ropes_s, km_s, qm_s
            dk = f("cache_da_k")[b]
            m["c_dak"] = np.ascontiguousarray(dk.transpose(0, 2, 3, 4, 1))
            dv = f("cache_da_v")[b]
            m["c_dav"] = np.ascontiguousarray(dv.reshape(L, 2, 128, 4, 128).transpose(0, 3, 2, 1, 4))
            ck = f("cache_mla_ckv")[b]
            m["c_ckv"] = np.ascontiguousarray(ck.reshape(L, 256, 2, 128).transpose(0, 3, 2, 1))
            m["c_kr"] = np.ascontiguousarray(f("cache_mla_krope")[b].transpose(0, 2, 1))
            nk = f("cache_na_k")[b]
            m["c_nak"] = np.ascontiguousarray(nk.transpose(0, 2, 3, 1))
            nv = f("cache_na_v")[b]
            m["c_nav"] = np.ascontiguousarray(nv.reshape(L, 2, 128, 4, 128).transpose(0, 3, 2, 1, 4))
            m["nabias"] = np.stack([_na_bias_sample(rpb[l]) for l in range(L)], axis=0)
        else:
            rr = r if r < 4 else 0
            m["xT"] = _fm(xp[rr * 8:(rr + 1) * 8].reshape(2048, 2048))
            m["cond"] = _pcol(cctx)
            m["ropec"], m["ropes"], m["kmask"], m["qmask"] = ropec_p, ropes_p, km_p, qm_p
            m.update(zc)
            m["nabias"] = nab_p
        in_maps.append(m)

    return in_maps


def kernel(**inp):
    in_maps = prep(inp)
    if "nc" not in _NC_CACHE:
        _NC_CACHE["nc"] = build()
    nc = _NC_CACHE["nc"]
    res = run_bass_kernel_spmd(nc, in_maps, core_ids=list(range(8)))
    return post(res.results)


def post(R):

    def tm(yT):
        return yT.transpose(2, 1, 0).reshape(2048, 2048)

    y_p = np.concatenate([tm(R[r]["yT"]).reshape(8, 256, 2048) for r in range(4)], axis=0)
    y_s = np.stack([tm(R[4]["yT"]), tm(R[5]["yT"])], axis=0)
    dak = np.concatenate([R[r]["o_dak"].transpose(4, 0, 1, 2, 3).reshape(8, 256, L, 4, 2, 64).transpose(0, 2, 1, 3, 4, 5)
                          for r in range(4)], axis=0)
    dav = np.concatenate([R[r]["o_dav"].transpose(3, 2, 0, 1, 4).reshape(8, 256, L, 4, 128).transpose(0, 2, 1, 3, 4)
                          for r in range(4)], axis=0)
    ckv = np.concatenate([R[r]["o_ckv"].transpose(3, 0, 1, 2).reshape(8, 256, L, 256).transpose(0, 2, 1, 3)
                          for r in range(4)], axis=0)
    kr = np.concatenate([R[r]["o_kr"].transpose(2, 0, 1).reshape(8, 256, L, 64).transpose(0, 2, 1, 3)
                         for r in range(4)], axis=0)
    nak = np.concatenate([R[r]["o_nak"].transpose(3, 0, 1, 2).reshape(8, 256, L, 4, 128).transpose(0, 2, 1, 3, 4)
                          for r in range(4)], axis=0)
    nav = np.concatenate([R[r]["o_nav"].transpose(3, 2, 0, 1, 4).reshape(8, 256, L, 4, 128).transpose(0, 2, 1, 3, 4)
                          for r in range(4)], axis=0)
    outs = (y_p, y_s, dak, dav, ckv, kr, nak, nav)
    return tuple(np.ascontiguousarray(o, dtype=np.float32) for o in outs)
```
